# Optimizing a Trainium2 kernel written in Bass

```python
import math
import jax
import jax.numpy as jnp
from jax import lax
import numpy as np

D_MODEL = 1024
BATCH = 4
SEQ = 4096
DEPTH = 4

GRID_W = 64
CTX_LEN = 256
N_EVEN = (DEPTH + 1) // 2
N_ODD = DEPTH // 2
N_MOD = 6
RMS_EPS = 1e-6
ROPE_BASE = 10000.0
Q_BLOCK = 128

S5_WIDTH = D_MODEL // 2
S5_GROUP = 16
S5_GROUPS = S5_WIDTH // S5_GROUP
S5_STATE = 64
S5_DT_MIN = 1e-3
S5_DT_MAX = 1e-1

MLA_V = 64
MLA_HEADS = (D_MODEL - S5_WIDTH) // MLA_V
MLA_NOPE = 64
MLA_ROPE = 32
MLA_QK = MLA_NOPE + MLA_ROPE
MLA_Q_RANK = D_MODEL // 4
MLA_KV_RANK = D_MODEL // 8
EVEN_IN = S5_WIDTH + MLA_Q_RANK + MLA_KV_RANK + MLA_ROPE
EVEN_MIX = S5_WIDTH + MLA_HEADS * MLA_V

GQA_DIM = 128
GQA_HEADS = D_MODEL // GQA_DIM
GQA_KV_HEADS = GQA_HEADS // 2
GQA_GROUP = GQA_HEADS // GQA_KV_HEADS
ODD_Q = GQA_HEADS * GQA_DIM
ODD_KV = GQA_KV_HEADS * GQA_DIM
ODD_IN = ODD_Q + 2 * ODD_KV
ODD_MIX = GQA_HEADS * GQA_DIM

MOE_GROUPS = 4
MOE_PER_GROUP = 8
MOE_EXPERTS = MOE_GROUPS * MOE_PER_GROUP
MOE_TOP_K = 2
MOE_FF = D_MODEL // 2
MOE_BLOCK = 128

kernel_name = 'hybrid_s5_mla_gqa_hmoe_dit'


def rms_norm(x, g):
    xf = x.astype(jnp.float32)
    y = xf * lax.rsqrt(jnp.mean(xf * xf, axis=-1, keepdims=True) + RMS_EPS)
    return y.astype(x.dtype) * g


def ada_chunks(sc, w, b):
    return jnp.split(sc @ w + b, N_MOD, axis=-1)


def modulate(h, shift, scale):
    return h * (1 + scale) + shift


def rope_tables(row, col, d_rot):
    n_freq = d_rot // 4
    inv = ROPE_BASE ** (-jnp.arange(n_freq, dtype=jnp.float32) / n_freq)
    ang = jnp.stack([row[:, None] * inv, col[:, None] * inv], axis=1)
    return jnp.cos(ang)[:, None], jnp.sin(ang)[:, None]


def apply_rope(x, cos, sin):
    xr = x.reshape(*x.shape[:-1], 2, 2, x.shape[-1] // 4)
    x1, x2 = xr[..., 0, :], xr[..., 1, :]
    cos, sin = cos.astype(x.dtype), sin.astype(x.dtype)
    out = jnp.stack([x1 * cos - x2 * sin, x2 * cos + x1 * sin], axis=-2)
    return out.reshape(x.shape)


def attend_dense(q, k, v, scale):
    s = jnp.einsum('bqkgd,bskd->bkgqs', q, k, preferred_element_type=jnp.float32) * scale
    p = jax.nn.softmax(s, axis=-1).astype(v.dtype)
    return jnp.einsum('bkgqs,bskd->bqkgd', p, v)


def attend_latent(q, k, v, scale):
    b, lq = q.shape[:2]
    nb = lq // Q_BLOCK
    qb = jnp.moveaxis(q.reshape(b, nb, Q_BLOCK, *q.shape[2:]), 1, 0)
    ob = lax.map(lambda qi: attend_dense(qi, k, v, scale), qb)
    return jnp.moveaxis(ob, 0, 1).reshape(b, lq, *ob.shape[3:])


def _cmul(ar, ai, br, bi):
    return ar * br - ai * bi, ar * bi + ai * br


def _s5_combine(e1, e2):
    a1r, a1i, b1r, b1i = e1
    a2r, a2i, b2r, b2i = e2
    ar, ai = _cmul(a2r, a2i, a1r, a1i)
    br, bi = _cmul(a2r, a2i, b1r, b1i)
    return ar, ai, br + b2r, bi + b2i


def s5_direction(u_ctx, u_lat, a_re, a_im, log_dt, b_re, b_im, c_re, c_im, reverse, need_ctx):
    dt = jnp.exp(log_dt)[:, None]
    mag = jnp.exp(dt * a_re)
    th = dt * a_im
    ab_r, ab_i = mag * jnp.cos(th), mag * jnp.sin(th)
    den = a_re * a_re + a_im * a_im
    nr = ab_r - 1
    z_r = (nr * a_re + ab_i * a_im) / den
    z_i = (ab_i * a_re - nr * a_im) / den
    bb_r, bb_i = _cmul(z_r[..., None], z_i[..., None], b_re, b_im)

    def drive(u):
        return (jnp.einsum('blgp,gnp->blgn', u, bb_r), jnp.einsum('blgp,gnp->blgn', u, bb_i))

    def scan(bu_r, bu_i):
        shp = bu_r.shape
        _, _, h_r, h_i = lax.associative_scan(
            _s5_combine, (jnp.broadcast_to(ab_r, shp), jnp.broadcast_to(ab_i, shp), bu_r, bu_i),
            axis=1, reverse=reverse)
        return h_r, h_i

    def readout(h_r, h_i):
        return jnp.einsum('blgn,gpn->blgp', h_r, c_re) - jnp.einsum('blgn,gpn->blgp', h_i, c_im)

    hc_r, hc_i = scan(*drive(u_ctx))
    end = 0 if reverse else -1
    first = -1 if reverse else 0
    carry_r, carry_i = _cmul(ab_r, ab_i, hc_r[:, end], hc_i[:, end])
    bu_r, bu_i = drive(u_lat)
    bu_r = bu_r.at[:, first].add(carry_r)
    bu_i = bu_i.at[:, first].add(carry_i)
    y_lat = readout(*scan(bu_r, bu_i))
    y_ctx = readout(hc_r, hc_i) if need_ctx else None
    return y_lat, y_ctx


def s5_glu(y, u, d_skip, w_glu):
    g = jax.nn.gelu(y + d_skip * u)
    return g * jax.nn.sigmoid(g @ w_glu)


def s5_mixer(u_lat, u_ctx, p, need_ctx):
    b, l, _ = u_lat.shape
    ug_lat = u_lat.reshape(b, l, S5_GROUPS, S5_GROUP)
    ug_ctx = u_ctx.reshape(b, u_ctx.shape[1], S5_GROUPS, S5_GROUP)
    outs = [s5_direction(ug_ctx, ug_lat, p['a_re'][dd], p['a_im'][dd], p['log_dt'][dd],
                         p['b_re'][dd], p['b_im'][dd], p['c_re'][dd], p['c_im'][dd], rev, need_ctx)
            for dd, rev in enumerate((False, True))]
    y_lat = (outs[0][0] + outs[1][0]).reshape(u_lat.shape)
    out_lat = s5_glu(y_lat, u_lat, p['d'], p['w_glu'])
    out_ctx = None
    if need_ctx:
        y_ctx = (outs[0][1] + outs[1][1]).reshape(u_ctx.shape)
        out_ctx = s5_glu(y_ctx, u_ctx, p['d'], p['w_glu'])
    return out_lat, out_ctx


def mla_qkv(cq, ckv, kr, p, rope):
    b, l = cq.shape[:2]
    q = (rms_norm(cq, p['gq']) @ p['w_uq']).reshape(b, l, MLA_HEADS, MLA_QK)
    kv = (rms_norm(ckv, p['gkv']) @ p['w_ukv']).reshape(b, l, MLA_HEADS, MLA_NOPE + MLA_V)
    k_nope, v = kv[..., :MLA_NOPE], kv[..., MLA_NOPE:]
    k_rope = jnp.broadcast_to(kr[:, :, None, :], (b, l, MLA_HEADS, MLA_ROPE))
    k = jnp.concatenate([k_nope, k_rope], axis=-1)
    q = rms_norm(q, p['qn'])
    k = rms_norm(k, p['kn'])
    if rope is not None:
        cos, sin = rope
        q = jnp.concatenate([q[..., :MLA_NOPE], apply_rope(q[..., MLA_NOPE:], cos, sin)], axis=-1)
        k = jnp.concatenate([k[..., :MLA_NOPE], apply_rope(k[..., MLA_NOPE:], cos, sin)], axis=-1)
    return q, k, v


def mla_mixer(lat_parts, ctx_parts, p, rope, need_ctx):
    b, l = lat_parts[0].shape[:2]
    scale = MLA_QK ** -0.5
    q_l, k_l, v_l = mla_qkv(*lat_parts, p, rope)
    q_c, k_c, v_c = mla_qkv(*ctx_parts, p, None)
    k_all = jnp.concatenate([k_c, k_l], axis=1)
    v_all = jnp.concatenate([v_c, v_l], axis=1)
    o_lat = attend_latent(q_l[:, :, :, None], k_all, v_all, scale).reshape(b, l, MLA_HEADS * MLA_V)
    o_ctx = None
    if need_ctx:
        o_ctx = attend_dense(q_c[:, :, :, None], k_c, v_c, scale).reshape(b, q_c.shape[1], MLA_HEADS * MLA_V)
    return o_lat, o_ctx


def even_mixer(h_lat, h_ctx, p, rope, need_ctx):
    cuts = [S5_WIDTH, S5_WIDTH + MLA_Q_RANK, S5_WIDTH + MLA_Q_RANK + MLA_KV_RANK]
    u_l, cq_l, ckv_l, kr_l = jnp.split(h_lat @ p['w_in'], cuts, axis=-1)
    u_c, cq_c, ckv_c, kr_c = jnp.split(h_ctx @ p['w_in'], cuts, axis=-1)
    y_l, y_c = s5_mixer(u_l, u_c, p, need_ctx)
    o_l, o_c = mla_mixer((cq_l, ckv_l, kr_l), (cq_c, ckv_c, kr_c), p, rope, need_ctx)
    out_lat = jnp.concatenate([y_l, o_l], axis=-1) @ p['w_out']
    out_ctx = jnp.concatenate([y_c, o_c], axis=-1) @ p['w_out'] if need_ctx else None
    return out_lat, out_ctx


def gqa_qkv(h, p, rope):
    b, l = h.shape[:2]
    q, k, v = jnp.split(h @ p['w_qkv'], [ODD_Q, ODD_Q + ODD_KV], axis=-1)
    q = rms_norm(q.reshape(b, l, GQA_HEADS, GQA_DIM), p['qn'])
    k = rms_norm(k.reshape(b, l, GQA_KV_HEADS, GQA_DIM), p['kn'])
    v = v.reshape(b, l, GQA_KV_HEADS, GQA_DIM)
    if rope is not None:
        q = apply_rope(q, *rope)
        k = apply_rope(k, *rope)
    return q.reshape(b, l, GQA_KV_HEADS, GQA_GROUP, GQA_DIM), k, v


def odd_mixer(h_lat, h_ctx, p, rope, need_ctx):
    b, l = h_lat.shape[:2]
    scale = GQA_DIM ** -0.5
    q_l, k_l, v_l = gqa_qkv(h_lat, p, rope)
    q_c, k_c, v_c = gqa_qkv(h_ctx, p, None)
    k_all = jnp.concatenate([k_c, k_l], axis=1)
    v_all = jnp.concatenate([v_c, v_l], axis=1)
    out_lat = attend_latent(q_l, k_all, v_all, scale).reshape(b, l, ODD_MIX) @ p['w_out']
    out_ctx = None
    if need_ctx:
        out_ctx = attend_dense(q_c, k_c, v_c, scale).reshape(b, h_ctx.shape[1], ODD_MIX) @ p['w_out']
    return out_lat, out_ctx


def hier_moe(h, p):
    t, d = h.shape
    n_rows = t * MOE_TOP_K
    n_blocks = (n_rows + MOE_EXPERTS * (MOE_BLOCK - 1) + MOE_BLOCK - 1) // MOE_BLOCK
    lg1 = jnp.einsum('td,dg->tg', h, p['w_r1'], preferred_element_type=jnp.float32) + p['b_r1']
    p1 = jax.nn.softmax(lg1, axis=-1)
    grp = jnp.argmax(lg1, axis=-1)
    tok_idx = jnp.arange(t)
    p_grp = p1[tok_idx, grp]
    lg2 = (jnp.einsum('td,de->te', h, p['w_r2'], preferred_element_type=jnp.float32) + p['b_r2'])
    lg2 = lg2.reshape(t, MOE_GROUPS, MOE_PER_GROUP)[tok_idx, grp]
    top_v, top_i = lax.top_k(lg2, MOE_TOP_K)
    gates = p_grp[:, None] * jax.nn.softmax(top_v, axis=-1)
    expert = (grp[:, None] * MOE_PER_GROUP + top_i).reshape(-1)
    token = jnp.repeat(tok_idx, MOE_TOP_K)
    onehot = jax.nn.one_hot(expert, MOE_EXPERTS, dtype=jnp.int32)
    rank = jnp.cumsum(onehot, axis=0)[jnp.arange(n_rows), expert] - 1
    counts = onehot.sum(axis=0)
    padded = (counts + MOE_BLOCK - 1) // MOE_BLOCK * MOE_BLOCK
    pad_end = jnp.cumsum(padded)
    dest = (pad_end - padded)[expert] + rank
    xs = jnp.zeros((n_blocks * MOE_BLOCK, d), h.dtype).at[dest].set(h[token])
    blk_expert = jnp.minimum(jnp.searchsorted(pad_end, jnp.arange(n_blocks) * MOE_BLOCK, side='right'),
                             MOE_EXPERTS - 1)

    def run_block(args):
        xb, e = args
        return (jax.nn.silu(xb @ p['w_gate'][e]) * (xb @ p['w_up'][e])) @ p['w_down'][e]

    ys = lax.map(run_block, (xs.reshape(n_blocks, MOE_BLOCK, d), blk_expert)).reshape(-1, d)
    return (ys[dest] * gates.reshape(-1, 1).astype(h.dtype)).reshape(t, MOE_TOP_K, d).sum(axis=1)


def setup_inputs(seed: int = 0) -> dict:
    key = jax.random.key(seed)
    ks = iter(jax.random.split(key, 40))

    def nrm(shape, std):
        return std * jax.random.normal(next(ks), shape, jnp.float32)

    def gain(shape):
        return 1.0 + nrm(shape, 0.05)

    n_idx = jnp.arange(S5_STATE, dtype=jnp.float32)
    return {
        'x': nrm((BATCH, SEQ, D_MODEL), 1.0),
        'c': nrm((BATCH, D_MODEL), 1.0),
        'ctx': nrm((BATCH, CTX_LEN, D_MODEL), 1.0),
        'c_ctx': nrm((D_MODEL,), 1.0),
        'w_ada': nrm((DEPTH, D_MODEL, N_MOD * D_MODEL), 0.5 * D_MODEL ** -0.5),
        'b_ada': nrm((DEPTH, N_MOD * D_MODEL), 0.02),
        'norm1_g': gain((DEPTH, D_MODEL)),
        'norm2_g': gain((DEPTH, D_MODEL)),
        'w_in_e': nrm((N_EVEN, D_MODEL, EVEN_IN), D_MODEL ** -0.5),
        'w_out_e': nrm((N_EVEN, EVEN_MIX, D_MODEL), EVEN_MIX ** -0.5),
        's5_a_re': -0.5 + nrm((N_EVEN, 2, S5_GROUPS, S5_STATE), 0.01),
        's5_a_im': jnp.pi * n_idx + nrm((N_EVEN, 2, S5_GROUPS, S5_STATE), 0.01),
        's5_log_dt': jax.random.uniform(next(ks), (N_EVEN, 2, S5_GROUPS), jnp.float32,
                                        minval=math.log(S5_DT_MIN), maxval=math.log(S5_DT_MAX)),
        's5_b_re': nrm((N_EVEN, 2, S5_GROUPS, S5_STATE, S5_GROUP), (2 * S5_GROUP) ** -0.5),
        's5_b_im': nrm((N_EVEN, 2, S5_GROUPS, S5_STATE, S5_GROUP), (2 * S5_GROUP) ** -0.5),
        's5_c_re': nrm((N_EVEN, 2, S5_GROUPS, S5_GROUP, S5_STATE), S5_STATE ** -0.5),
        's5_c_im': nrm((N_EVEN, 2, S5_GROUPS, S5_GROUP, S5_STATE), S5_STATE ** -0.5),
        's5_d': nrm((N_EVEN, S5_WIDTH), 0.5),
        's5_w_glu': nrm((N_EVEN, S5_WIDTH, S5_WIDTH), S5_WIDTH ** -0.5),
        'mla_gq': gain((N_EVEN, MLA_Q_RANK)),
        'mla_w_uq': nrm((N_EVEN, MLA_Q_RANK, MLA_HEADS * MLA_QK), MLA_Q_RANK ** -0.5),
        'mla_gkv': gain((N_EVEN, MLA_KV_RANK)),
        'mla_w_ukv': nrm((N_EVEN, MLA_KV_RANK, MLA_HEADS * (MLA_NOPE + MLA_V)), MLA_KV_RANK ** -0.5),
        'mla_qn': gain((N_EVEN, MLA_QK)),
        'mla_kn': gain((N_EVEN, MLA_QK)),
        'w_qkv_o': nrm((N_ODD, D_MODEL, ODD_IN), D_MODEL ** -0.5),
        'w_out_o': nrm((N_ODD, ODD_MIX, D_MODEL), ODD_MIX ** -0.5),
        'gqa_qn': gain((N_ODD, GQA_DIM)),
        'gqa_kn': gain((N_ODD, GQA_DIM)),
        'moe_w_r1': nrm((DEPTH, D_MODEL, MOE_GROUPS), D_MODEL ** -0.5),
        'moe_b_r1': nrm((DEPTH, MOE_GROUPS), 0.01),
        'moe_w_r2': nrm((DEPTH, D_MODEL, MOE_EXPERTS), D_MODEL ** -0.5),
        'moe_b_r2': nrm((DEPTH, MOE_EXPERTS), 0.01),
        'moe_w_gate': nrm((DEPTH, MOE_EXPERTS, D_MODEL, MOE_FF), D_MODEL ** -0.5),
        'moe_w_up': nrm((DEPTH, MOE_EXPERTS, D_MODEL, MOE_FF), D_MODEL ** -0.5),
        'moe_w_down': nrm((DEPTH, MOE_EXPERTS, MOE_FF, D_MODEL), MOE_FF ** -0.5),
    }


def reference(x, c, ctx, c_ctx, w_ada, b_ada, norm1_g, norm2_g, w_in_e, w_out_e,
              s5_a_re, s5_a_im, s5_log_dt, s5_b_re, s5_b_im, s5_c_re, s5_c_im, s5_d, s5_w_glu,
              mla_gq, mla_w_uq, mla_gkv, mla_w_ukv, mla_qn, mla_kn,
              w_qkv_o, w_out_o, gqa_qn, gqa_kn,
              moe_w_r1, moe_b_r1, moe_w_r2, moe_b_r2, moe_w_gate, moe_w_up, moe_w_down):
    b, n_lat, _ = x.shape
    rows = n_lat // GRID_W
    row = jnp.repeat(jnp.arange(rows, dtype=jnp.float32), GRID_W)
    col = jnp.tile(jnp.arange(GRID_W, dtype=jnp.float32), rows)
    rope_mla = rope_tables(row, col, MLA_ROPE)
    rope_gqa = rope_tables(row, col, GQA_DIM)
    sc = jax.nn.silu(c)
    sc_ctx = jax.nn.silu(c_ctx)
    x_lat, x_ctx = x, ctx
    for l in range(DEPTH):
        last = l == DEPTH - 1
        m_lat = [m[:, None, :] for m in ada_chunks(sc, w_ada[l], b_ada[l])]
        m_ctx = ada_chunks(sc_ctx, w_ada[l], b_ada[l])
        h_lat = modulate(rms_norm(x_lat, norm1_g[l]), m_lat[0], m_lat[1])
        h_ctx = modulate(rms_norm(x_ctx, norm1_g[l]), m_ctx[0], m_ctx[1])
        if l % 2 == 0:
            i = l // 2
            p = dict(w_in=w_in_e[i], w_out=w_out_e[i], a_re=s5_a_re[i], a_im=s5_a_im[i],
                     log_dt=s5_log_dt[i], b_re=s5_b_re[i], b_im=s5_b_im[i], c_re=s5_c_re[i],
                     c_im=s5_c_im[i], d=s5_d[i], w_glu=s5_w_glu[i], gq=mla_gq[i], w_uq=mla_w_uq[i],
                     gkv=mla_gkv[i], w_ukv=mla_w_ukv[i], qn=mla_qn[i], kn=mla_kn[i])
            mix_lat, mix_ctx = even_mixer(h_lat, h_ctx, p, rope_mla, not last)
        else:
            i = l // 2
            p = dict(w_qkv=w_qkv_o[i], w_out=w_out_o[i], qn=gqa_qn[i], kn=gqa_kn[i])
            mix_lat, mix_ctx = odd_mixer(h_lat, h_ctx, p, rope_gqa, not last)
        x_lat = x_lat + m_lat[2] * mix_lat
        p_moe = dict(w_r1=moe_w_r1[l], b_r1=moe_b_r1[l], w_r2=moe_w_r2[l], b_r2=moe_b_r2[l],
                     w_gate=moe_w_gate[l], w_up=moe_w_up[l], w_down=moe_w_down[l])
        g_lat = modulate(rms_norm(x_lat, norm2_g[l]), m_lat[3], m_lat[4])
        if last:
            f_lat = hier_moe(g_lat.reshape(-1, D_MODEL), p_moe).reshape(x_lat.shape)
        else:
            x_ctx = x_ctx + m_ctx[2] * mix_ctx
            g_ctx = modulate(rms_norm(x_ctx, norm2_g[l]), m_ctx[3], m_ctx[4])
            f_all = hier_moe(jnp.concatenate([g_lat.reshape(-1, D_MODEL), g_ctx.reshape(-1, D_MODEL)], axis=0), p_moe)
            f_lat = f_all[:b * n_lat].reshape(x_lat.shape)
            x_ctx = x_ctx + m_ctx[5] * f_all[b * n_lat:].reshape(x_ctx.shape)
        x_lat = x_lat + m_lat[5] * f_lat
    return x_lat
```

```python
import math
import numpy as np
import ml_dtypes
import concourse.bass as bass
import concourse.mybir as mybir
from concourse.bass_utils import run_bass_kernel_spmd

F32 = mybir.dt.float32
BF16 = mybir.dt.bfloat16
I32 = mybir.dt.int32
U32 = mybir.dt.uint32
AF = mybir.ActivationFunctionType
ALU = mybir.AluOpType
AX = mybir.AxisListType

D = 1024
NCTX = 256
NLAT = 4096
T = NCTX + NLAT
NT = T // 128
CHUNKS = [(0, 256)] + [(256 + 512 * i, 512) for i in range(8)]
EPS = 1e-6
NEXP = 32
CAP = 512
NSLOT = NEXP * CAP
EPOCH = 20000


class Buf:
    __slots__ = ("name", "lw", "rd", "dsem", "dcnt", "dbase", "dq")

    def __init__(self, name):
        self.name = name
        self.lw = None
        self.rd = {}
        self.dsem = None
        self.dcnt = 0
        self.dbase = 0
        self.dq = None


class K:
    def __init__(self, nc):
        self.nc = nc
        self.E = {"pe": nc.tensor, "act": nc.scalar, "dve": nc.vector, "pool": nc.gpsimd, "sp": nc.sync}
        self.sems = {e: [] for e in self.E}
        self.cnt = {e: 0 for e in self.E}
        self.seen = {e: {} for e in self.E}
        self.bufs = []
        self.ninstr = 0
        self.sempool = {"sp": [], "pool": []}
        self.nsem = 0
        self.keep = set()
        self.pool_q = []

    def buf(self, name):
        b = Buf(name)
        self.bufs.append(b)
        return b

    def _csem(self, e, n):
        idx = (n - 1) // EPOCH
        while len(self.sems[e]) <= idx:
            self.sems[e].append(self.nc.alloc_semaphore(f"c_{e}_{len(self.sems[e])}"))
        return self.sems[e][idx], (n - 1) % EPOCH + 1

    def _wait(self, e, tok):
        if tok is None:
            return
        if tok[0] == "c":
            _, e2, n = tok
            if e2 == e and e in ("pe", "sp"):
                return
            if self.seen[e].get(e2, 0) >= n:
                return
            s, v = self._csem(e2, n)
            self.E[e].wait_ge(s, v)
            self.ninstr += 1
            self.seen[e][e2] = n
        else:
            B = tok[1]
            if B.dsem is None:
                return
            tgt = B.dbase + B.dcnt * 16
            assert tgt < 65000, B.name
            key = ("d", id(B.dsem))
            if self.seen[e].get(key, 0) >= tgt:
                return
            self.E[e].wait_ge(B.dsem, tgt)
            self.ninstr += 1
            self.seen[e][key] = tgt

    def _deps(self, e, reads, writes):
        for b in reads:
            self._wait(e, b.lw)
        for b in writes:
            self._wait(e, b.lw)
            for tok in list(b.rd.values()):
                self._wait(e, tok)

    def op(self, e, meth, reads, writes, *args, **kw):
        self._deps(e, reads, writes)
        ins = getattr(self.E[e], meth)(*args, **kw)
        self.cnt[e] += 1
        n = self.cnt[e]
        s, _ = self._csem(e, n)
        ins.then_inc(s, 1)
        self.ninstr += 1
        tok = ("c", e, n)
        for b in reads:
            b.rd[e] = tok
        for b in writes:
            b.lw = tok
            b.rd = {}
        return ins

    def dma(self, q, out, in_, reads, writes, indirect=None, **kw):
        self._deps(q, reads, writes)
        B = writes[0]
        if B.dsem is not None:
            assert B.dq == q, (B.name, B.dq, q)
        if B.dsem is None:
            B.dq = q
            if self.sempool[q]:
                B.dsem, B.dbase = self.sempool[q].pop()
            else:
                self.nsem += 1
                B.dsem, B.dbase = self.nc.alloc_semaphore(f"d{self.nsem}"), 0
            B.dcnt = 0
        B.dcnt += 1
        if indirect is None:
            ins = self.E[q].dma_start(out=out, in_=in_, **kw)
        else:
            ins = indirect(self.E[q])
        ins.then_inc(B.dsem, 16)
        self.ninstr += 1
        if q == "pool":
            self.pool_q.append((B.dsem, B.dbase + B.dcnt * 16))
            if len(self.pool_q) > 24:
                s_, v_ = self.pool_q.pop(0)
                key_ = ("d", id(s_))
                if self.seen[q].get(key_, 0) < v_:
                    self.E[q].wait_ge(s_, v_)
                    self.ninstr += 1
                    self.seen[q][key_] = v_
        tok = ("d", B)
        for b in reads:
            b.rd[("d", id(B))] = tok
        for b in writes:
            b.lw = tok
            b.rd = {}

    def barrier(self):
        for e in self.E:
            for e2 in self.E:
                if e2 != e and self.cnt[e2] > 0:
                    self._wait(e, ("c", e2, self.cnt[e2]))
            for b in self.bufs:
                if b.dsem is not None and b.dcnt > 0:
                    self._wait(e, ("d", b))
        self.pool_q = []
        for b in self.bufs:
            b.lw = None
            b.rd = {}
            if b.dsem is not None:
                v = b.dbase + b.dcnt * 16
                if v < 40000:
                    self.sempool[b.dq].append((b.dsem, v))
                b.dsem = None
                b.dcnt = 0


def _rope_tab(d_rot, n_freq):
    rows = NLAT // 64
    row = np.repeat(np.arange(rows, dtype=np.float32), 64)
    col = np.tile(np.arange(64, dtype=np.float32), rows)
    inv = (10000.0 ** (-np.arange(n_freq, dtype=np.float32) / n_freq)).astype(np.float32)
    ang = np.stack([row[:, None] * inv, col[:, None] * inv], axis=1).astype(np.float32)
    cos = np.cos(ang)
    sin = np.sin(ang)
    c = np.zeros((d_rot, T), np.float32)
    s = np.zeros((d_rot, T), np.float32)
    c[:, :NCTX] = 1.0
    for ax in range(2):
        for half in range(2):
            for i in range(n_freq):
                d = ax * 2 * n_freq + half * n_freq + i
                c[d, NCTX:] = cos[:, ax, i]
                s[d, NCTX:] = sin[:, ax, i]
    return c, s


def _rot_mat(d_total, off, n_freq):
    Rm = np.zeros((128, 128), np.float32)
    for ax in range(2):
        for i in range(n_freq):
            first = off + ax * 2 * n_freq + i
            second = first + n_freq
            Rm[second, first] = -1.0
            Rm[first, second] = 1.0
    return Rm


def make_consts():
    c = {}
    m = np.zeros((6, 128, 128), np.float32)
    m[0] = np.eye(128)
    m[1] = 1.0
    m[2] = _rot_mat(128, 0, 32)
    m[3] = _rot_mat(96, 64, 8)
    m[4] = np.triu(np.ones((128, 128), np.float32), 1)
    c["cmat"] = np.ascontiguousarray(m.transpose(1, 0, 2))
    gc, gs = _rope_tab(128, 32)
    c["rope_g"] = np.stack([gc, gs], axis=1)
    mc = np.zeros((128, T), np.float32)
    ms = np.zeros((128, T), np.float32)
    mc[:64] = 1.0
    rc, rs = _rope_tab(32, 8)
    mc[64:96] = rc
    ms[64:96] = rs
    c["rope_m"] = np.stack([mc, ms], axis=1)
    ec = np.zeros((128, 32), np.float32)
    ec[:] = np.arange(32, dtype=np.float32) * CAP
    c["ecap"] = ec
    m2 = np.zeros((128, 3), np.float32)
    for p in range(128):
        m2[p, (p // 16) % 2] = 1.0
    m2[96:, 2] = 1.0
    c["mask2"] = m2
    return c


PASSES = [[(0, 256), (256, 512), (768, 512), (1280, 512), (1792, 384)],
          [(2176, 512), (2688, 512), (3200, 512), (3712, 512), (4224, 128)]]
PASS_T = 2176
PASS_NT = 17


class Prog:
    def __init__(self, layers, stop=None):
        self.layers = layers
        self.stop = stop
        self.nc = bass.Bass("TRN2", target_bir_lowering=False)
        self.k = K(self.nc)
        self.din = {}
        self._ncnt = 0

    def inp(self, name, shape, dtype=F32):
        t = self.nc.dram_tensor(name, list(shape), dtype, kind="ExternalInput")
        self.din[name] = tuple(shape)
        return t.ap()

    def scratch(self, name, shape, dtype):
        return self.nc.dram_tensor(name, list(shape), dtype, kind="Internal").ap()

    def nm(self, s):
        self._ncnt += 1
        return f"{s}_{self._ncnt}"

    def sbp(self, name, shape, dtype):
        t = self.nc.alloc_sbuf_tensor(self.nm(name), list(shape), dtype)
        return t, self.k.buf(name)

    def tl(self, stack, name, shape, dtype):
        t = stack.enter_context(self.nc.sbuf_tensor(self.nm(name), list(shape), dtype))
        return t, self.k.buf(name)

    def bank(self, i):
        t = self.pst[i // 2]
        return t[:, (i % 2) * 512:(i % 2 + 1) * 512], self.pb[i]

    def build(self):
        nc, k = self.nc, self.k
        L = self.layers
        x_in = self.inp("x", [NLAT, D])
        ctx_in = self.inp("ctx", [NCTX, D])
        self._cvec = self.inp("cvec", [2, D])
        cmat_d = self.inp("cmat", [128, 6, 128])
        self.rope_g_d = self.inp("rope_g", [128, 2, T])
        self.rope_m_d = self.inp("rope_m", [128, 2, T])
        mask2_d = self.inp("mask2", [128, 3])
        W = {}
        for l in L:
            W[l] = {}
            for nm_, shp in [("w_ada", [D, 6 * D]), ("b_ada", [1, 6 * D]), ("n1", [1, D]), ("n2", [1, D]),
                             ("w_r", [D, 36]), ("b_r", [1, 36]), ("w_out", [D, D])]:
                W[l][nm_] = self.inp(f"{nm_}{l}", shp)
            self.Wt = getattr(self, "Wt", {})
            self.Wt[l] = {}
            for nm_, shp in [("w_gate", [NEXP * 128, 4096]), ("w_up", [NEXP * 128, 4096]), ("w_down", [NEXP * 128, 4096])]:
                t_ = self.nc.dram_tensor(f"{nm_}{l}", shp, F32, kind="ExternalInput")
                self.din[f"{nm_}{l}"] = tuple(shp)
                self.Wt[l][nm_] = t_
            if l % 2 == 1:
                for nm_, shp in [("w_qkv", [D, 2048]), ("qn", [128, 1]), ("kn", [128, 1])]:
                    W[l][nm_] = self.inp(f"{nm_}{l}", shp)
            else:
                for nm_, shp in [("w_in", [D, 928]), ("a_re", [2, 32, 64]), ("a_im", [2, 32, 64]),
                                 ("log_dt", [2, 32, 1]), ("b_re", [2, 32, 64, 16]), ("b_im", [2, 32, 64, 16]),
                                 ("c_re", [2, 32, 16, 64]), ("c_im", [2, 32, 16, 64]),
                                 ("s5_d", [512, 1]), ("w_glu", [512, 512]), ("gq", [256, 1]),
                                 ("w_uq", [256, 768]), ("gkv", [128, 1]), ("w_ukv", [128, 1024]),
                                 ("mqn", [96, 1]), ("mkn", [96, 1])]:
                    W[l][nm_] = self.inp(f"{nm_}{l}", shp)
        self.W = W
        self.y_out = nc.dram_tensor("y", [NLAT, D], F32, kind="ExternalOutput").ap()
        self.xT = self.scratch("xT", [8, 128, T], F32)
        self.b_xT = k.buf("xT")
        self.qT = self.scratch("qT", [8, 128, T], BF16)
        self.b_qT = k.buf("qT")
        self.kT = self.scratch("kT", [8, 128, T], BF16)
        self.b_kT = k.buf("kT")
        self.vS = self.scratch("vS", [T, 512], BF16)
        self.b_vS = k.buf("vS")
        self.mixT = self.scratch("mixT", [8, 128, T], BF16)
        self.b_mixT = k.buf("mixT")
        self.b_y = k.buf("y")
        self.cm_f, self.b_cmf = self.sbp("cm_f", [128, 6, 128], F32)
        self.cm_b, self.b_cmb = self.sbp("cm_b", [128, 6, 128], BF16)
        k.dma("sp", self.cm_f[:], cmat_d, [], [self.b_cmf])
        k.dma("pool", self.cm_b[:], cmat_d, [], [self.b_cmb])
        self.mask2, self.b_mask2 = self.sbp("mask2", [128, 3], F32)
        k.dma("sp", self.mask2[:], mask2_d, [], [self.b_mask2])
        nl = len(L)
        self.mT, self.b_mT = self.sbp("mT", [128, nl, 48, 2], F32)
        self.gsc, self.b_gsc = self.sbp("gsc", [128, nl, 2, 8, 2], F32)
        self.pst = [nc.alloc_psum_tensor(self.nm("ps"), [128, 1024], F32) for _ in range(4)]
        self.pb = [k.buf(f"psb{i}") for i in range(8)]

        self.phase_adaln()
        self.phase_load_x(x_in, ctx_in)
        for li, l in enumerate(L):
            if l % 2 == 1:
                self.phase_odd_proj(li, l)
                if self.stop == "O1":
                    continue
                self.phase_attention(8, 4, lambda h: h // 2, 128, 128, 128 ** -0.5,
                                     lambda h: (h, 0))
            else:
                self.phase_even(li, l)
            if self.stop and self.stop != "OUT":
                continue
            self.phase_outproj(li, l)
            if self.stop == "OUT":
                continue
            self.phase_moe(li, l)
        self.phase_store_y()
        return nc

    def phase_adaln(self):
        nc, k = self.nc, self.k
        from contextlib import ExitStack
        with ExitStack() as st:
            scT, b_scT = self.tl(st, "scT", [128, 2, 8], F32)
            was = [self.tl(st, f"wa{i}", [128, 3072], F32) for i in range(2)]
            mrow, b_mrow = self.tl(st, "mrow", [2, 6144], F32)
            brow, b_brow = self.tl(st, "brow", [2, 6144], F32)
            ngT, b_ngT = self.tl(st, "ngT", [128, 2, 8], F32)
            idf = self.cm_f[:, 0, :]
            for j in range(2):
                k.dma("sp", scT[:, j, :], self._cvec[j:j + 1, :].rearrange("o (k p) -> p (o k)", p=128), [], [b_scT],
                      allow_slow_non_contiguous=True)
            k.op("act", "activation", [b_scT], [b_scT], out=scT[:], in_=scT[:], func=AF.Silu)
            cnt = 0
            for li, l in enumerate(self.layers):
                w = self.W[l]
                k.dma("sp", brow[:], w["b_ada"][0:1, :].partition_broadcast(2), [], [b_brow])
                for half in range(2):
                    for kt in range(8):
                        wa, b_wa = was[cnt % 2]
                        cnt += 1
                        k.dma("sp", wa[:], w["w_ada"][kt * 128:(kt + 1) * 128, half * 3072:(half + 1) * 3072],
                              [], [b_wa])
                        for c6 in range(6):
                            pt, b_pt = self.bank(c6)
                            k.op("pe", "matmul", [b_scT, b_wa], [b_pt], pt[0:2, :], lhsT=scT[:, :, kt],
                                 rhs=wa[:, c6 * 512:(c6 + 1) * 512], start=(kt == 0), stop=(kt == 7))
                    for c6 in range(6):
                        pt, b_pt = self.bank(c6)
                        col = half * 3072 + c6 * 512
                        k.op("dve", "tensor_tensor", [b_pt, b_brow], [b_mrow], out=mrow[:, col:col + 512],
                             in0=pt[0:2, :], in1=brow[:, col:col + 512], op=ALU.add)
                pt, b_pt = self.bank(6)
                for ft in range(48):
                    k.op("pe", "transpose", [b_mrow, self.b_cmf], [b_pt], pt[:, ft * 2:ft * 2 + 2],
                         mrow[:, ft * 128:(ft + 1) * 128], idf[0:2, 0:2])
                k.op("dve", "tensor_copy", [b_pt], [self.b_mT], out=self.mT[:, li, :, :],
                     in_=pt[:, 0:96].rearrange("p (f j) -> p f j", j=2))
                k.dma("sp", ngT[:, 0, :], w["n1"].rearrange("o (k p) -> p (o k)", p=128), [], [b_ngT],
                      allow_slow_non_contiguous=True)
                k.dma("sp", ngT[:, 1, :], w["n2"].rearrange("o (k p) -> p (o k)", p=128), [], [b_ngT],
                      allow_slow_non_contiguous=True)
                for which, scc in ((0, 1), (1, 4)):
                    for j in range(2):
                        k.op("dve", "scalar_tensor_tensor", [self.b_mT, b_ngT], [self.b_gsc],
                             out=self.gsc[:, li, which, :, j], in0=self.mT[:, li, scc * 8:(scc + 1) * 8, j],
                             scalar=1.0, in1=ngT[:, which, :], op0=ALU.add, op1=ALU.mult)
            k.barrier()

    def phase_load_x(self, x_in, ctx_in):
        nc, k = self.nc, self.k
        from contextlib import ExitStack
        idf = self.cm_f[:, 0, :]
        with ExitStack() as st:
            xas = [self.tl(st, f"xa{i}", [128, D], F32) for i in range(2)]
            xos = [self.tl(st, f"xo{i}", [128, 8, 128], F32) for i in range(2)]
            for ti in range(NT):
                src = ctx_in[ti * 128:(ti + 1) * 128, :] if ti < 2 else x_in[(ti - 2) * 128:(ti - 1) * 128, :]
                xa, b_xa = xas[ti % 2]
                xo, b_xo = xos[ti % 2]
                k.dma("sp", xa[:], src, [], [b_xa])
                for half in range(2):
                    pt, b_pt = self.bank((ti % 2) * 2 + half)
                    for kk in range(4):
                        kt = half * 4 + kk
                        k.op("pe", "transpose", [b_xa, self.b_cmf], [b_pt], pt[:, kk * 128:(kk + 1) * 128],
                             xa[:, kt * 128:(kt + 1) * 128], idf)
                    src_v = pt.rearrange("p (a b) -> p a b", b=128)
                    if half == 0:
                        k.op("act", "activation", [b_pt], [b_xo], out=xo[:, 0:4, :], in_=src_v, func=AF.Copy)
                    else:
                        k.op("dve", "tensor_copy", [b_pt], [b_xo], out=xo[:, 4:8, :], in_=src_v)
                k.dma("sp", self.xT[:, :, ti * 128:(ti + 1) * 128].rearrange("k p t -> p k t"), xo[:],
                      [b_xo], [self.b_xT])
            k.barrier()

    def phase_store_y(self):
        nc, k = self.nc, self.k
        from contextlib import ExitStack
        idf = self.cm_f[:, 0, :]
        with ExitStack() as st:
            xas = [self.tl(st, f"ya{i}", [128, 8, 128], F32) for i in range(2)]
            xos = [self.tl(st, f"yo{i}", [128, D], F32) for i in range(2)]
            for ti in range(2, NT):
                xa, b_xa = xas[ti % 2]
                xo, b_xo = xos[ti % 2]
                k.dma("sp", xa[:], self.xT[:, :, ti * 128:(ti + 1) * 128].rearrange("k p t -> p k t"),
                      [self.b_xT], [b_xa])
                for half in range(2):
                    pt, b_pt = self.bank((ti % 2) * 2 + half)
                    for kk in range(4):
                        kt = half * 4 + kk
                        k.op("pe", "transpose", [b_xa, self.b_cmf], [b_pt], pt[:, kk * 128:(kk + 1) * 128],
                             xa[:, kt, :], idf)
                    if half == 0:
                        k.op("act", "activation", [b_pt], [b_xo], out=xo[:, 0:512], in_=pt, func=AF.Copy)
                    else:
                        k.op("dve", "tensor_copy", [b_pt], [b_xo], out=xo[:, 512:1024], in_=pt)
                k.dma("sp", self.y_out[(ti - 2) * 128:(ti - 1) * 128, :], xo[:], [b_xo], [self.b_y])
            if self.stop:
                self.dbg_names = []
                for nm_, shp, dt_ in (("uT", [4, 128, T], BF16), ("qT", [8, 128, T], BF16), ("kT", [8, 128, T], BF16),
                                      ("vS", [T, 512], BF16), ("yT", [4, 128, T], F32), ("mixT", [8, 128, T], BF16)):
                    if hasattr(self, nm_):
                        o = nc.dram_tensor("dbg_" + nm_, shp, dt_, kind="ExternalOutput").ap()
                        b_o = k.buf("dbg_" + nm_)
                        k.dma("sp", o, getattr(self, nm_), [getattr(self, "b_" + nm_)], [b_o])
                        self.dbg_names.append("dbg_" + nm_)
            k.barrier()

    def norm_mod(self, xc, b_xc, n, li, which, j, out_fn, b_out, sq, b_sq, rs, b_rs, tmp, b_tmp, psb):
        k = self.k
        ones_b = self.cm_b[:, 1, :]
        pt, b_pt = self.bank(psb)
        k.op("act", "activation", [b_xc], [b_sq], out=sq[:, :, :n], in_=xc[:, :, :n], func=AF.Square)
        for kt in range(8):
            k.op("pe", "matmul", [b_sq, self.b_cmb], [b_pt], pt[:, :n], lhsT=ones_b, rhs=sq[:, kt, :n],
                 start=(kt == 0), stop=(kt == 7))
        k.op("act", "activation", [b_pt], [b_rs], out=rs[:, :n], in_=pt[:, :n], func=AF.Sqrt,
             scale=1.0 / D, bias=EPS)
        k.op("dve", "reciprocal", [b_rs], [b_rs], out=rs[:, :n], in_=rs[:, :n])
        k.op("dve", "tensor_tensor", [b_xc, b_rs], [b_tmp], out=tmp[:, :, :n], in0=xc[:, :, :n],
             in1=rs[:, :n].unsqueeze(1).broadcast_to([128, 8, n]), op=ALU.mult)
        shc = 0 if which == 0 else 3
        for kt in range(8):
            k.op("dve" if kt % 2 == 0 else "pool", "tensor_scalar", [b_tmp, self.b_gsc, self.b_mT], [b_out],
                 out=out_fn(kt), in0=tmp[:, kt, :n], scalar1=self.gsc[:, li, which, kt, j:j + 1],
                 scalar2=self.mT[:, li, shc * 8 + kt, j:j + 1], op0=ALU.mult, op1=ALU.add)

    def qk_norm_rope(self, ps, b_ps, Dh, n, gain, b_gain, cos, sin, b_rope, Rm, out, b_out, wk, pss, prot):
        k = self.k
        sqh, b_sqh, rsh, b_rsh, xg, b_xg, t1, b_t1, t2, b_t2 = wk[self._wkc % 2]
        if self._wkc % 2 == 1:
            pss = 7
        self._wkc += 1
        ones_b = self.cm_b[0:Dh, 1, 0:Dh]
        p2, b_p2 = self.bank(pss)
        p3, b_p3 = self.bank(prot)
        k.op("act", "activation", [b_ps], [b_sqh], out=sqh[0:Dh, :n], in_=ps[0:Dh, :n], func=AF.Square)
        k.op("pe", "matmul", [b_sqh, self.b_cmb], [b_p2], p2[0:Dh, :n], lhsT=ones_b, rhs=sqh[0:Dh, :n],
             start=True, stop=True)
        k.op("act", "activation", [b_p2], [b_rsh], out=rsh[0:Dh, :n], in_=p2[0:Dh, :n], func=AF.Sqrt,
             scale=1.0 / Dh, bias=EPS)
        k.op("dve", "reciprocal", [b_rsh], [b_rsh], out=rsh[0:Dh, :n], in_=rsh[0:Dh, :n])
        k.op("dve", "scalar_tensor_tensor", [b_ps, b_gain, b_rsh], [b_xg], out=xg[0:Dh, :n], in0=ps[0:Dh, :n],
             scalar=gain, in1=rsh[0:Dh, :n], op0=ALU.mult, op1=ALU.mult)
        k.op("pe", "matmul", [b_xg, self.b_cmb], [b_p3], p3[0:Dh, :n], lhsT=Rm, rhs=xg[0:Dh, :n],
             start=True, stop=True)
        k.op("pool", "tensor_tensor", [b_xg, b_rope], [b_t1], out=t1[0:Dh, :n], in0=xg[0:Dh, :n], in1=cos,
             op=ALU.mult)
        k.op("dve", "tensor_tensor", [b_p3, b_rope], [b_t2], out=t2[0:Dh, :n], in0=p3[0:Dh, :n], in1=sin,
             op=ALU.mult)
        k.op("pool", "tensor_tensor", [b_t1, b_t2], [b_out], out=out, in0=t1[0:Dh, :n], in1=t2[0:Dh, :n],
             op=ALU.add)

    def qk_work(self, st):
        sets = []
        for i in range(2):
            wk = []
            for nm_, dt_ in (("sqh", BF16), ("rsh", F32), ("xg", BF16), ("t1", F32), ("t2", F32)):
                t, b = self.tl(st, f"{nm_}{i}", [128, 512], dt_)
                wk += [t, b]
            sets.append(wk)
        self._wkc = 0
        return sets

    def phase_odd_proj(self, li, l):
        nc, k = self.nc, self.k
        from contextlib import ExitStack
        w = self.W[l]
        with ExitStack() as st:
            wq, b_wq = self.tl(st, "wq", [128, 8, 2048], BF16)
            rope, b_rope = self.tl(st, "rope", [128, 2, T], F32)
            xcs = [self.tl(st, f"xc{i}", [128, 8, 512], F32) for i in range(2)]
            hTs = [self.tl(st, f"hT{i}", [128, 8, 512], BF16) for i in range(2)]
            sq, b_sq = self.tl(st, "sq", [128, 8, 512], BF16)
            tmp, b_tmp = self.tl(st, "tmp", [128, 8, 512], F32)
            rs, b_rs = self.tl(st, "rs", [128, 512], F32)
            gn, b_gn = self.tl(st, "gn", [128, 2], F32)
            qos = [self.tl(st, f"qo{i}", [128, 512], BF16) for i in range(2)]
            vts = [self.tl(st, f"vt{i}", [128, 512], BF16) for i in range(2)]
            wk = self.qk_work(st)
            for kt in range(8):
                k.dma("pool", wq[:, kt, :], w["w_qkv"][kt * 128:(kt + 1) * 128, :], [], [b_wq])
            k.dma("sp", rope[:], self.rope_g_d, [], [b_rope])
            k.dma("sp", gn[:, 0:1], w["qn"], [], [b_gn])
            k.dma("sp", gn[:, 1:2], w["kn"], [], [b_gn])
            Rm = self.cm_b[:, 2, :]
            cnt = 0
            for ci, (t0, n) in enumerate(CHUNKS):
                j = 1 if ci == 0 else 0
                xc, b_xc = xcs[ci % 2]
                hT, b_hT = hTs[ci % 2]
                k.dma("sp", xc[:, :, :n], self.xT[:, :, t0:t0 + n].rearrange("k p t -> p k t"), [self.b_xT], [b_xc])
                self.norm_mod(xc, b_xc, n, li, 0, j, lambda kt: hT[:, kt, :n], b_hT, sq, b_sq, rs, b_rs,
                              tmp, b_tmp, 0)
                for hh in range(12):
                    pt, b_pt = self.bank(1 + hh % 2)
                    for kt in range(8):
                        k.op("pe", "matmul", [b_wq, b_hT], [b_pt], pt[:, :n],
                             lhsT=wq[:, kt, hh * 128:(hh + 1) * 128], rhs=hT[:, kt, :n],
                             start=(kt == 0), stop=(kt == 7))
                    qo, b_qo = qos[cnt % 2]
                    cnt += 1
                    gi = 0 if hh < 8 else 1
                    self.qk_norm_rope(pt, b_pt, 128, n, gn[:, gi:gi + 1], b_gn, rope[:, 0, t0:t0 + n],
                                      rope[:, 1, t0:t0 + n], b_rope, Rm, qo[:, :n], b_qo, wk, 3, 4)
                    if hh < 8:
                        k.dma("sp", self.qT[hh, :, t0:t0 + n], qo[:, :n], [b_qo], [self.b_qT])
                    else:
                        k.dma("sp", self.kT[hh - 8, :, t0:t0 + n], qo[:, :n], [b_qo], [self.b_kT])
                for s in range(n // 128):
                    pv, b_pv = self.bank(5 + s % 2)
                    vt, b_vt = vts[s % 2]
                    for kt in range(8):
                        k.op("pe", "matmul", [b_wq, b_hT], [b_pv], pv, lhsT=hT[:, kt, s * 128:(s + 1) * 128],
                             rhs=wq[:, kt, 1536:2048], start=(kt == 0), stop=(kt == 7))
                    k.op("act", "activation", [b_pv], [b_vt], out=vt[:], in_=pv, func=AF.Copy)
                    k.dma("sp", self.vS[t0 + s * 128:t0 + (s + 1) * 128, :], vt[:], [b_vt], [self.b_vS])
            k.barrier()

    def phase_attention(self, nh, nkv, kvmap, Dh, dv, scale, mixdst):
        nc, k = self.nc, self.k
        from contextlib import ExitStack
        with ExitStack() as st:
            kTs, b_kTs = self.tl(st, "kTs", [128, T], BF16)
            vs, b_vs = self.tl(st, "vs", [128, NT, dv], BF16)
            qTs, b_qTs = self.tl(st, "qTs", [128, T], BF16)
            Pts = [self.tl(st, f"Pt{i}", [128, 2, 512], BF16) for i in range(2)]
            acc2, b_acc2 = self.tl(st, "acc2", [128, 2, 512], F32)
            accs, b_accs = self.tl(st, "accs", [128, 512], F32)
            rl, b_rl = self.tl(st, "rl", [128, 512], F32)
            obs = [self.tl(st, f"ob{i}", [128, 512], BF16) for i in range(2)]
            ones_f = self.cm_f[:, 1, 0:dv]
            gcnt = 0
            ccnt = 0
            for kh in range(nkv):
                k.dma("sp", kTs[0:Dh, :], self.kT[kh, 0:Dh, :], [self.b_kT], [b_kTs])
                k.dma("sp", vs[:], self.vS[:, kh * dv:(kh + 1) * dv].rearrange("(kt p) d -> p kt d", p=128),
                      [self.b_vS], [b_vs])
                for h in range(nh):
                    if kvmap(h) != kh:
                        continue
                    k.dma("sp", qTs[0:Dh, :], self.qT[h, 0:Dh, :], [self.b_qT], [b_qTs])
                    for ci, (t0, n) in enumerate(CHUNKS):
                        nk = 2 if ci == 0 else NT
                        pO, b_pO = self.bank(4 + ccnt % 2)
                        G_ = nk // 2

                        def emit_qk(g, gc):
                            sp_t = self.pst[gc % 2]
                            bS = [self.pb[(gc % 2) * 2], self.pb[(gc % 2) * 2 + 1]]
                            for jj in range(2):
                                kt = g * 2 + jj
                                k.op("pe", "matmul", [b_kTs, b_qTs], [bS[jj]], sp_t[:, jj * 512:jj * 512 + n],
                                     lhsT=kTs[0:Dh, kt * 128:(kt + 1) * 128], rhs=qTs[0:Dh, t0:t0 + n],
                                     start=True, stop=True)

                        def emit_rest(g, gc):
                            sp_t = self.pst[gc % 2]
                            bS = [self.pb[(gc % 2) * 2], self.pb[(gc % 2) * 2 + 1]]
                            Pt, b_Pt = Pts[gc % 2]
                            k.op("act", "activation", bS, [b_Pt], out=Pt[:, :, :n],
                                 in_=sp_t.rearrange("p (a b) -> p a b", b=512)[:, :, :n], func=AF.Exp, scale=scale)
                            for jj in range(2):
                                kt = g * 2 + jj
                                k.op("pe", "matmul", [b_vs, b_Pt], [b_pO], pO[0:dv, :n], lhsT=vs[:, kt, :],
                                     rhs=Pt[:, jj, :n], start=(kt == 0), stop=(kt == nk - 1))
                            eng_ = "dve"
                            if g == 0:
                                k.op(eng_, "tensor_copy", [b_Pt], [b_acc2], out=acc2[:, :, :n], in_=Pt[:, :, :n])
                            else:
                                k.op(eng_, "tensor_tensor", [b_Pt, b_acc2], [b_acc2], out=acc2[:, :, :n],
                                     in0=acc2[:, :, :n], in1=Pt[:, :, :n], op=ALU.add)

                        emit_qk(0, gcnt)
                        for g in range(G_):
                            if g + 1 < G_:
                                emit_qk(g + 1, gcnt + 1)
                            emit_rest(g, gcnt)
                            gcnt += 1
                        k.op("pool", "tensor_tensor", [b_acc2], [b_accs], out=accs[:, :n], in0=acc2[:, 0, :n],
                             in1=acc2[:, 1, :n], op=ALU.add)
                        pL, b_pL = self.bank(6)
                        k.op("pe", "matmul", [b_accs, self.b_cmf], [b_pL], pL[0:dv, :n], lhsT=ones_f,
                             rhs=accs[:, :n], start=True, stop=True)
                        k.op("dve", "reciprocal", [b_pL], [b_rl], out=rl[0:dv, :n], in_=pL[0:dv, :n])
                        ob, b_ob = obs[ccnt % 2]
                        ccnt += 1
                        k.op("dve", "tensor_tensor", [b_pO, b_rl], [b_ob], out=ob[0:dv, :n], in0=pO[0:dv, :n],
                             in1=rl[0:dv, :n], op=ALU.mult)
                        mk, r0 = mixdst(h)
                        k.dma("sp", self.mixT[mk, r0:r0 + dv, t0:t0 + n], ob[0:dv, :n], [b_ob], [self.b_mixT])
            k.barrier()

    def phase_outproj(self, li, l):
        nc, k = self.nc, self.k
        from contextlib import ExitStack
        w = self.W[l]
        with ExitStack() as st:
            wo, b_wo = self.tl(st, "wo", [128, 8, D], BF16)
            mixs = [self.tl(st, f"mix{i}", [128, 8, 512], BF16) for i in range(2)]
            xcs = [self.tl(st, f"xc{i}", [128, 8, 512], F32) for i in range(2)]
            for kt in range(8):
                k.dma("pool", wo[:, kt, :], w["w_out"][kt * 128:(kt + 1) * 128, :], [], [b_wo])
            for ci, (t0, n) in enumerate(CHUNKS):
                j = 1 if ci == 0 else 0
                xc, b_xc = xcs[ci % 2]
                mix, b_mix = mixs[ci % 2]
                k.dma("sp", xc[:, :, :n], self.xT[:, :, t0:t0 + n].rearrange("k p t -> p k t"), [self.b_xT], [b_xc])
                k.dma("sp", mix[:, :, :n], self.mixT[:, :, t0:t0 + n].rearrange("k p t -> p k t"),
                      [self.b_mixT], [b_mix])
                for ot in range(8):
                    pt, b_pt = self.bank(ot % 4)
                    for kt in range(8):
                        k.op("pe", "matmul", [b_wo, b_mix], [b_pt], pt[:, :n],
                             lhsT=wo[:, kt, ot * 128:(ot + 1) * 128], rhs=mix[:, kt, :n],
                             start=(kt == 0), stop=(kt == 7))
                    k.op("dve", "scalar_tensor_tensor", [b_pt, self.b_mT, b_xc], [b_xc], out=xc[:, ot, :n],
                         in0=pt[:, :n], scalar=self.mT[:, li, 16 + ot, j:j + 1], in1=xc[:, ot, :n],
                         op0=ALU.mult, op1=ALU.add)
                k.dma("sp", self.xT[:, :, t0:t0 + n].rearrange("k p t -> p k t"), xc[:, :, :n], [b_xc], [self.b_xT])
            k.barrier()

    def phase_moe_dense(self, li, l):
        nc, k = self.nc, self.k
        from contextlib import ExitStack
        w = self.W[l]
        idf = self.cm_f[:, 0, :]
        with ExitStack() as st:
            XT, b_XT = self.tl(st, "XT", [128, 8, PASS_T], BF16)
            gate, b_gate = self.tl(st, "gate", [128, PASS_NT, 32], F32)
            wsets = []
            for i in range(2):
                wg, b_wg = self.tl(st, f"wg{i}", [128, 8, 512], BF16)
                wu, b_wu = self.tl(st, f"wu{i}", [128, 8, 512], BF16)
                wd, b_wd = self.tl(st, f"wd{i}", [128, 4, D], BF16)
                wsets.append((wg, b_wg, wu, b_wu, wd, b_wd))
            H, b_H = self.tl(st, "H", [128, 4, 512], BF16)
            sg, b_sg = self.tl(st, "sg", [128, 512], F32)
            wr, b_wr = self.tl(st, "wr", [128, 8, 36], F32)
            brt, b_brt = self.tl(st, "brt", [128, 36], F32)
            k.dma("sp", wr[:], w["w_r"].rearrange("(k p) e -> p k e", p=128), [], [b_wr])
            k.dma("sp", brt[:], w["b_r"][0:1, :].partition_broadcast(128), [], [b_brt])
            wcnt = 0
            for pi, subs in enumerate(PASSES):
                p0 = pi * PASS_T
                with ExitStack() as st2:
                    xc, b_xc = self.tl(st2, "xc", [128, 8, 512], F32)
                    gT, b_gT = self.tl(st2, "gT", [128, 8, 512], F32)
                    sq, b_sq = self.tl(st2, "sq", [128, 8, 512], BF16)
                    tmp, b_tmp = self.tl(st2, "tmp", [128, 8, 512], F32)
                    rs, b_rs = self.tl(st2, "rs", [128, 512], F32)
                    sm, b_sm = self.tl(st2, "sm", [128, 160], F32)
                    for (t0, n) in subs:
                        j = 1 if t0 < NCTX else 0
                        lc = t0 - p0
                        k.dma("sp", xc[:, :, :n], self.xT[:, :, t0:t0 + n].rearrange("k p t -> p k t"),
                              [self.b_xT], [b_xc])
                        self.norm_mod(xc, b_xc, n, li, 1, j, lambda kt: gT[:, kt, :n], b_gT, sq, b_sq, rs, b_rs,
                                      tmp, b_tmp, 7)
                        k.op("act", "activation", [b_gT], [b_XT], out=XT[:, :, lc:lc + n], in_=gT[:, :, :n],
                             func=AF.Copy)
                        for s in range(n // 128):
                            ti = (lc + s * 128) // 128
                            pr, b_pr = self.bank(6)
                            for kt in range(8):
                                k.op("pe", "matmul", [b_gT, b_wr], [b_pr], pr[:, 0:36],
                                     lhsT=gT[:, kt, s * 128:(s + 1) * 128], rhs=wr[:, kt, :],
                                     start=(kt == 0), stop=(kt == 7))
                            self.router(pr, b_pr, brt, b_brt, sm, b_sm, gate[:, ti, :], b_gate)
                    k.barrier()
                st3 = ExitStack()
                facc, b_facc = self.tl(st3, "facc", [128, PASS_NT, D], F32)
                for e in range(NEXP):
                    wg, b_wg, wu, b_wu, wd, b_wd = wsets[wcnt % 2]
                    wcnt += 1
                    k.dma("pool", wg[:], w["w_gate"][e].rearrange("(k p) f -> p k f", p=128), [], [b_wg])
                    k.dma("pool", wu[:], w["w_up"][e].rearrange("(k p) f -> p k f", p=128), [], [b_wu])
                    k.dma("pool", wd[:], w["w_down"][e].rearrange("(k p) f -> p k f", p=128), [], [b_wd])
                    for (t0, n) in subs:
                        lc = t0 - p0
                        for ft in range(4):
                            pg, b_pg = self.bank(ft % 2)
                            pu, b_pu = self.bank(2 + ft % 2)
                            for kt in range(8):
                                k.op("pe", "matmul", [b_wg, b_XT], [b_pg], pg[:, :n],
                                     lhsT=wg[:, kt, ft * 128:(ft + 1) * 128], rhs=XT[:, kt, lc:lc + n],
                                     start=(kt == 0), stop=(kt == 7))
                            for kt in range(8):
                                k.op("pe", "matmul", [b_wu, b_XT], [b_pu], pu[:, :n],
                                     lhsT=wu[:, kt, ft * 128:(ft + 1) * 128], rhs=XT[:, kt, lc:lc + n],
                                     start=(kt == 0), stop=(kt == 7))
                            k.op("act", "activation", [b_pg], [b_sg], out=sg[:, :n], in_=pg[:, :n], func=AF.Silu)
                            k.op("dve", "tensor_tensor", [b_sg, b_pu], [b_H], out=H[:, ft, :n], in0=sg[:, :n],
                                 in1=pu[:, :n], op=ALU.mult)
                        for s in range(n // 128):
                            ti = (lc + s * 128) // 128
                            for half in range(2):
                                pd, b_pd = self.bank(4 + half)
                                for ft in range(4):
                                    k.op("pe", "matmul", [b_H, b_wd], [b_pd], pd,
                                         lhsT=H[:, ft, s * 128:(s + 1) * 128],
                                         rhs=wd[:, ft, half * 512:(half + 1) * 512],
                                         start=(ft == 0), stop=(ft == 3))
                                fa = facc[:, ti, half * 512:(half + 1) * 512]
                                if e == 0:
                                    k.op("dve", "tensor_scalar", [b_pd, b_gate], [b_facc], out=fa, in0=pd,
                                         scalar1=gate[:, ti, e:e + 1], scalar2=None, op0=ALU.mult)
                                else:
                                    k.op("dve", "scalar_tensor_tensor", [b_pd, b_gate, b_facc], [b_facc], out=fa,
                                         in0=pd, scalar=gate[:, ti, e:e + 1], in1=fa, op0=ALU.mult, op1=ALU.add)
                k.barrier()
                with ExitStack() as st2:
                    xts = [self.tl(st2, f"xt{i}", [128, 8, 128], F32) for i in range(2)]
                    for ti in range(PASS_NT):
                        t0 = p0 + ti * 128
                        j = 1 if t0 < NCTX else 0
                        xt, b_xt = xts[ti % 2]
                        k.dma("sp", xt[:], self.xT[:, :, t0:t0 + 128].rearrange("k p t -> p k t"),
                              [self.b_xT], [b_xt])
                        for half in range(2):
                            pt, b_pt = self.bank((ti % 2) * 2 + half)
                            for kk in range(4):
                                kt = half * 4 + kk
                                k.op("pe", "transpose", [b_facc, self.b_cmf], [b_pt], pt[:, kk * 128:(kk + 1) * 128],
                                     facc[:, ti, kt * 128:(kt + 1) * 128], idf)
                            for kk in range(4):
                                kt = half * 4 + kk
                                k.op("dve", "scalar_tensor_tensor", [b_pt, self.b_mT, b_xt], [b_xt],
                                     out=xt[:, kt, :], in0=pt[:, kk * 128:(kk + 1) * 128],
                                     scalar=self.mT[:, li, 40 + kt, j:j + 1], in1=xt[:, kt, :],
                                     op0=ALU.mult, op1=ALU.add)
                        k.dma("sp", self.xT[:, :, t0:t0 + 128].rearrange("k p t -> p k t"), xt[:],
                              [b_xt], [self.b_xT])
                    k.barrier()
                st3.close()

    def phase_moe(self, li, l):
        nc, k = self.nc, self.k
        from contextlib import ExitStack
        w = self.W[l]
        idf = self.cm_f[:, 0, :]
        NBLK = 100
        if not hasattr(self, "Xs"):
            self.Xs_t = nc.dram_tensor("Xs", [NBLK * 128, D], BF16, kind="Internal")
            self.Ys_t = nc.dram_tensor("Ys", [NBLK * 128, D], F32, kind="Internal")
            self.Xs, self.Ys = self.Xs_t.ap(), self.Ys_t.ap()
            self.b_Xs, self.b_Ys = k.buf("Xs"), k.buf("Ys")
        with ExitStack() as st:
            dest, b_dest = self.tl(st, "dest", [128, NT, 2], I32)
            gw, b_gw = self.tl(st, "gw", [128, NT, 2], F32)
            idxG, b_idxG = self.tl(st, "idxG", [128, NBLK], I32)
            with ExitStack() as st2:
                gall, b_gall = self.tl(st2, "gall", [128, NT, D], BF16)
                selA, b_selA = self.tl(st2, "selA", [128, NT, 2, 32], F32)
                rk, b_rk = self.tl(st2, "rk", [128, NT, 2], F32)
                sm, b_sm = self.tl(st2, "sm", [128, 320], F32)
                selb, b_selb = self.tl(st2, "selb", [128, 32], BF16)
                tot, b_tot = self.tl(st2, "tot", [128, 32], F32)
                wr, b_wr = self.tl(st2, "wr", [128, 8, 36], F32)
                brt, b_brt = self.tl(st2, "brt", [128, 36], F32)
                k.dma("sp", wr[:], w["w_r"].rearrange("(k p) e -> p k e", p=128), [], [b_wr])
                k.dma("sp", brt[:], w["b_r"][0:1, :].partition_broadcast(128), [], [b_brt])
                k.op("dve", "memset", [], [b_tot], tot[:], 0.0)
                Ltri = self.cm_b[:, 4, :]
                ones_b = self.cm_b[:, 1, :]
                R, Wm = [b_sm], [b_sm]
                with ExitStack() as st3:
                    xc, b_xc = self.tl(st3, "xc", [128, 8, 512], F32)
                    gT, b_gT = self.tl(st3, "gT", [128, 8, 512], F32)
                    sq, b_sq = self.tl(st3, "sq", [128, 8, 512], BF16)
                    tmp, b_tmp = self.tl(st3, "tmp", [128, 8, 512], F32)
                    rs, b_rs = self.tl(st3, "rs", [128, 512], F32)
                    for (t0, n) in CHUNKS:
                        j = 1 if t0 < NCTX else 0
                        k.dma("sp", xc[:, :, :n], self.xT[:, :, t0:t0 + n].rearrange("k p t -> p k t"),
                              [self.b_xT], [b_xc])
                        self.norm_mod(xc, b_xc, n, li, 1, j, lambda kt: gT[:, kt, :n], b_gT, sq, b_sq, rs, b_rs,
                                      tmp, b_tmp, 7)
                        for s in range(n // 128):
                            ti = (t0 + s * 128) // 128
                            pr, b_pr = self.bank(6)
                            for kt in range(8):
                                k.op("pe", "matmul", [b_gT, b_wr], [b_pr], pr[:, 0:36],
                                     lhsT=gT[:, kt, s * 128:(s + 1) * 128], rhs=wr[:, kt, :],
                                     start=(kt == 0), stop=(kt == 7))
                            f = self.router2(pr, b_pr, brt, b_brt, sm, b_sm)
                            k.op("dve", "tensor_tensor", R, [b_selb], out=selb[:], in0=f["sel1"], in1=f["sel2"], op=ALU.add)
                            k.op("dve", "tensor_copy", R, [b_selA], out=selA[:, ti, 0, :], in_=f["sel1"])
                            k.op("dve", "tensor_copy", R, [b_selA], out=selA[:, ti, 1, :], in_=f["sel2"])
                            pc, b_pc = self.bank(5)
                            k.op("pe", "matmul", [b_selb, self.b_cmb], [b_pc], pc[:, 0:32], lhsT=Ltri, rhs=selb[:],
                                 start=True, stop=True)
                            k.op("pe", "matmul", [b_selb, self.b_cmb], [b_pc], pc[:, 32:64], lhsT=ones_b, rhs=selb[:],
                                 start=True, stop=True)
                            slot = sm[:, 200:232]
                            k.op("dve", "tensor_tensor", [b_pc, b_tot], Wm, out=slot, in0=pc[:, 0:32], in1=tot[:], op=ALU.add)
                            k.op("dve", "tensor_tensor", [b_pc, b_tot], [b_tot], out=tot[:], in0=pc[:, 32:64], in1=tot[:],
                                 op=ALU.add)
                            t32 = sm[:, 232:264]
                            for kk, nm_ in enumerate(("sel1", "sel2")):
                                k.op("dve", "tensor_tensor", R, Wm, out=t32, in0=f[nm_], in1=slot, op=ALU.mult)
                                k.op("dve", "tensor_reduce", R, [b_rk], out=rk[:, ti, kk:kk + 1], in_=t32, axis=AX.X, op=ALU.add)
                            k.op("dve", "tensor_copy", R, [b_gw], out=gw[:, ti, 0:1], in_=f["gA"])
                            k.op("dve", "tensor_copy", R, [b_gw], out=gw[:, ti, 1:2], in_=f["gB"])
                            for half in range(2):
                                pt, b_pt = self.bank(half)
                                for kk in range(4):
                                    kt = half * 4 + kk
                                    k.op("pe", "transpose", [b_gT, self.b_cmf], [b_pt], pt[:, kk * 128:(kk + 1) * 128],
                                         gT[:, kt, s * 128:(s + 1) * 128], idf)
                                if half == 0:
                                    k.op("act", "activation", [b_pt], [b_gall], out=gall[:, ti, 0:512], in_=pt, func=AF.Copy)
                                else:
                                    k.op("dve", "tensor_copy", [b_pt], [b_gall], out=gall[:, ti, 512:1024], in_=pt)
                with ExitStack() as st3:
                    nb, b_nb = self.tl(st3, "nb", [128, 32], F32)
                    pe_, b_pe = self.tl(st3, "pend", [128, 32], F32)
                    pst, b_pst = self.tl(st3, "pstart", [128, 32], F32)
                    onesr, b_onesr = self.tl(st3, "onesr", [128, 32], F32)
                    bio, b_bio = self.tl(st3, "bio", [128, NBLK], F32)
                    bioi, b_bioi = self.tl(st3, "bioi", [128, NBLK], I32)
                    bexp, b_bexp = self.tl(st3, "bexp", [128, NBLK], F32)
                    basei, b_basei = self.tl(st3, "basei", [128, 1], I32)
                    basef, b_basef = self.tl(st3, "basef", [128, 1], F32)
                    idf32, b_idf32 = self.tl(st3, "idf32", [128, NBLK], F32)
                    k.op("dve", "tensor_scalar", [b_tot], [b_nb], out=nb[:], in0=tot[:], scalar1=0.0, scalar2=None,
                         op0=ALU.is_gt)
                    for jj in range(1, 70):
                        k.op("dve", "scalar_tensor_tensor", [b_tot, b_nb], [b_nb], out=nb[:], in0=tot[:],
                             scalar=float(128 * jj), in1=nb[:], op0=ALU.is_gt, op1=ALU.add)
                    k.op("dve", "memset", [], [b_onesr], onesr[:], 1.0)
                    k.op("dve", "tensor_tensor_scan", [b_onesr, b_nb], [b_pe], out=pe_[:], data0=onesr[:], data1=nb[:],
                         initial=0.0, op0=ALU.mult, op1=ALU.add)
                    k.op("dve", "tensor_tensor", [b_pe, b_nb], [b_pst], out=pst[:], in0=pe_[:], in1=nb[:], op=ALU.subtract)
                    k.op("dve", "tensor_scalar", [b_pst], [b_pst], out=pst[:], in0=pst[:], scalar1=128.0, scalar2=None,
                         op0=ALU.mult)
                    k.op("pool", "iota", [], [b_bioi], bioi[:], pattern=[[1, NBLK]], base=0, channel_multiplier=0)
                    k.op("dve", "tensor_copy", [b_bioi], [b_bio], out=bio[:], in_=bioi[:])
                    k.op("dve", "tensor_scalar", [b_bio, b_pe], [b_bexp], out=bexp[:], in0=bio[:], scalar1=pe_[:, 0:1],
                         scalar2=None, op0=ALU.is_ge)
                    for e in range(1, 32):
                        k.op("dve", "scalar_tensor_tensor", [b_bio, b_pe, b_bexp], [b_bexp], out=bexp[:], in0=bio[:],
                             scalar=pe_[:, e:e + 1], in1=bexp[:], op0=ALU.is_ge, op1=ALU.add)
                    k.op("dve", "tensor_scalar", [b_bexp], [b_bexp], out=bexp[:], in0=bexp[:], scalar1=31.0, scalar2=None,
                         op0=ALU.min)
                    k.op("pool", "iota", [], [b_basei], basei[:], pattern=[[0, 1]], base=0, channel_multiplier=1)
                    k.op("dve", "tensor_copy", [b_basei], [b_basef], out=basef[:], in_=basei[:])
                    k.op("dve", "tensor_scalar", [b_bexp, b_basef], [b_idf32], out=idf32[:], in0=bexp[:], scalar1=128.0,
                         scalar2=basef[:, 0:1], op0=ALU.mult, op1=ALU.add)
                    k.op("dve", "tensor_copy", [b_idf32], [b_idxG], out=idxG[:], in_=idf32[:])
                    t32 = sm[:, 232:264]
                    df = sm[:, 264:266]
                    for ti in range(NT):
                        for kk in range(2):
                            k.op("dve", "tensor_tensor", [b_selA, b_pst], Wm, out=t32, in0=selA[:, ti, kk, :], in1=pst[:],
                                 op=ALU.mult)
                            k.op("dve", "tensor_reduce", R, Wm, out=df[:, kk:kk + 1], in_=t32, axis=AX.X, op=ALU.add)
                        k.op("dve", "tensor_tensor", R + [b_rk], Wm, out=df, in0=df, in1=rk[:, ti, :], op=ALU.add)
                        k.op("dve", "tensor_copy", R, [b_dest], out=dest[:, ti, :], in_=df)
                        for kk in range(2):
                            k.dma("pool", None, None, [b_gall, b_dest], [self.b_Xs],
                                  indirect=lambda e, kk=kk, ti=ti: e.indirect_dma_start(
                                      out=self.Xs_t[:, :],
                                      out_offset=bass.IndirectOffsetOnAxis(ap=dest[:, ti, kk:kk + 1], axis=0),
                                      in_=gall[:, ti, :], in_offset=None))
                k.barrier()
            with ExitStack() as st2:
                wsets = []
                for i in range(2):
                    wg, b_wg = self.tl(st2, f"wg{i}", [128, 8, 512], BF16)
                    wu, b_wu = self.tl(st2, f"wu{i}", [128, 8, 512], BF16)
                    wd, b_wd = self.tl(st2, f"wd{i}", [128, 4, D], BF16)
                    wsets.append((wg, b_wg, wu, b_wu, wd, b_wd))
                xes = [self.tl(st2, f"xe{i}", [128, D], BF16) for i in range(2)]
                XeTs = [self.tl(st2, f"XeT{i}", [128, 8, 128], BF16) for i in range(2)]
                H, b_H = self.tl(st2, "H", [128, 4, 128], BF16)
                sg, b_sg = self.tl(st2, "sg", [128, 512], F32)
                Yts = [self.tl(st2, f"Yt{i}", [128, D], F32) for i in range(2)]
                idb = self.cm_b[:, 0, :]
                wgt = self.Wt[l]["w_gate"]
                wut = self.Wt[l]["w_up"]
                wdt = self.Wt[l]["w_down"]
                for b in range(NBLK):
                    wg, b_wg, wu, b_wu, wd, b_wd = wsets[b % 2]
                    xe, b_xe = xes[b % 2]
                    XeT, b_XeT = XeTs[b % 2]
                    Yt, b_Yt = Yts[b % 2]
                    for (wt_, dst, b_dst) in ((wgt, wg, b_wg), (wut, wu, b_wu), (wdt, wd, b_wd)):
                        k.dma("pool", None, None, [b_idxG], [b_dst],
                              indirect=lambda e, wt_=wt_, dst=dst, b=b: e.indirect_dma_start(
                                  out=dst[:].rearrange("p k f -> p (k f)"), out_offset=None, in_=wt_[:, :],
                                  in_offset=bass.IndirectOffsetOnAxis(ap=idxG[:, b:b + 1], axis=0)))
                    k.dma("sp", xe[:], self.Xs[b * 128:(b + 1) * 128, :], [self.b_Xs], [b_xe])
                    pt, b_pt = self.bank(6 + b % 2)
                    ptb = pt.bitcast(BF16)
                    for kt in range(8):
                        k.op("pe", "transpose", [b_xe, self.b_cmb], [b_pt], ptb[:, kt * 128:(kt + 1) * 128],
                             xe[:, kt * 128:(kt + 1) * 128], idb)
                    src_v = ptb.rearrange("p (a b) -> p a b", b=128)
                    if b % 2 == 0:
                        k.op("act", "activation", [b_pt], [b_XeT], out=XeT[:], in_=src_v, func=AF.Copy)
                    else:
                        k.op("dve", "tensor_copy", [b_pt], [b_XeT], out=XeT[:], in_=src_v)
                    pg, b_pg = self.bank(b % 2)
                    pu, b_pu = self.bank(2 + b % 2)
                    for ft in range(4):
                        for kt in range(8):
                            k.op("pe", "matmul", [b_wg, b_XeT], [b_pg], pg[:, ft * 128:(ft + 1) * 128],
                                 lhsT=wg[:, kt, ft * 128:(ft + 1) * 128], rhs=XeT[:, kt, :], start=(kt == 0), stop=(kt == 7))
                    for ft in range(4):
                        for kt in range(8):
                            k.op("pe", "matmul", [b_wu, b_XeT], [b_pu], pu[:, ft * 128:(ft + 1) * 128],
                                 lhsT=wu[:, kt, ft * 128:(ft + 1) * 128], rhs=XeT[:, kt, :], start=(kt == 0), stop=(kt == 7))
                    k.op("act", "activation", [b_pg], [b_sg], out=sg[:], in_=pg, func=AF.Silu)
                    k.op("dve", "tensor_tensor", [b_sg, b_pu], [b_H], out=H[:].rearrange("p a b -> p (a b)"), in0=sg[:],
                         in1=pu, op=ALU.mult)
                    for half in range(2):
                        pd, b_pd = self.bank(4 + half)
                        for ft in range(4):
                            k.op("pe", "matmul", [b_H, b_wd], [b_pd], pd, lhsT=H[:, ft, :],
                                 rhs=wd[:, ft, half * 512:(half + 1) * 512], start=(ft == 0), stop=(ft == 3))
                        if half == 0:
                            k.op("act", "activation", [b_pd], [b_Yt], out=Yt[:, 0:512], in_=pd, func=AF.Copy)
                        else:
                            k.op("dve", "tensor_copy", [b_pd], [b_Yt], out=Yt[:, 512:1024], in_=pd)
                    k.dma("sp", self.Ys[b * 128:(b + 1) * 128, :], Yt[:], [b_Yt], [self.b_Ys])
                k.barrier()
            with ExitStack() as st2:
                xts = [self.tl(st2, f"xt{i}", [128, 8, 128], F32) for i in range(2)]
                Y0s = [self.tl(st2, f"Y0{i}", [128, D], F32) for i in range(2)]
                Y1s = [self.tl(st2, f"Y1{i}", [128, D], F32) for i in range(2)]
                for ti in range(NT):
                    t0 = ti * 128
                    j = 1 if t0 < NCTX else 0
                    xt, b_xt = xts[ti % 2]
                    Y0, b_Y0 = Y0s[ti % 2]
                    Y1, b_Y1 = Y1s[ti % 2]
                    k.dma("sp", xt[:], self.xT[:, :, t0:t0 + 128].rearrange("k p t -> p k t"), [self.b_xT], [b_xt])
                    for kk, (Yk, b_Yk) in enumerate(((Y0, b_Y0), (Y1, b_Y1))):
                        k.dma("pool", None, None, [self.b_Ys, b_dest], [b_Yk],
                              indirect=lambda e, kk=kk, Yk=Yk, ti=ti: e.indirect_dma_start(
                                  out=Yk[:, :], out_offset=None, in_=self.Ys_t[:, :],
                                  in_offset=bass.IndirectOffsetOnAxis(ap=dest[:, ti, kk:kk + 1], axis=0)))
                    k.op("dve", "tensor_scalar", [b_Y0, b_gw], [b_Y0], out=Y0[:], in0=Y0[:], scalar1=gw[:, ti, 0:1],
                         scalar2=None, op0=ALU.mult)
                    k.op("dve", "scalar_tensor_tensor", [b_Y1, b_gw, b_Y0], [b_Y0], out=Y0[:], in0=Y1[:],
                         scalar=gw[:, ti, 1:2], in1=Y0[:], op0=ALU.mult, op1=ALU.add)
                    for half in range(2):
                        pt, b_pt = self.bank((ti % 2) * 2 + half)
                        for kk in range(4):
                            kt = half * 4 + kk
                            k.op("pe", "transpose", [b_Y0, self.b_cmf], [b_pt], pt[:, kk * 128:(kk + 1) * 128],
                                 Y0[:, kt * 128:(kt + 1) * 128], idf)
                        for kk in range(4):
                            kt = half * 4 + kk
                            k.op("dve", "scalar_tensor_tensor", [b_pt, self.b_mT, b_xt], [b_xt],
                                 out=xt[:, kt, :], in0=pt[:, kk * 128:(kk + 1) * 128],
                                 scalar=self.mT[:, li, 40 + kt, j:j + 1], in1=xt[:, kt, :],
                                 op0=ALU.mult, op1=ALU.add)
                    k.dma("sp", self.xT[:, :, t0:t0 + 128].rearrange("k p t -> p k t"), xt[:], [b_xt], [self.b_xT])
                k.barrier()

    def router2(self, pr, b_pr, brt, b_brt, sm, b_sm):
        k = self.k
        R, W_ = [b_sm], [b_sm]
        lg = sm[:, 0:36]
        m1 = sm[:, 36:37]
        nm1 = sm[:, 37:38]
        oh1 = sm[:, 40:44]
        e1 = sm[:, 44:48]
        s1 = sm[:, 48:49]
        pg = sm[:, 49:50]
        pen = sm[:, 52:56]
        lg2m = sm[:, 56:88]
        top8 = sm[:, 88:96]
        d21 = sm[:, 96:97]
        ex = sm[:, 97:98]
        den = sm[:, 98:99]
        w1 = sm[:, 99:100]
        w2 = sm[:, 100:101]
        gA = sm[:, 101:102]
        gB = sm[:, 102:103]
        sel1 = sm[:, 104:136]
        sel2 = sm[:, 136:168]
        k.op("dve", "tensor_tensor", [b_pr, b_brt], W_, out=lg, in0=pr[:, 0:36], in1=brt[:, :], op=ALU.add)
        k.op("dve", "tensor_reduce", R, W_, out=m1, in_=lg[:, 0:4], axis=AX.X, op=ALU.max)
        k.op("dve", "tensor_scalar", R, W_, out=oh1, in0=lg[:, 0:4], scalar1=m1, scalar2=None, op0=ALU.is_equal)
        k.op("dve", "tensor_scalar", R, W_, out=nm1, in0=m1, scalar1=-1.0, scalar2=None, op0=ALU.mult)
        k.op("act", "activation", R, W_, out=e1, in_=lg[:, 0:4], func=AF.Exp, bias=nm1, scale=1.0)
        k.op("dve", "tensor_reduce", R, W_, out=s1, in_=e1, axis=AX.X, op=ALU.add)
        k.op("dve", "reciprocal", R, W_, out=pg, in_=s1)
        k.op("dve", "tensor_scalar", R, W_, out=pen, in0=oh1, scalar1=1e30, scalar2=-1e30, op0=ALU.mult, op1=ALU.add)
        k.op("dve", "tensor_tensor", R, W_, out=lg2m.rearrange("p (g i) -> p g i", i=8),
             in0=lg[:, 4:36].rearrange("p (g i) -> p g i", i=8),
             in1=pen.unsqueeze(2).broadcast_to([128, 4, 8]), op=ALU.add)
        k.op("dve", "max", R, W_, out=top8, in_=lg2m)
        k.op("dve", "tensor_tensor", R, W_, out=d21, in0=top8[:, 1:2], in1=top8[:, 0:1], op=ALU.subtract)
        k.op("act", "activation", R, W_, out=ex, in_=d21, func=AF.Exp)
        k.op("dve", "tensor_scalar", R, W_, out=den, in0=ex, scalar1=1.0, scalar2=None, op0=ALU.add)
        k.op("dve", "reciprocal", R, W_, out=w1, in_=den)
        k.op("dve", "tensor_tensor", R, W_, out=w2, in0=ex, in1=w1, op=ALU.mult)
        k.op("dve", "tensor_tensor", R, W_, out=gA, in0=w1, in1=pg, op=ALU.mult)
        k.op("dve", "tensor_tensor", R, W_, out=gB, in0=w2, in1=pg, op=ALU.mult)
        k.op("dve", "tensor_scalar", R, W_, out=sel1, in0=lg2m, scalar1=top8[:, 0:1], scalar2=None, op0=ALU.is_equal)
        k.op("dve", "tensor_scalar", R, W_, out=sel2, in0=lg2m, scalar1=top8[:, 1:2], scalar2=None, op0=ALU.is_equal)
        return dict(sel1=sel1, sel2=sel2, gA=gA, gB=gB)

    def router(self, pr, b_pr, brt, b_brt, sm, b_sm, gout, b_gate):
        k = self.k
        R, W_ = [b_sm], [b_sm]
        lg = sm[:, 0:36]
        m1 = sm[:, 36:37]
        nm1 = sm[:, 37:38]
        oh1 = sm[:, 40:44]
        e1 = sm[:, 44:48]
        s1 = sm[:, 48:49]
        pg = sm[:, 49:50]
        pen = sm[:, 52:56]
        lg2m = sm[:, 56:88]
        top8 = sm[:, 88:96]
        d21 = sm[:, 96:97]
        ex = sm[:, 97:98]
        den = sm[:, 98:99]
        w1 = sm[:, 99:100]
        w2 = sm[:, 100:101]
        gA = sm[:, 101:102]
        gB = sm[:, 102:103]
        sel = sm[:, 104:136]
        k.op("dve", "tensor_tensor", [b_pr, b_brt], W_, out=lg, in0=pr[:, 0:36], in1=brt[:, :], op=ALU.add)
        k.op("dve", "tensor_reduce", R, W_, out=m1, in_=lg[:, 0:4], axis=AX.X, op=ALU.max)
        k.op("dve", "tensor_scalar", R, W_, out=oh1, in0=lg[:, 0:4], scalar1=m1, scalar2=None, op0=ALU.is_equal)
        k.op("dve", "tensor_scalar", R, W_, out=nm1, in0=m1, scalar1=-1.0, scalar2=None, op0=ALU.mult)
        k.op("act", "activation", R, W_, out=e1, in_=lg[:, 0:4], func=AF.Exp, bias=nm1, scale=1.0)
        k.op("dve", "tensor_reduce", R, W_, out=s1, in_=e1, axis=AX.X, op=ALU.add)
        k.op("dve", "reciprocal", R, W_, out=pg, in_=s1)
        k.op("dve", "tensor_scalar", R, W_, out=pen, in0=oh1, scalar1=1e30, scalar2=-1e30, op0=ALU.mult, op1=ALU.add)
        k.op("dve", "tensor_tensor", R, W_, out=lg2m.rearrange("p (g i) -> p g i", i=8),
             in0=lg[:, 4:36].rearrange("p (g i) -> p g i", i=8),
             in1=pen.unsqueeze(2).broadcast_to([128, 4, 8]), op=ALU.add)
        k.op("dve", "max", R, W_, out=top8, in_=lg2m)
        k.op("dve", "tensor_tensor", R, W_, out=d21, in0=top8[:, 1:2], in1=top8[:, 0:1], op=ALU.subtract)
        k.op("act", "activation", R, W_, out=ex, in_=d21, func=AF.Exp)
        k.op("dve", "tensor_scalar", R, W_, out=den, in0=ex, scalar1=1.0, scalar2=None, op0=ALU.add)
        k.op("dve", "reciprocal", R, W_, out=w1, in_=den)
        k.op("dve", "tensor_tensor", R, W_, out=w2, in0=ex, in1=w1, op=ALU.mult)
        k.op("dve", "tensor_tensor", R, W_, out=gA, in0=w1, in1=pg, op=ALU.mult)
        k.op("dve", "tensor_tensor", R, W_, out=gB, in0=w2, in1=pg, op=ALU.mult)
        k.op("dve", "tensor_scalar", R, W_, out=sel, in0=lg2m, scalar1=top8[:, 0:1], scalar2=gA,
             op0=ALU.is_equal, op1=ALU.mult)
        k.op("dve", "tensor_scalar", R, W_, out=lg2m, in0=lg2m, scalar1=top8[:, 1:2], scalar2=gB,
             op0=ALU.is_equal, op1=ALU.mult)
        k.op("dve", "tensor_tensor", R, [b_gate], out=gout, in0=sel, in1=lg2m, op=ALU.add)

    def s5_params(self, st, are, aim, ldt, P, C, tag):
        k = self.k
        pool_t, b_t = self.tl(st, "s5p" + tag, [128, 24, C], F32)
        R, Wr = [b_t], [b_t]
        nxt = [0]

        def new():
            i = nxt[0]
            nxt[0] += 1
            assert i < 24
            return pool_t[0:P, i, :]

        def ts(out, in0, s1, s2, op0, op1=None):
            if op1 is None:
                k.op("dve", "tensor_scalar", R, Wr, out=out, in0=in0, scalar1=s1, scalar2=None, op0=op0)
            else:
                k.op("dve", "tensor_scalar", R, Wr, out=out, in0=in0, scalar1=s1, scalar2=s2, op0=op0, op1=op1)

        def tt(out, a, b, op):
            k.op("dve", "tensor_tensor", R, Wr, out=out, in0=a, in1=b, op=op)

        def stt(out, in0, sc, in1, op0, op1):
            k.op("dve", "scalar_tensor_tensor", R, Wr, out=out, in0=in0, scalar=sc, in1=in1, op0=op0, op1=op1)

        dt = new()
        k.op("act", "activation", R + [self._b_s5raw], Wr, out=dt, in_=ldt, func=AF.Exp)
        xr = new()
        k.op("dve", "tensor_tensor", R + [self._b_s5raw], Wr, out=xr, in0=dt, in1=are, op=ALU.mult)
        p = new()
        ts(p, xr, 1.0 / 120, None, ALU.mult)
        for c in (1.0 / 24, 1.0 / 6, 0.5, 1.0):
            stt(p, p, c, xr, ALU.add, ALU.mult)
        mag = new()
        ts(mag, p, 1.0, None, ALU.add)
        th = new()
        k.op("dve", "tensor_tensor", R + [self._b_s5raw], Wr, out=th, in0=dt, in1=aim, op=ALU.mult)
        kk = new()
        ts(kk, th, math.pi, None, ALU.is_gt)
        for j in range(2, 6):
            stt(kk, th, (2 * j - 1) * math.pi, kk, ALU.is_gt, ALU.add)
        thr = new()
        stt(thr, kk, -2.0 * math.pi, th, ALU.mult, ALU.add)
        x8 = new()
        ts(x8, thr, 0.125, None, ALU.mult)
        x2 = new()
        tt(x2, x8, x8, ALU.mult)
        s = new()
        ts(s, x2, 1.0 / 362880, None, ALU.mult)
        for c in (-1.0 / 5040, 1.0 / 120, -1.0 / 6):
            stt(s, s, c, x2, ALU.add, ALU.mult)
        stt(s, s, 1.0, x8, ALU.add, ALU.mult)
        cc = new()
        ts(cc, x2, 1.0 / 40320, None, ALU.mult)
        for c in (-1.0 / 720, 1.0 / 24, -0.5):
            stt(cc, cc, c, x2, ALU.add, ALU.mult)
        ts(cc, cc, 1.0, None, ALU.add)
        s_b, c_b, t_b = new(), new(), new()
        cur_c, cur_s, oth_c, oth_s = cc, s, c_b, s_b
        for _ in range(3):
            stt(oth_s, cur_c, 2.0, cur_s, ALU.mult, ALU.mult)
            tt(t_b, cur_s, cur_s, ALU.mult)
            tt(oth_c, cur_c, cur_c, ALU.mult)
            tt(oth_c, oth_c, t_b, ALU.subtract)
            cur_c, cur_s, oth_c, oth_s = oth_c, oth_s, cur_c, cur_s
        c1, s1 = cur_c, cur_s
        abr, abi = new(), new()
        tt(abr, mag, c1, ALU.mult)
        tt(abi, mag, s1, ALU.mult)
        den = new()
        k.op("dve", "tensor_tensor", R + [self._b_s5raw], Wr, out=den, in0=are, in1=are, op=ALU.mult)
        k.op("dve", "tensor_tensor", R + [self._b_s5raw], Wr, out=t_b, in0=aim, in1=aim, op=ALU.mult)
        tt(den, den, t_b, ALU.add)
        k.op("dve", "reciprocal", R, Wr, out=den, in_=den)
        nr = new()
        ts(nr, abr, -1.0, None, ALU.add)
        zr, zi = new(), new()
        k.op("dve", "tensor_tensor", R + [self._b_s5raw], Wr, out=zr, in0=nr, in1=are, op=ALU.mult)
        k.op("dve", "tensor_tensor", R + [self._b_s5raw], Wr, out=t_b, in0=abi, in1=aim, op=ALU.mult)
        tt(zr, zr, t_b, ALU.add)
        tt(zr, zr, den, ALU.mult)
        k.op("dve", "tensor_tensor", R + [self._b_s5raw], Wr, out=zi, in0=abi, in1=are, op=ALU.mult)
        k.op("dve", "tensor_tensor", R + [self._b_s5raw], Wr, out=t_b, in0=nr, in1=aim, op=ALU.mult)
        tt(zi, zi, t_b, ALU.subtract)
        tt(zi, zi, den, ALU.mult)
        return dict(r=mag, c1=c1, s1=s1, zr=zr, zi=zi, buf=b_t)

    def phase_even(self, li, l):
        nc, k = self.nc, self.k
        from contextlib import ExitStack
        w = self.W[l]
        if not hasattr(self, "uT"):
            self.uT = self.scratch("uT", [4, 128, T], BF16)
            self.b_uT = k.buf("uT")
            self.yT = self.scratch("yT", [4, 128, T], F32)
            self.b_yT = k.buf("yT")
        idf = self.cm_f[:, 0, :]
        with ExitStack() as st:
            wi, b_wi = self.tl(st, "wi", [128, 8, 928], BF16)
            wuq, b_wuq = self.tl(st, "wuq", [128, 2, 768], BF16)
            wukv, b_wukv = self.tl(st, "wukv", [128, 1024], BF16)
            rope, b_rope = self.tl(st, "rope", [128, 2, T], F32)
            xcs = [self.tl(st, f"xc{i}", [128, 8, 512], F32) for i in range(2)]
            hTs = [self.tl(st, f"hT{i}", [128, 8, 512], BF16) for i in range(2)]
            sq, b_sq = self.tl(st, "sq", [128, 8, 512], BF16)
            tmp, b_tmp = self.tl(st, "tmp", [128, 8, 512], F32)
            rs, b_rs = self.tl(st, "rs", [128, 512], F32)
            gn, b_gn = self.tl(st, "gn", [128, 5], F32)
            qos = [self.tl(st, f"qo{i}", [128, 512], BF16) for i in range(2)]
            vts = [self.tl(st, f"vt{i}", [128, 512], BF16) for i in range(2)]
            uos = [self.tl(st, f"uo{i}", [128, 512], BF16) for i in range(2)]
            cqf, b_cqf = self.tl(st, "cqf", [128, 3, 512], F32)
            cqs, b_cqs = self.tl(st, "cqs", [128, 3, 512], BF16)
            cqn, b_cqn = self.tl(st, "cqn", [128, 3, 512], BF16)
            rs2, b_rs2 = self.tl(st, "rs2", [128, 2, 512], F32)
            krb, b_krb = self.tl(st, "krb", [32, 512], BF16)
            wk = self.qk_work(st)
            for kt in range(8):
                k.dma("pool", wi[:, kt, :], w["w_in"][kt * 128:(kt + 1) * 128, :], [], [b_wi])
            for i in range(2):
                k.dma("pool", wuq[:, i, :], w["w_uq"][i * 128:(i + 1) * 128, :], [], [b_wuq])
            k.dma("pool", wukv[:], w["w_ukv"], [], [b_wukv])
            k.dma("sp", rope[:], self.rope_m_d, [], [b_rope])
            k.dma("sp", gn[:, 0:1], w["gq"][0:128, :], [], [b_gn])
            k.dma("sp", gn[:, 1:2], w["gq"][128:256, :], [], [b_gn])
            k.dma("sp", gn[:, 2:3], w["gkv"], [], [b_gn])
            k.dma("sp", gn[0:96, 3:4], w["mqn"], [], [b_gn])
            k.dma("sp", gn[0:96, 4:5], w["mkn"], [], [b_gn])
            Rm = self.cm_b[0:96, 3, 0:96]
            ones_b = self.cm_b[:, 1, :]
            idb = self.cm_b[0:32, 0, 0:32]
            cnt = 0
            ucnt = 0
            for ci, (t0, n) in enumerate(CHUNKS):
                j = 1 if ci == 0 else 0
                xc, b_xc = xcs[ci % 2]
                hT, b_hT = hTs[ci % 2]
                k.dma("sp", xc[:, :, :n], self.xT[:, :, t0:t0 + n].rearrange("k p t -> p k t"), [self.b_xT], [b_xc])
                self.norm_mod(xc, b_xc, n, li, 0, j, lambda kt: hT[:, kt, :n], b_hT, sq, b_sq, rs, b_rs,
                              tmp, b_tmp, 0)
                for ut in range(4):
                    pt, b_pt = self.bank(1 + ut % 2)
                    for kt in range(8):
                        k.op("pe", "matmul", [b_wi, b_hT], [b_pt], pt[:, :n], lhsT=wi[:, kt, ut * 128:(ut + 1) * 128],
                             rhs=hT[:, kt, :n], start=(kt == 0), stop=(kt == 7))
                    uo, b_uo = uos[ucnt % 2]
                    ucnt += 1
                    k.op("act", "activation", [b_pt], [b_uo], out=uo[:, :n], in_=pt[:, :n], func=AF.Copy)
                    k.dma("sp", self.uT[ut, :, t0:t0 + n], uo[:, :n], [b_uo], [self.b_uT])
                if self.stop == "E1u":
                    continue
                for i3 in range(3):
                    pt, b_pt = self.bank(1 + i3 % 2)
                    c0 = 512 + i3 * 128
                    for kt in range(8):
                        k.op("pe", "matmul", [b_wi, b_hT], [b_pt], pt[:, :n], lhsT=wi[:, kt, c0:c0 + 128],
                             rhs=hT[:, kt, :n], start=(kt == 0), stop=(kt == 7))
                    k.op("act", "activation", [b_pt], [b_cqf], out=cqf[:, i3, :n], in_=pt[:, :n], func=AF.Copy)
                    k.op("act", "activation", [b_cqf], [b_cqs], out=cqs[:, i3, :n], in_=cqf[:, i3, :n], func=AF.Square)
                if self.stop == "E1c1":
                    continue
                pt, b_pt = self.bank(1)
                for kt in range(8):
                    k.op("pe", "matmul", [b_wi, b_hT], [b_pt], pt[0:32, :n], lhsT=wi[:, kt, 896:928],
                         rhs=hT[:, kt, :n], start=(kt == 0), stop=(kt == 7))
                k.op("act", "activation", [b_pt], [b_krb], out=krb[:, :n], in_=pt[0:32, :n], func=AF.Copy)
                if self.stop == "E1c2":
                    continue
                p7, b_p7 = self.bank(7)
                for i in range(2):
                    k.op("pe", "matmul", [b_cqs, self.b_cmb], [b_p7], p7[:, :n], lhsT=ones_b, rhs=cqs[:, i, :n],
                         start=(i == 0), stop=(i == 1))
                k.op("act", "activation", [b_p7], [b_rs2], out=rs2[:, 0, :n], in_=p7[:, :n], func=AF.Sqrt,
                     scale=1.0 / 256, bias=EPS)
                k.op("pe", "matmul", [b_cqs, self.b_cmb], [b_p7], p7[:, :n], lhsT=ones_b, rhs=cqs[:, 2, :n],
                     start=True, stop=True)
                k.op("act", "activation", [b_p7], [b_rs2], out=rs2[:, 1, :n], in_=p7[:, :n], func=AF.Sqrt,
                     scale=1.0 / 128, bias=EPS)
                k.op("dve", "reciprocal", [b_rs2], [b_rs2], out=rs2[:, :, :n], in_=rs2[:, :, :n])
                for i3 in range(3):
                    k.op("dve", "scalar_tensor_tensor", [b_cqf, b_gn, b_rs2], [b_cqn], out=cqn[:, i3, :n],
                         in0=cqf[:, i3, :n], scalar=gn[:, i3:i3 + 1], in1=rs2[:, 0 if i3 < 2 else 1, :n],
                         op0=ALU.mult, op1=ALU.mult)
                if self.stop == "E1c":
                    continue
                for hh in range(16 if self.stop != "E1q" else 8):
                    h = hh % 8
                    pt, b_pt = self.bank(1 + hh % 2)
                    if hh < 8:
                        for i in range(2):
                            k.op("pe", "matmul", [b_wuq, b_cqn], [b_pt], pt[0:96, :n],
                                 lhsT=wuq[:, i, h * 96:(h + 1) * 96], rhs=cqn[:, i, :n], start=(i == 0), stop=(i == 1))
                    else:
                        k.op("pe", "matmul", [b_wukv, b_cqn], [b_pt], pt[0:64, :n],
                             lhsT=wukv[:, h * 128:h * 128 + 64], rhs=cqn[:, 2, :n], start=True, stop=True)
                        k.op("pe", "matmul", [b_krb, self.b_cmb], [b_pt], pt[64:96, :n], lhsT=idb, rhs=krb[:, :n],
                             start=True, stop=True)
                    qo, b_qo = qos[cnt % 2]
                    cnt += 1
                    gi = 3 if hh < 8 else 4
                    self.qk_norm_rope(pt, b_pt, 96, n, gn[0:96, gi:gi + 1], b_gn, rope[0:96, 0, t0:t0 + n],
                                      rope[0:96, 1, t0:t0 + n], b_rope, Rm, qo[0:96, :n], b_qo, wk, 3, 4)
                    if hh < 8:
                        k.dma("sp", self.qT[h, 0:96, t0:t0 + n], qo[0:96, :n], [b_qo], [self.b_qT])
                    else:
                        k.dma("sp", self.kT[h, 0:96, t0:t0 + n], qo[0:96, :n], [b_qo], [self.b_kT])
                if self.stop in ("E1q", "E1k"):
                    continue
                for s in range(n // 128):
                    pv, b_pv = self.bank(5 + s % 2)
                    vt, b_vt = vts[s % 2]
                    k.op("pe", "matmul", [b_wukv, b_cqn], [b_pv], pv, lhsT=cqn[:, 2, s * 128:(s + 1) * 128],
                         rhs=wukv[:].rearrange("p (h c) -> p h c", c=128)[:, :, 64:128], start=True, stop=True)
                    k.op("act", "activation", [b_pv], [b_vt], out=vt[:], in_=pv, func=AF.Copy)
                    k.dma("sp", self.vS[t0 + s * 128:t0 + (s + 1) * 128, :], vt[:], [b_vt], [self.b_vS])
            k.barrier()
        if self.stop and self.stop.startswith("E1"):
            return
        self.phase_s5(li, l)
        if self.stop in ("S5", "S5prep"):
            return
        with ExitStack() as st:
            wgl, b_wgl = self.tl(st, "wgl", [128, 4, 512], BF16)
            dsk, b_dsk = self.tl(st, "dsk", [128, 4], F32)
            ycs = [self.tl(st, f"yc{i}", [128, 4, 512], F32) for i in range(2)]
            ucs = [self.tl(st, f"uc{i}", [128, 4, 512], BF16) for i in range(2)]
            gpre, b_gpre = self.tl(st, "gpre", [128, 4, 512], F32)
            gb, b_gb = self.tl(st, "gb", [128, 4, 512], BF16)
            sig, b_sig = self.tl(st, "sig", [128, 512], F32)
            sos = [self.tl(st, f"so{i}", [128, 512], BF16) for i in range(2)]
            for kt in range(4):
                k.dma("pool", wgl[:, kt, :], w["w_glu"][kt * 128:(kt + 1) * 128, :], [], [b_wgl])
                k.dma("sp", dsk[:, kt:kt + 1], w["s5_d"][kt * 128:(kt + 1) * 128, :], [], [b_dsk])
            scnt = 0
            for ci, (t0, n) in enumerate(CHUNKS):
                yc, b_yc = ycs[ci % 2]
                uc, b_uc = ucs[ci % 2]
                k.dma("sp", yc[:, :, :n], self.yT[:, :, t0:t0 + n].rearrange("k p t -> p k t"), [self.b_yT], [b_yc])
                k.dma("sp", uc[:, :, :n], self.uT[:, :, t0:t0 + n].rearrange("k p t -> p k t"), [self.b_uT], [b_uc])
                for ut in range(4):
                    k.op("dve", "scalar_tensor_tensor", [b_uc, b_dsk, b_yc], [b_gpre], out=gpre[:, ut, :n],
                         in0=uc[:, ut, :n], scalar=dsk[:, ut:ut + 1], in1=yc[:, ut, :n], op0=ALU.mult, op1=ALU.add)
                k.op("act", "activation", [b_gpre], [b_gb], out=gb[:, :, :n], in_=gpre[:, :, :n],
                     func=AF.Gelu_apprx_tanh)
                for nt_ in range(4):
                    pt, b_pt = self.bank(nt_ % 2)
                    for kt in range(4):
                        k.op("pe", "matmul", [b_wgl, b_gb], [b_pt], pt[:, :n], lhsT=wgl[:, kt, nt_ * 128:(nt_ + 1) * 128],
                             rhs=gb[:, kt, :n], start=(kt == 0), stop=(kt == 3))
                    k.op("act", "activation", [b_pt], [b_sig], out=sig[:, :n], in_=pt[:, :n], func=AF.Sigmoid)
                    so, b_so = sos[scnt % 2]
                    scnt += 1
                    k.op("dve", "tensor_tensor", [b_sig, b_gb], [b_so], out=so[:, :n], in0=sig[:, :n],
                         in1=gb[:, nt_, :n], op=ALU.mult)
                    k.dma("sp", self.mixT[nt_, :, t0:t0 + n], so[:, :n], [b_so], [self.b_mixT])
            k.barrier()
        if self.stop == "GLU":
            return
        self.phase_attention(8, 8, lambda h: h, 96, 64, 96 ** -0.5, lambda h: (4 + h // 2, (h % 2) * 64))

    def _s5_table_thunks(self, k, sp2, q, Ct, b_Ct, St, b_St, scb, tA, b_tA, tB, b_tB, b_sp2):
        sc_, b_sc = scb
        th = []
        th.append(lambda: k.op("dve", "memset", [], [b_Ct], Ct[:, 0:1], 1.0))
        th.append(lambda: k.op("pool", "memset", [], [b_St], St[:, 0:1], 0.0))
        th.append(lambda: k.op("dve", "tensor_copy", [b_sp2], [b_sc], out=sc_[:, 0:2], in_=sp2[:, 1:3, q]))
        ln = 1
        cur = 0
        while ln < T:
            m = min(ln, T - ln)
            ck = sc_[:, cur:cur + 1]
            sk = sc_[:, cur + 1:cur + 2]
            for m0 in range(0, m, 1024):
                mm = min(1024, m - m0)

                def lvl(m0=m0, mm=mm, ln=ln, ck=ck, sk=sk):
                    k.op("act", "activation", [b_St, b_sc], [b_tA], out=tA[:, :mm], in_=St[:, m0:m0 + mm],
                         func=AF.Copy, scale=sk)
                    k.op("act", "activation", [b_Ct, b_sc], [b_tB], out=tB[:, 0, :mm], in_=Ct[:, m0:m0 + mm],
                         func=AF.Copy, scale=sk)
                    k.op("act", "activation", [b_St, b_sc], [b_tB], out=tB[:, 1, :mm], in_=St[:, m0:m0 + mm],
                         func=AF.Copy, scale=ck)
                    k.op("dve", "scalar_tensor_tensor", [b_Ct, b_sc, b_tA], [b_Ct], out=Ct[:, ln + m0:ln + m0 + mm],
                         in0=Ct[:, m0:m0 + mm], scalar=ck, in1=tA[:, :mm], op0=ALU.mult, op1=ALU.subtract)
                    k.op("pool", "tensor_tensor", [b_tB], [b_St], out=St[:, ln + m0:ln + m0 + mm], in0=tB[:, 0, :mm],
                         in1=tB[:, 1, :mm], op=ALU.add)
                th.append(lvl)
            nx = 2 if cur == 0 else 0

            def sqr(ck=ck, sk=sk, nx=nx):
                k.op("dve", "scalar_tensor_tensor", [b_sc], [b_sc], out=sc_[:, nx + 1:nx + 2], in0=ck, scalar=2.0,
                     in1=sk, op0=ALU.mult, op1=ALU.mult)
                k.op("dve", "tensor_tensor", [b_sc], [b_sc], out=sc_[:, 4:5], in0=sk, in1=sk, op=ALU.mult)
                k.op("dve", "tensor_tensor", [b_sc], [b_sc], out=sc_[:, nx:nx + 1], in0=ck, in1=ck, op=ALU.mult)
                k.op("dve", "tensor_tensor", [b_sc], [b_sc], out=sc_[:, nx:nx + 1], in0=sc_[:, nx:nx + 1],
                     in1=sc_[:, 4:5], op=ALU.subtract)
            th.append(sqr)
            cur = nx
            ln *= 2
        return th

    def phase_s5(self, li, l):
        nc, k = self.nc, self.k
        from contextlib import ExitStack
        w = self.W[l]
        idf = self.cm_f[:, 0, :]
        with ExitStack() as st:
            Bl, b_Bl = self.tl(st, "Bl", [128, 2, 4, 2, 128], BF16)
            Cl, b_Cl = self.tl(st, "Cl", [128, 2, 4, 2, 128], BF16)
            Bl3, b_Bl3 = self.tl(st, "Bl3", [128, 2, 4, 2, 128], BF16)
            Cl3, b_Cl3 = self.tl(st, "Cl3", [128, 2, 4, 2, 128], BF16)
            sp2, b_sp2 = self.tl(st, "sp2", [128, 3, 32], F32)
            self._b_s5raw = k.buf("s5raw")
            with ExitStack() as st1:
                raw2, _ = self.tl(st1, "raw2", [128, 3, 32], F32)
                raw1, _ = self.tl(st1, "raw1", [64, 3, 64], F32)
                nat1, b_nat1 = self.tl(st1, "nat1", [64, 2, 64], F32)
                nat2, b_nat2 = self.tl(st1, "nat2", [32, 2, 128], F32)
                ldb, b_ldb = self.tl(st1, "ldb", [128, 64], F32)
                for i, nm_ in enumerate(("a_re", "a_im")):
                    k.dma("sp", nat1[:, i, :], w[nm_].rearrange("d g n -> (d g) n"), [], [b_nat1])
                    k.dma("sp", nat2[:, i, :], w[nm_].rearrange("d (q s) n -> (d q) (s n)", s=2), [], [b_nat2])
                k.dma("sp", ldb[:], w["log_dt"].rearrange("d g o -> o (d g)").partition_broadcast(128), [], [b_ldb])
                for i in range(2):
                    pt, b_pt = self.bank(i)
                    k.op("pe", "transpose", [b_nat1, self.b_cmf], [b_pt], pt[0:64, 0:64], nat1[:, i, :], idf[0:64, 0:64])
                    k.op("dve", "tensor_copy", [b_pt], [self._b_s5raw], out=raw1[:, i, :], in_=pt[0:64, 0:64])
                    pt, b_pt = self.bank(2 + i)
                    k.op("pe", "transpose", [b_nat2, self.b_cmf], [b_pt], pt[:, 0:32], nat2[:, i, :], idf[0:32, 0:32])
                    k.op("dve", "tensor_copy", [b_pt], [self._b_s5raw], out=raw2[:, i, :], in_=pt[:, 0:32])
                k.op("dve", "tensor_copy", [b_ldb], [self._b_s5raw], out=raw1[:, 2, :], in_=ldb[0:64, :])
                for gs in range(2):
                    k.op("dve", "tensor_copy", [b_ldb], [self._b_s5raw], out=raw2[gs * 64:(gs + 1) * 64, 2, :],
                         in_=ldb[gs * 64:(gs + 1) * 64, :].rearrange("p (q s) -> p q s", s=2)[:, :, gs])
                pr2 = self.s5_params(st1, raw2[:, 0, :], raw2[:, 1, :], raw2[:, 2, :], 128, 32, "b")
                for i, nm_ in enumerate(("r", "c1", "s1")):
                    k.op("dve", "tensor_copy", [pr2["buf"]], [b_sp2], out=sp2[:, i, :], in_=pr2[nm_])
                pr1 = self.s5_params(st1, raw1[:, 0, :], raw1[:, 1, :], raw1[:, 2, :], 64, 64, "a")
                braw, b_braw = self.tl(st1, "braw", [64, 2, 64, 16], F32)
                bb, b_bb = self.tl(st1, "bb", [64, 2, 64, 16], F32)
                btmp, b_btmp = self.tl(st1, "btmp", [64, 64, 16], F32)
                for i, nm_ in enumerate(("b_re", "b_im")):
                    for d_ in range(2):
                        for gh in range(2):
                            k.dma("sp", braw[:, i, d_ * 32 + gh * 16:d_ * 32 + gh * 16 + 16, :],
                                  w[nm_][d_, gh * 16:(gh + 1) * 16].rearrange("g n p -> n g p"), [], [b_braw])
                zr = pr1["zr"].unsqueeze(2).broadcast_to([64, 64, 16])
                zi = pr1["zi"].unsqueeze(2).broadcast_to([64, 64, 16])
                pb1 = pr1["buf"]
                k.op("dve", "tensor_tensor", [b_braw, pb1], [b_bb], out=bb[:, 0], in0=braw[:, 0], in1=zr, op=ALU.mult)
                k.op("dve", "tensor_tensor", [b_braw, pb1], [b_btmp], out=btmp[:], in0=braw[:, 1], in1=zi, op=ALU.mult)
                k.op("dve", "tensor_tensor", [b_bb, b_btmp], [b_bb], out=bb[:, 0], in0=bb[:, 0], in1=btmp[:],
                     op=ALU.subtract)
                k.op("dve", "tensor_tensor", [b_braw, pb1], [b_bb], out=bb[:, 1], in0=braw[:, 1], in1=zr, op=ALU.mult)
                k.op("dve", "tensor_tensor", [b_braw, pb1], [b_btmp], out=btmp[:], in0=braw[:, 0], in1=zi, op=ALU.mult)
                k.op("dve", "tensor_tensor", [b_bb, b_btmp], [b_bb], out=bb[:, 1], in0=bb[:, 1], in1=btmp[:],
                     op=ALU.add)
                m2b = self.mask2[:, 0:2].unsqueeze(2).broadcast_to([128, 2, 64])
                tcnt = 0
                for d in range(2):
                    for ut in range(4):
                        for c in range(2):
                            pt, b_pt = self.bank(tcnt % 4)
                            tcnt += 1
                            g0 = d * 32 + ut * 8
                            k.op("pe", "transpose", [b_bb, self.b_cmf], [b_pt], pt[:, 0:64],
                                 bb[:, c, g0:g0 + 8, :].rearrange("n g p -> n (g p)"), idf[0:64, 0:64])
                            k.op("dve", "tensor_tensor", [b_pt, self.b_mask2], [b_Bl],
                                 out=Bl[:, d, ut, c, :].rearrange("p (s n) -> p s n", n=64),
                                 in0=pt[:, 0:64].unsqueeze(1).broadcast_to([128, 2, 64]), in1=m2b, op=ALU.mult)
                craw, b_craw = self.tl(st1, "craw", [128, 2, 8, 64], F32)
                cm_, b_cm_ = self.tl(st1, "cmk", [128, 2, 64], F32)
                k.dma("sp", craw[:, 0], w["c_re"].rearrange("d (u g) p n -> (g p) (d u) n", g=8), [], [b_craw])
                k.dma("sp", craw[:, 1], w["c_im"].rearrange("d (u g) p n -> (g p) (d u) n", g=8), [], [b_craw])
                for d in range(2):
                    for ut in range(4):
                        for c in range(2):
                            pt, b_pt = self.bank(tcnt % 4)
                            tcnt += 1
                            k.op("dve", "scalar_tensor_tensor", [b_craw, self.b_mask2], [b_cm_], out=cm_[:],
                                 in0=craw[:, c, d * 4 + ut, :].unsqueeze(1).broadcast_to([128, 2, 64]),
                                 scalar=(1.0 if c == 0 else -1.0), in1=m2b, op0=ALU.mult, op1=ALU.mult)
                            k.op("pe", "transpose", [b_cm_, self.b_cmf], [b_pt], pt[:, 0:128],
                                 cm_[:].rearrange("p s n -> p (s n)"), idf)
                            k.op("act", "activation", [b_pt], [b_Cl], out=Cl[:, d, ut, c, :], in_=pt[:, 0:128],
                                 func=AF.Copy)
                k.op("dve", "tensor_scalar", [b_Bl, self.b_mask2], [b_Bl3], out=Bl3[:].rearrange("p a b c n -> p (a b c n)"),
                     in0=Bl[:].rearrange("p a b c n -> p (a b c n)"), scalar1=self.mask2[:, 2:3], scalar2=None, op0=ALU.mult)
                k.op("dve", "tensor_copy", [b_Cl], [b_Cl3], out=Cl3[:].rearrange("p a b c n -> p (a b c n)"),
                     in_=Cl[:].rearrange("p a b c n -> p (a b c n)"))
                k.op("dve", "memset", [], [b_Cl3], Cl3[:].rearrange("p a b c n -> p (a b c) n")[:, :, 64:96], 0.0)
                k.barrier()
            if self.stop == "S5prep":
                return
            uTs2 = [self.tl(st, "uTs0", [128, T], BF16)] * 2
            tabs = [(self.tl(st, f"Ct{i}", [128, T], F32), self.tl(st, f"St{i}", [128, T], F32)) for i in range(2)]
            itc = 0
            Gr, b_Gr = self.tl(st, "Gr", [128, T], F32)
            Gi, b_Gi = self.tl(st, "Gi", [128, T], F32)
            Hs = [[self.tl(st, f"H{d}{c}", [128, T], BF16) for c in range(2)] for d in range(2)]
            bss = [[self.tl(st, f"bs{i}{c}", [128, 512], F32) for c in range(2)] for i in range(2)]
            mAs = [self.tl(st, f"mA{i}", [128, 2, 512], F32) for i in range(2)]
            mBs = [self.tl(st, f"mB{i}", [128, 2, 512], F32) for i in range(2)]
            tA, b_tA = self.tl(st, "tA", [128, 1024], F32)
            tB, b_tB = self.tl(st, "tB", [128, 2, 1024], F32)
            sc2 = [self.tl(st, f"scal{i}", [128, 8], F32) for i in range(2)]
            yst = [self.tl(st, f"yst{i}", [128, 512], F32) for i in range(2)]
            ycnt = 0
            for ut in range(4):
                uTs, b_uTs = uTs2[ut % 2]
                k.dma("sp", uTs[:], self.uT[ut], [self.b_uT], [b_uTs])
                for jq in range(4):
                    r0 = 32 * jq
                    for d in range(2):
                        (Ct, b_Ct), (St, b_St) = tabs[itc % 2]
                        itc += 1
                        q = d * 16 + ut * 4 + jq
                        rcol = sp2[:, 0, q:q + 1]
                        if itc == 1:
                            pend = self._s5_table_thunks(k, sp2, q, Ct, b_Ct, St, b_St, sc2[0], tA, b_tA, tB, b_tB, b_sp2)
                            for th in pend:
                                th()
                        nxt_thunks = []
                        if itc < 64:
                            it_n = itc
                            ut_n, rem = divmod(it_n, 8)
                            jq_n, d_n = divmod(rem, 2)
                            q_n = d_n * 16 + ut_n * 4 + jq_n
                            (Ct_n, b_Ct_n), (St_n, b_St_n) = tabs[it_n % 2]
                            nxt_thunks = self._s5_table_thunks(k, sp2, q_n, Ct_n, b_Ct_n, St_n, b_St_n, sc2[it_n % 2],
                                                               tA, b_tA, tB, b_tB, b_sp2)
                        per_step = (len(nxt_thunks) + 17) // 18

                        def drain(nth=per_step):
                            for _ in range(nth):
                                if nxt_thunks:
                                    nxt_thunks.pop(0)()
                        def tab(tt_, t0, n):
                            if d == 0:
                                return tt_[:, t0:t0 + n]
                            if t0 < NCTX:
                                return tt_[:, 0:NCTX][:, ::-1]
                            lo = T + NCTX - t0 - n
                            return tt_[:, lo:lo + n][:, ::-1]
                        for ci, (t0, n) in enumerate(CHUNKS):
                            pr_, b_pr = self.bank(4 + 2 * (ci % 2))
                            pi_, b_pi = self.bank(5 + 2 * (ci % 2))
                            bsr, b_bsr = bss[ci % 2][0]
                            bsi, b_bsi = bss[ci % 2][1]
                            if jq < 3:
                                lr, li_ = Bl[r0:r0 + 32, d, ut, 0, :], Bl[r0:r0 + 32, d, ut, 1, :]
                                ur = uTs[r0:r0 + 32, t0:t0 + n]
                            else:
                                lr, li_ = Bl3[64:128, d, ut, 0, :], Bl3[64:128, d, ut, 1, :]
                                ur = uTs[64:128, t0:t0 + n]
                            k.op("pe", "matmul", [b_Bl, b_Bl3, b_uTs], [b_pr], pr_[:, :n], lhsT=lr, rhs=ur,
                                 start=True, stop=True)
                            k.op("pe", "matmul", [b_Bl, b_Bl3, b_uTs], [b_pi], pi_[:, :n], lhsT=li_, rhs=ur,
                                 start=True, stop=True)
                            k.op("act", "activation", [b_pr], [b_bsr], out=bsr[:, :n], in_=pr_[:, :n], func=AF.Copy)
                            k.op("act", "activation", [b_pi], [b_bsi], out=bsi[:, :n], in_=pi_[:, :n], func=AF.Copy)
                            cT, sT = tab(Ct, t0, n), tab(St, t0, n)
                            mA, b_mA = mAs[ci % 2]
                            mB, b_mB = mBs[ci % 2]
                            k.op("dve", "tensor_tensor", [b_bsr, b_Ct], [b_mA], out=mA[:, 0, :n], in0=bsr[:, :n], in1=cT,
                                 op=ALU.mult)
                            k.op("dve", "tensor_tensor", [b_bsi, b_St], [b_mA], out=mA[:, 1, :n], in0=bsi[:, :n], in1=sT,
                                 op=ALU.mult)
                            k.op("dve", "tensor_tensor", [b_mA], [b_Gr], out=Gr[:, t0:t0 + n], in0=mA[:, 0, :n],
                                 in1=mA[:, 1, :n], op=ALU.add)
                            k.op("dve", "tensor_tensor", [b_bsi, b_Ct], [b_mB], out=mB[:, 0, :n], in0=bsi[:, :n], in1=cT,
                                 op=ALU.mult)
                            k.op("pool", "tensor_tensor", [b_bsr, b_St], [b_mB], out=mB[:, 1, :n], in0=bsr[:, :n], in1=sT,
                                 op=ALU.mult)
                            k.op("pool", "tensor_tensor", [b_mB], [b_Gi], out=Gi[:, t0:t0 + n], in0=mB[:, 0, :n],
                                 in1=mB[:, 1, :n], op=ALU.subtract)
                            drain()
                        for G_, b_G in ((Gr, b_Gr), (Gi, b_Gi)):
                            if d == 0:
                                segs = [(G_[:, 0:NCTX], 0.0), (G_[:, NCTX:T], G_[:, NCTX - 1:NCTX])]
                            else:
                                segs = [(G_[:, 0:NCTX][:, ::-1], 0.0), (G_[:, NCTX:T][:, ::-1], G_[:, 0:1])]
                            for seg, init in segs:
                                nn = seg.shape[1]
                                k.op("dve", "tensor_tensor_scan", [b_G, b_sp2], [b_G], out=seg,
                                     data0=rcol.broadcast_to([128, nn]), data1=seg, initial=init,
                                     op0=ALU.mult, op1=ALU.add)
                        Hr, b_Hr = Hs[d][0]
                        Hi, b_Hi = Hs[d][1]
                        for ci, (t0, n) in enumerate(CHUNKS):
                            cT, sT = tab(Ct, t0, n), tab(St, t0, n)
                            mA, b_mA = mAs[ci % 2]
                            mB, b_mB = mBs[ci % 2]
                            k.op("dve", "tensor_tensor", [b_Gr, b_Ct], [b_mA], out=mA[:, 0, :n], in0=Gr[:, t0:t0 + n],
                                 in1=cT, op=ALU.mult)
                            k.op("dve", "tensor_tensor", [b_Gi, b_St], [b_mA], out=mA[:, 1, :n], in0=Gi[:, t0:t0 + n],
                                 in1=sT, op=ALU.mult)
                            k.op("dve", "tensor_tensor", [b_mA], [b_Hr], out=Hr[:, t0:t0 + n], in0=mA[:, 0, :n],
                                 in1=mA[:, 1, :n], op=ALU.subtract)
                            k.op("dve", "tensor_tensor", [b_Gr, b_St], [b_mB], out=mB[:, 0, :n], in0=Gr[:, t0:t0 + n],
                                 in1=sT, op=ALU.mult)
                            k.op("pool", "tensor_tensor", [b_Gi, b_Ct], [b_mB], out=mB[:, 1, :n], in0=Gi[:, t0:t0 + n],
                                 in1=cT, op=ALU.mult)
                            k.op("pool", "tensor_tensor", [b_mB], [b_Hi], out=Hi[:, t0:t0 + n], in0=mB[:, 0, :n],
                                 in1=mB[:, 1, :n], op=ALU.add)
                            drain()
                        drain(10 ** 6)
                    for ci, (t0, n) in enumerate(CHUNKS):
                        py, b_py = self.bank(ci % 2)
                        i4 = 0
                        for d in range(2):
                            for c in range(2):
                                Hc, b_Hc = Hs[d][c]
                                if jq < 3:
                                    k.op("pe", "matmul", [b_Cl, b_Hc], [b_py], py[r0:r0 + 32, :n],
                                         lhsT=Cl[:, d, ut, c, r0:r0 + 32], rhs=Hc[:, t0:t0 + n],
                                         start=(i4 == 0), stop=(i4 == 3))
                                else:
                                    k.op("pe", "matmul", [b_Cl3, b_Hc], [b_py], py[64:128, :n],
                                         lhsT=Cl3[:, d, ut, c, 64:128], rhs=Hc[:, t0:t0 + n],
                                         start=(i4 == 0), stop=(i4 == 3))
                                i4 += 1
                        ys, b_ys = yst[ycnt % 2]
                        ycnt += 1
                        c0_ = r0 if jq < 3 else 64
                        k.op("act", "activation", [b_py], [b_ys], out=ys[c0_:r0 + 32, :n], in_=py[c0_:r0 + 32, :n],
                             func=AF.Copy)
                        k.dma("sp", self.yT[ut, r0:r0 + 32, t0:t0 + n], ys[r0:r0 + 32, :n], [b_ys], [self.b_yT])
            k.barrier()


def layer_inputs(inp, l):
    f = np.ascontiguousarray
    d = {}
    d[f"w_ada{l}"] = f(inp["w_ada"][l])
    d[f"b_ada{l}"] = f(inp["b_ada"][l][None, :])
    d[f"n1{l}"] = f(inp["norm1_g"][l][None, :])
    d[f"n2{l}"] = f(inp["norm2_g"][l][None, :])
    d[f"w_r{l}"] = f(np.concatenate([inp["moe_w_r1"][l], inp["moe_w_r2"][l]], axis=1))
    d[f"b_r{l}"] = f(np.concatenate([inp["moe_b_r1"][l], inp["moe_b_r2"][l]])[None, :])
    d[f"w_gate{l}"] = f(inp["moe_w_gate"][l].reshape(NEXP, 8, 128, 512).transpose(0, 2, 1, 3)).reshape(NEXP * 128, 4096)
    d[f"w_up{l}"] = f(inp["moe_w_up"][l].reshape(NEXP, 8, 128, 512).transpose(0, 2, 1, 3)).reshape(NEXP * 128, 4096)
    d[f"w_down{l}"] = f(inp["moe_w_down"][l].reshape(NEXP, 4, 128, D).transpose(0, 2, 1, 3)).reshape(NEXP * 128, 4096)
    i = l // 2
    if l % 2 == 1:
        d[f"w_out{l}"] = f(inp["w_out_o"][i])
        d[f"w_qkv{l}"] = f(inp["w_qkv_o"][i])
        d[f"qn{l}"] = f(inp["gqa_qn"][i][:, None])
        d[f"kn{l}"] = f(inp["gqa_kn"][i][:, None])
    else:
        d[f"w_out{l}"] = f(inp["w_out_e"][i])
        d[f"w_in{l}"] = f(inp["w_in_e"][i])
        d[f"a_re{l}"] = f(inp["s5_a_re"][i])
        d[f"a_im{l}"] = f(inp["s5_a_im"][i])
        d[f"log_dt{l}"] = f(inp["s5_log_dt"][i][:, :, None])
        d[f"b_re{l}"] = f(inp["s5_b_re"][i])
        d[f"b_im{l}"] = f(inp["s5_b_im"][i])
        d[f"c_re{l}"] = f(inp["s5_c_re"][i])
        d[f"c_im{l}"] = f(inp["s5_c_im"][i])
        d[f"s5_d{l}"] = f(inp["s5_d"][i][:, None])
        d[f"w_glu{l}"] = f(inp["s5_w_glu"][i])
        d[f"gq{l}"] = f(inp["mla_gq"][i][:, None])
        d[f"w_uq{l}"] = f(inp["mla_w_uq"][i])
        d[f"gkv{l}"] = f(inp["mla_gkv"][i][:, None])
        d[f"w_ukv{l}"] = f(inp["mla_w_ukv"][i])
        d[f"mqn{l}"] = f(inp["mla_qn"][i][:, None])
        d[f"mkn{l}"] = f(inp["mla_kn"][i][:, None])
    return d


def core_inputs(inp, b, shared):
    d = dict(shared)
    d["x"] = np.ascontiguousarray(inp["x"][b])
    d["ctx"] = np.ascontiguousarray(inp["ctx"][b])
    d["cvec"] = np.ascontiguousarray(np.stack([inp["c"][b], inp["c_ctx"]], axis=0))
    return d


def run_layers(inp, layers, batches, stop=None, trace=False):
    inp = {k_: np.asarray(v, dtype=np.float32) for k_, v in inp.items()}
    prog = Prog(layers, stop)
    nc = prog.build()
    shared = make_consts()
    for l in layers:
        shared.update(layer_inputs(inp, l))
    in_maps = []
    for b in batches:
        m = core_inputs(inp, b, shared)
        in_maps.append({name: m[name] for name in prog.din})
    if trace:
        res = run_bass_kernel_spmd(nc, in_maps, core_ids=list(range(len(batches))), trace=True)
        print("EXEC_TIME_NS", layers, stop, res.exec_time_ns)
    else:
        res = run_bass_kernel_spmd(nc, in_maps, core_ids=list(range(len(batches))))
    if stop:
        return [r for r in res.results]
    return [r["y"] for r in res.results]


def kernel(**inputs):
    outs = run_layers(inputs, [0, 1, 2, 3], [0, 1, 2, 3])
    return np.stack(outs, axis=0).astype(np.float32)
```

```python
import math
import numpy as np
import ml_dtypes
import concourse.bass as bass
import concourse.mybir as mybir
from concourse.bass_utils import run_bass_kernel_spmd

F32 = mybir.dt.float32
BF16 = mybir.dt.bfloat16
I32 = mybir.dt.int32
U32 = mybir.dt.uint32
AF = mybir.ActivationFunctionType
ALU = mybir.AluOpType
AX = mybir.AxisListType

D = 1024
NCTX = 256
NLAT = 4096
T = NCTX + NLAT
NT = T // 128
CHUNKS = [(0, 256)] + [(256 + 512 * i, 512) for i in range(8)]
EPS = 1e-6
NEXP = 32
CAP = 512
NSLOT = NEXP * CAP
EPOCH = 20000


class Buf:
    __slots__ = ("name", "lw", "rd", "dsem", "dcnt", "dbase", "dq")

    def __init__(self, name):
        self.name = name
        self.lw = None
        self.rd = {}
        self.dsem = None
        self.dcnt = 0
        self.dbase = 0
        self.dq = None


class K:
    def __init__(self, nc):
        self.nc = nc
        self.E = {"pe": nc.tensor, "act": nc.scalar, "dve": nc.vector, "pool": nc.gpsimd, "sp": nc.sync}
        self.sems = {e: [] for e in self.E}
        self.cnt = {e: 0 for e in self.E}
        self.seen = {e: {} for e in self.E}
        self.bufs = []
        self.ninstr = 0
        self.sempool = {"sp": [], "pool": []}
        self.nsem = 0
        self.keep = set()
        self.pool_q = []

    def buf(self, name):
        b = Buf(name)
        self.bufs.append(b)
        return b

    def _csem(self, e, n):
        idx = (n - 1) // EPOCH
        while len(self.sems[e]) <= idx:
            self.sems[e].append(self.nc.alloc_semaphore(f"c_{e}_{len(self.sems[e])}"))
        return self.sems[e][idx], (n - 1) % EPOCH + 1

    def _wait(self, e, tok):
        if tok is None:
            return
        if tok[0] == "c":
            _, e2, n = tok
            if e2 == e and e in ("pe", "sp"):
                return
            if self.seen[e].get(e2, 0) >= n:
                return
            s, v = self._csem(e2, n)
            self.E[e].wait_ge(s, v)
            self.ninstr += 1
            self.seen[e][e2] = n
        else:
            B = tok[1]
            if B.dsem is None:
                return
            tgt = B.dbase + B.dcnt * 16
            assert tgt < 65000, B.name
            key = ("d", id(B.dsem))
            if self.seen[e].get(key, 0) >= tgt:
                return
            self.E[e].wait_ge(B.dsem, tgt)
            self.ninstr += 1
            self.seen[e][key] = tgt

    def _deps(self, e, reads, writes):
        for b in reads:
            self._wait(e, b.lw)
        for b in writes:
            self._wait(e, b.lw)
            for tok in list(b.rd.values()):
                self._wait(e, tok)

    def op(self, e, meth, reads, writes, *args, **kw):
        self._deps(e, reads, writes)
        ins = getattr(self.E[e], meth)(*args, **kw)
        self.cnt[e] += 1
        n = self.cnt[e]
        s, _ = self._csem(e, n)
        ins.then_inc(s, 1)
        self.ninstr += 1
        tok = ("c", e, n)
        for b in reads:
            b.rd[e] = tok
        for b in writes:
            b.lw = tok
            b.rd = {}
        return ins

    def dma(self, q, out, in_, reads, writes, indirect=None, **kw):
        self._deps(q, reads, writes)
        B = writes[0]
        if B.dsem is not None:
            assert B.dq == q, (B.name, B.dq, q)
        if B.dsem is None:
            B.dq = q
            if self.sempool[q]:
                B.dsem, B.dbase = self.sempool[q].pop()
            else:
                self.nsem += 1
                B.dsem, B.dbase = self.nc.alloc_semaphore(f"d{self.nsem}"), 0
            B.dcnt = 0
        B.dcnt += 1
        if indirect is None:
            ins = self.E[q].dma_start(out=out, in_=in_, **kw)
        else:
            ins = indirect(self.E[q])
        ins.then_inc(B.dsem, 16)
        self.ninstr += 1
        if q == "pool":
            self.pool_q.append((B.dsem, B.dbase + B.dcnt * 16))
            if len(self.pool_q) > 24:
                s_, v_ = self.pool_q.pop(0)
                key_ = ("d", id(s_))
                if self.seen[q].get(key_, 0) < v_:
                    self.E[q].wait_ge(s_, v_)
                    self.ninstr += 1
                    self.seen[q][key_] = v_
        tok = ("d", B)
        for b in reads:
            b.rd[("d", id(B))] = tok
        for b in writes:
            b.lw = tok
            b.rd = {}

    def barrier(self):
        for e in self.E:
            for e2 in self.E:
                if e2 != e and self.cnt[e2] > 0:
                    self._wait(e, ("c", e2, self.cnt[e2]))
            for b in self.bufs:
                if b.dsem is not None and b.dcnt > 0:
                    self._wait(e, ("d", b))
        self.pool_q = []
        for b in self.bufs:
            b.lw = None
            b.rd = {}
            if b.dsem is not None:
                v = b.dbase + b.dcnt * 16
                if v < 40000:
                    self.sempool[b.dq].append((b.dsem, v))
                b.dsem = None
                b.dcnt = 0


def _rope_tab(d_rot, n_freq):
    rows = NLAT // 64
    row = np.repeat(np.arange(rows, dtype=np.float32), 64)
    col = np.tile(np.arange(64, dtype=np.float32), rows)
    inv = (10000.0 ** (-np.arange(n_freq, dtype=np.float32) / n_freq)).astype(np.float32)
    ang = np.stack([row[:, None] * inv, col[:, None] * inv], axis=1).astype(np.float32)
    cos = np.cos(ang)
    sin = np.sin(ang)
    c = np.zeros((d_rot, T), np.float32)
    s = np.zeros((d_rot, T), np.float32)
    c[:, :NCTX] = 1.0
    for ax in range(2):
        for half in range(2):
            for i in range(n_freq):
                d = ax * 2 * n_freq + half * n_freq + i
                c[d, NCTX:] = cos[:, ax, i]
                s[d, NCTX:] = sin[:, ax, i]
    return c, s


def _rot_mat(d_total, off, n_freq):
    Rm = np.zeros((128, 128), np.float32)
    for ax in range(2):
        for i in range(n_freq):
            first = off + ax * 2 * n_freq + i
            second = first + n_freq
            Rm[second, first] = -1.0
            Rm[first, second] = 1.0
    return Rm


def make_consts():
    c = {}
    m = np.zeros((6, 128, 128), np.float32)
    m[0] = np.eye(128)
    m[1] = 1.0
    m[2] = _rot_mat(128, 0, 32)
    m[3] = _rot_mat(96, 64, 8)
    m[4] = np.triu(np.ones((128, 128), np.float32), 1)
    c["cmat"] = np.ascontiguousarray(m.transpose(1, 0, 2))
    gc, gs = _rope_tab(128, 32)
    c["rope_g"] = np.stack([gc, gs], axis=1)
    mc = np.zeros((128, T), np.float32)
    ms = np.zeros((128, T), np.float32)
    mc[:64] = 1.0
    rc, rs = _rope_tab(32, 8)
    mc[64:96] = rc
    ms[64:96] = rs
    c["rope_m"] = np.stack([mc, ms], axis=1)
    ec = np.zeros((128, 32), np.float32)
    ec[:] = np.arange(32, dtype=np.float32) * CAP
    c["ecap"] = ec
    m2 = np.zeros((128, 3), np.float32)
    for p in range(128):
        m2[p, (p // 16) % 2] = 1.0
    m2[96:, 2] = 1.0
    c["mask2"] = m2
    return c


PASSES = [[(0, 256), (256, 512), (768, 512), (1280, 512), (1792, 384)],
          [(2176, 512), (2688, 512), (3200, 512), (3712, 512), (4224, 128)]]
PASS_T = 2176
PASS_NT = 17


class Prog:
    def __init__(self, layers, stop=None):
        self.layers = layers
        self.stop = stop
        self.nc = bass.Bass("TRN2", target_bir_lowering=False)
        self.k = K(self.nc)
        self.din = {}
        self._ncnt = 0

    def inp(self, name, shape, dtype=F32):
        t = self.nc.dram_tensor(name, list(shape), dtype, kind="ExternalInput")
        self.din[name] = tuple(shape)
        return t.ap()

    def scratch(self, name, shape, dtype):
        return self.nc.dram_tensor(name, list(shape), dtype, kind="Internal").ap()

    def nm(self, s):
        self._ncnt += 1
        return f"{s}_{self._ncnt}"

    def sbp(self, name, shape, dtype):
        t = self.nc.alloc_sbuf_tensor(self.nm(name), list(shape), dtype)
        return t, self.k.buf(name)

    def tl(self, stack, name, shape, dtype):
        t = stack.enter_context(self.nc.sbuf_tensor(self.nm(name), list(shape), dtype))
        return t, self.k.buf(name)

    def bank(self, i):
        t = self.pst[i // 2]
        return t[:, (i % 2) * 512:(i % 2 + 1) * 512], self.pb[i]

    def build(self):
        nc, k = self.nc, self.k
        L = self.layers
        x_in = self.inp("x", [NLAT, D])
        ctx_in = self.inp("ctx", [NCTX, D])
        self._cvec = self.inp("cvec", [2, D])
        cmat_d = self.inp("cmat", [128, 6, 128])
        self.rope_g_d = self.inp("rope_g", [128, 2, T])
        self.rope_m_d = self.inp("rope_m", [128, 2, T])
        mask2_d = self.inp("mask2", [128, 3])
        W = {}
        for l in L:
            W[l] = {}
            for nm_, shp in [("w_ada", [D, 6 * D]), ("b_ada", [1, 6 * D]), ("n1", [1, D]), ("n2", [1, D]),
                             ("w_r", [D, 36]), ("b_r", [1, 36]), ("w_out", [D, D])]:
                W[l][nm_] = self.inp(f"{nm_}{l}", shp)
            self.Wt = getattr(self, "Wt", {})
            self.Wt[l] = {}
            for nm_, shp in [("w_gate", [NEXP * 128, 4096]), ("w_up", [NEXP * 128, 4096]), ("w_down", [NEXP * 128, 4096])]:
                t_ = self.nc.dram_tensor(f"{nm_}{l}", shp, F32, kind="ExternalInput")
                self.din[f"{nm_}{l}"] = tuple(shp)
                self.Wt[l][nm_] = t_
            if l % 2 == 1:
                for nm_, shp in [("w_qkv", [D, 2048]), ("qn", [128, 1]), ("kn", [128, 1])]:
                    W[l][nm_] = self.inp(f"{nm_}{l}", shp)
            else:
                for nm_, shp in [("w_in", [D, 928]), ("a_re", [2, 32, 64]), ("a_im", [2, 32, 64]),
                                 ("log_dt", [2, 32, 1]), ("b_re", [2, 32, 64, 16]), ("b_im", [2, 32, 64, 16]),
                                 ("c_re", [2, 32, 16, 64]), ("c_im", [2, 32, 16, 64]),
                                 ("s5_d", [512, 1]), ("w_glu", [512, 512]), ("gq", [256, 1]),
                                 ("w_uq", [256, 768]), ("gkv", [128, 1]), ("w_ukv", [128, 1024]),
                                 ("mqn", [96, 1]), ("mkn", [96, 1])]:
                    W[l][nm_] = self.inp(f"{nm_}{l}", shp)
        self.W = W
        self.y_out = nc.dram_tensor("y", [NLAT, D], F32, kind="ExternalOutput").ap()
        self.xT = self.scratch("xT", [8, 128, T], F32)
        self.b_xT = k.buf("xT")
        self.qT = self.scratch("qT", [8, 128, T], BF16)
        self.b_qT = k.buf("qT")
        self.kT = self.scratch("kT", [8, 128, T], BF16)
        self.b_kT = k.buf("kT")
        self.vS = self.scratch("vS", [T, 512], BF16)
        self.b_vS = k.buf("vS")
        self.mixT = self.scratch("mixT", [8, 128, T], BF16)
        self.b_mixT = k.buf("mixT")
        self.b_y = k.buf("y")
        self.cm_f, self.b_cmf = self.sbp("cm_f", [128, 6, 128], F32)
        self.cm_b, self.b_cmb = self.sbp("cm_b", [128, 6, 128], BF16)
        k.dma("sp", self.cm_f[:], cmat_d, [], [self.b_cmf])
        k.dma("pool", self.cm_b[:], cmat_d, [], [self.b_cmb])
        self.mask2, self.b_mask2 = self.sbp("mask2", [128, 3], F32)
        k.dma("sp", self.mask2[:], mask2_d, [], [self.b_mask2])
        nl = len(L)
        self.mT, self.b_mT = self.sbp("mT", [128, nl, 48, 2], F32)
        self.gsc, self.b_gsc = self.sbp("gsc", [128, nl, 2, 8, 2], F32)
        self.pst = [nc.alloc_psum_tensor(self.nm("ps"), [128, 1024], F32) for _ in range(4)]
        self.pb = [k.buf(f"psb{i}") for i in range(8)]

        self.phase_adaln()
        self.phase_load_x(x_in, ctx_in)
        for li, l in enumerate(L):
            if l % 2 == 1:
                self.phase_odd_proj(li, l)
                if self.stop == "O1":
                    continue
                self.phase_attention(8, 4, lambda h: h // 2, 128, 128, 128 ** -0.5,
                                     lambda h: (h, 0))
            else:
                self.phase_even(li, l)
            if self.stop and self.stop != "OUT":
                continue
            self.phase_outproj(li, l)
            if self.stop == "OUT":
                continue
            self.phase_moe(li, l)
        self.phase_store_y()
        return nc

    def phase_adaln(self):
        nc, k = self.nc, self.k
        from contextlib import ExitStack
        with ExitStack() as st:
            scT, b_scT = self.tl(st, "scT", [128, 2, 8], F32)
            was = [self.tl(st, f"wa{i}", [128, 3072], F32) for i in range(2)]
            mrow, b_mrow = self.tl(st, "mrow", [2, 6144], F32)
            brow, b_brow = self.tl(st, "brow", [2, 6144], F32)
            ngT, b_ngT = self.tl(st, "ngT", [128, 2, 8], F32)
            idf = self.cm_f[:, 0, :]
            for j in range(2):
                k.dma("sp", scT[:, j, :], self._cvec[j:j + 1, :].rearrange("o (k p) -> p (o k)", p=128), [], [b_scT],
                      allow_slow_non_contiguous=True)
            k.op("act", "activation", [b_scT], [b_scT], out=scT[:], in_=scT[:], func=AF.Silu)
            cnt = 0
            for li, l in enumerate(self.layers):
                w = self.W[l]
                k.dma("sp", brow[:], w["b_ada"][0:1, :].partition_broadcast(2), [], [b_brow])
                for half in range(2):
                    for kt in range(8):
                        wa, b_wa = was[cnt % 2]
                        cnt += 1
                        k.dma("sp", wa[:], w["w_ada"][kt * 128:(kt + 1) * 128, half * 3072:(half + 1) * 3072],
                              [], [b_wa])
                        for c6 in range(6):
                            pt, b_pt = self.bank(c6)
                            k.op("pe", "matmul", [b_scT, b_wa], [b_pt], pt[0:2, :], lhsT=scT[:, :, kt],
                                 rhs=wa[:, c6 * 512:(c6 + 1) * 512], start=(kt == 0), stop=(kt == 7))
                    for c6 in range(6):
                        pt, b_pt = self.bank(c6)
                        col = half * 3072 + c6 * 512
                        k.op("dve", "tensor_tensor", [b_pt, b_brow], [b_mrow], out=mrow[:, col:col + 512],
                             in0=pt[0:2, :], in1=brow[:, col:col + 512], op=ALU.add)
                pt, b_pt = self.bank(6)
                for ft in range(48):
                    k.op("pe", "transpose", [b_mrow, self.b_cmf], [b_pt], pt[:, ft * 2:ft * 2 + 2],
                         mrow[:, ft * 128:(ft + 1) * 128], idf[0:2, 0:2])
                k.op("dve", "tensor_copy", [b_pt], [self.b_mT], out=self.mT[:, li, :, :],
                     in_=pt[:, 0:96].rearrange("p (f j) -> p f j", j=2))
                k.dma("sp", ngT[:, 0, :], w["n1"].rearrange("o (k p) -> p (o k)", p=128), [], [b_ngT],
                      allow_slow_non_contiguous=True)
                k.dma("sp", ngT[:, 1, :], w["n2"].rearrange("o (k p) -> p (o k)", p=128), [], [b_ngT],
                      allow_slow_non_contiguous=True)
                for which, scc in ((0, 1), (1, 4)):
                    for j in range(2):
                        k.op("dve", "scalar_tensor_tensor", [self.b_mT, b_ngT], [self.b_gsc],
                             out=self.gsc[:, li, which, :, j], in0=self.mT[:, li, scc * 8:(scc + 1) * 8, j],
                             scalar=1.0, in1=ngT[:, which, :], op0=ALU.add, op1=ALU.mult)
            k.barrier()

    def phase_load_x(self, x_in, ctx_in):
        nc, k = self.nc, self.k
        from contextlib import ExitStack
        idf = self.cm_f[:, 0, :]
        with ExitStack() as st:
            xas = [self.tl(st, f"xa{i}", [128, D], F32) for i in range(2)]
            xos = [self.tl(st, f"xo{i}", [128, 8, 128], F32) for i in range(2)]
            for ti in range(NT):
                src = ctx_in[ti * 128:(ti + 1) * 128, :] if ti < 2 else x_in[(ti - 2) * 128:(ti - 1) * 128, :]
                xa, b_xa = xas[ti % 2]
                xo, b_xo = xos[ti % 2]
                k.dma("sp", xa[:], src, [], [b_xa])
                for half in range(2):
                    pt, b_pt = self.bank((ti % 2) * 2 + half)
                    for kk in range(4):
                        kt = half * 4 + kk
                        k.op("pe", "transpose", [b_xa, self.b_cmf], [b_pt], pt[:, kk * 128:(kk + 1) * 128],
                             xa[:, kt * 128:(kt + 1) * 128], idf)
                    src_v = pt.rearrange("p (a b) -> p a b", b=128)
                    if half == 0:
                        k.op("act", "activation", [b_pt], [b_xo], out=xo[:, 0:4, :], in_=src_v, func=AF.Copy)
                    else:
                        k.op("dve", "tensor_copy", [b_pt], [b_xo], out=xo[:, 4:8, :], in_=src_v)
                k.dma("sp", self.xT[:, :, ti * 128:(ti + 1) * 128].rearrange("k p t -> p k t"), xo[:],
                      [b_xo], [self.b_xT])
            k.barrier()

    def phase_store_y(self):
        nc, k = self.nc, self.k
        from contextlib import ExitStack
        idf = self.cm_f[:, 0, :]
        with ExitStack() as st:
            xas = [self.tl(st, f"ya{i}", [128, 8, 128], F32) for i in range(2)]
            xos = [self.tl(st, f"yo{i}", [128, D], F32) for i in range(2)]
            for ti in range(2, NT):
                xa, b_xa = xas[ti % 2]
                xo, b_xo = xos[ti % 2]
                k.dma("sp", xa[:], self.xT[:, :, ti * 128:(ti + 1) * 128].rearrange("k p t -> p k t"),
                      [self.b_xT], [b_xa])
                for half in range(2):
                    pt, b_pt = self.bank((ti % 2) * 2 + half)
                    for kk in range(4):
                        kt = half * 4 + kk
                        k.op("pe", "transpose", [b_xa, self.b_cmf], [b_pt], pt[:, kk * 128:(kk + 1) * 128],
                             xa[:, kt, :], idf)
                    if half == 0:
                        k.op("act", "activation", [b_pt], [b_xo], out=xo[:, 0:512], in_=pt, func=AF.Copy)
                    else:
                        k.op("dve", "tensor_copy", [b_pt], [b_xo], out=xo[:, 512:1024], in_=pt)
                k.dma("sp", self.y_out[(ti - 2) * 128:(ti - 1) * 128, :], xo[:], [b_xo], [self.b_y])
            if self.stop:
                self.dbg_names = []
                for nm_, shp, dt_ in (("uT", [4, 128, T], BF16), ("qT", [8, 128, T], BF16), ("kT", [8, 128, T], BF16),
                                      ("vS", [T, 512], BF16), ("yT", [4, 128, T], F32), ("mixT", [8, 128, T], BF16)):
                    if hasattr(self, nm_):
                        o = nc.dram_tensor("dbg_" + nm_, shp, dt_, kind="ExternalOutput").ap()
                        b_o = k.buf("dbg_" + nm_)
                        k.dma("sp", o, getattr(self, nm_), [getattr(self, "b_" + nm_)], [b_o])
                        self.dbg_names.append("dbg_" + nm_)
            k.barrier()

    def norm_mod(self, xc, b_xc, n, li, which, j, out_fn, b_out, sq, b_sq, rs, b_rs, tmp, b_tmp, psb):
        k = self.k
        ones_b = self.cm_b[:, 1, :]
        pt, b_pt = self.bank(psb)
        k.op("act", "activation", [b_xc], [b_sq], out=sq[:, :, :n], in_=xc[:, :, :n], func=AF.Square)
        for kt in range(8):
            k.op("pe", "matmul", [b_sq, self.b_cmb], [b_pt], pt[:, :n], lhsT=ones_b, rhs=sq[:, kt, :n],
                 start=(kt == 0), stop=(kt == 7))
        k.op("act", "activation", [b_pt], [b_rs], out=rs[:, :n], in_=pt[:, :n], func=AF.Sqrt,
             scale=1.0 / D, bias=EPS)
        k.op("dve", "reciprocal", [b_rs], [b_rs], out=rs[:, :n], in_=rs[:, :n])
        k.op("dve", "tensor_tensor", [b_xc, b_rs], [b_tmp], out=tmp[:, :, :n], in0=xc[:, :, :n],
             in1=rs[:, :n].unsqueeze(1).broadcast_to([128, 8, n]), op=ALU.mult)
        shc = 0 if which == 0 else 3
        for kt in range(8):
            k.op("dve" if kt % 2 == 0 else "pool", "tensor_scalar", [b_tmp, self.b_gsc, self.b_mT], [b_out],
                 out=out_fn(kt), in0=tmp[:, kt, :n], scalar1=self.gsc[:, li, which, kt, j:j + 1],
                 scalar2=self.mT[:, li, shc * 8 + kt, j:j + 1], op0=ALU.mult, op1=ALU.add)

    def qk_norm_rope(self, ps, b_ps, Dh, n, gain, b_gain, cos, sin, b_rope, Rm, out, b_out, wk, pss, prot):
        k = self.k
        sqh, b_sqh, rsh, b_rsh, xg, b_xg, t1, b_t1, t2, b_t2 = wk[self._wkc % 2]
        if self._wkc % 2 == 1:
            pss = 7
        self._wkc += 1
        ones_b = self.cm_b[0:Dh, 1, 0:Dh]
        p2, b_p2 = self.bank(pss)
        p3, b_p3 = self.bank(prot)
        k.op("act", "activation", [b_ps], [b_sqh], out=sqh[0:Dh, :n], in_=ps[0:Dh, :n], func=AF.Square)
        k.op("pe", "matmul", [b_sqh, self.b_cmb], [b_p2], p2[0:Dh, :n], lhsT=ones_b, rhs=sqh[0:Dh, :n],
             start=True, stop=True)
        k.op("act", "activation", [b_p2], [b_rsh], out=rsh[0:Dh, :n], in_=p2[0:Dh, :n], func=AF.Sqrt,
             scale=1.0 / Dh, bias=EPS)
        k.op("dve", "reciprocal", [b_rsh], [b_rsh], out=rsh[0:Dh, :n], in_=rsh[0:Dh, :n])
        k.op("dve", "scalar_tensor_tensor", [b_ps, b_gain, b_rsh], [b_xg], out=xg[0:Dh, :n], in0=ps[0:Dh, :n],
             scalar=gain, in1=rsh[0:Dh, :n], op0=ALU.mult, op1=ALU.mult)
        k.op("pe", "matmul", [b_xg, self.b_cmb], [b_p3], p3[0:Dh, :n], lhsT=Rm, rhs=xg[0:Dh, :n],
             start=True, stop=True)
        k.op("pool", "tensor_tensor", [b_xg, b_rope], [b_t1], out=t1[0:Dh, :n], in0=xg[0:Dh, :n], in1=cos,
             op=ALU.mult)
        k.op("dve", "tensor_tensor", [b_p3, b_rope], [b_t2], out=t2[0:Dh, :n], in0=p3[0:Dh, :n], in1=sin,
             op=ALU.mult)
        k.op("pool", "tensor_tensor", [b_t1, b_t2], [b_out], out=out, in0=t1[0:Dh, :n], in1=t2[0:Dh, :n],
             op=ALU.add)

    def qk_work(self, st):
        sets = []
        for i in range(2):
            wk = []
            for nm_, dt_ in (("sqh", BF16), ("rsh", F32), ("xg", BF16), ("t1", F32), ("t2", F32)):
                t, b = self.tl(st, f"{nm_}{i}", [128, 512], dt_)
                wk += [t, b]
            sets.append(wk)
        self._wkc = 0
        return sets

    def phase_odd_proj(self, li, l):
        nc, k = self.nc, self.k
        from contextlib import ExitStack
        w = self.W[l]
        with ExitStack() as st:
            wq, b_wq = self.tl(st, "wq", [128, 8, 2048], BF16)
            rope, b_rope = self.tl(st, "rope", [128, 2, T], F32)
            xcs = [self.tl(st, f"xc{i}", [128, 8, 512], F32) for i in range(2)]
            hTs = [self.tl(st, f"hT{i}", [128, 8, 512], BF16) for i in range(2)]
            sq, b_sq = self.tl(st, "sq", [128, 8, 512], BF16)
            tmp, b_tmp = self.tl(st, "tmp", [128, 8, 512], F32)
            rs, b_rs = self.tl(st, "rs", [128, 512], F32)
            gn, b_gn = self.tl(st, "gn", [128, 2], F32)
            qos = [self.tl(st, f"qo{i}", [128, 512], BF16) for i in range(2)]
            vts = [self.tl(st, f"vt{i}", [128, 512], BF16) for i in range(2)]
            wk = self.qk_work(st)
            for kt in range(8):
                k.dma("pool", wq[:, kt, :], w["w_qkv"][kt * 128:(kt + 1) * 128, :], [], [b_wq])
            k.dma("sp", rope[:], self.rope_g_d, [], [b_rope])
            k.dma("sp", gn[:, 0:1], w["qn"], [], [b_gn])
            k.dma("sp", gn[:, 1:2], w["kn"], [], [b_gn])
            Rm = self.cm_b[:, 2, :]
            cnt = 0
            for ci, (t0, n) in enumerate(CHUNKS):
                j = 1 if ci == 0 else 0
                xc, b_xc = xcs[ci % 2]
                hT, b_hT = hTs[ci % 2]
                k.dma("sp", xc[:, :, :n], self.xT[:, :, t0:t0 + n].rearrange("k p t -> p k t"), [self.b_xT], [b_xc])
                self.norm_mod(xc, b_xc, n, li, 0, j, lambda kt: hT[:, kt, :n], b_hT, sq, b_sq, rs, b_rs,
                              tmp, b_tmp, 0)
                for hh in range(12):
                    pt, b_pt = self.bank(1 + hh % 2)
                    for kt in range(8):
                        k.op("pe", "matmul", [b_wq, b_hT], [b_pt], pt[:, :n],
                             lhsT=wq[:, kt, hh * 128:(hh + 1) * 128], rhs=hT[:, kt, :n],
                             start=(kt == 0), stop=(kt == 7))
                    qo, b_qo = qos[cnt % 2]
                    cnt += 1
                    gi = 0 if hh < 8 else 1
                    self.qk_norm_rope(pt, b_pt, 128, n, gn[:, gi:gi + 1], b_gn, rope[:, 0, t0:t0 + n],
                                      rope[:, 1, t0:t0 + n], b_rope, Rm, qo[:, :n], b_qo, wk, 3, 4)
                    if hh < 8:
                        k.dma("sp", self.qT[hh, :, t0:t0 + n], qo[:, :n], [b_qo], [self.b_qT])
                    else:
                        k.dma("sp", self.kT[hh - 8, :, t0:t0 + n], qo[:, :n], [b_qo], [self.b_kT])
                for s in range(n // 128):
                    pv, b_pv = self.bank(5 + s % 2)
                    vt, b_vt = vts[s % 2]
                    for kt in range(8):
                        k.op("pe", "matmul", [b_wq, b_hT], [b_pv], pv, lhsT=hT[:, kt, s * 128:(s + 1) * 128],
                             rhs=wq[:, kt, 1536:2048], start=(kt == 0), stop=(kt == 7))
                    k.op("act", "activation", [b_pv], [b_vt], out=vt[:], in_=pv, func=AF.Copy)
                    k.dma("sp", self.vS[t0 + s * 128:t0 + (s + 1) * 128, :], vt[:], [b_vt], [self.b_vS])
            k.barrier()

    def phase_attention(self, nh, nkv, kvmap, Dh, dv, scale, mixdst):
        nc, k = self.nc, self.k
        from contextlib import ExitStack
        with ExitStack() as st:
            kTs, b_kTs = self.tl(st, "kTs", [128, T], BF16)
            vs, b_vs = self.tl(st, "vs", [128, NT, dv], BF16)
            qTs, b_qTs = self.tl(st, "qTs", [128, T], BF16)
            Pts = [self.tl(st, f"Pt{i}", [128, 2, 512], BF16) for i in range(2)]
            acc2, b_acc2 = self.tl(st, "acc2", [128, 2, 512], F32)
            accs, b_accs = self.tl(st, "accs", [128, 512], F32)
            rl, b_rl = self.tl(st, "rl", [128, 512], F32)
            obs = [self.tl(st, f"ob{i}", [128, 512], BF16) for i in range(2)]
            ones_f = self.cm_f[:, 1, 0:dv]
            gcnt = 0
            ccnt = 0
            for kh in range(nkv):
                k.dma("sp", kTs[0:Dh, :], self.kT[kh, 0:Dh, :], [self.b_kT], [b_kTs])
                k.dma("sp", vs[:], self.vS[:, kh * dv:(kh + 1) * dv].rearrange("(kt p) d -> p kt d", p=128),
                      [self.b_vS], [b_vs])
                for h in range(nh):
                    if kvmap(h) != kh:
                        continue
                    k.dma("sp", qTs[0:Dh, :], self.qT[h, 0:Dh, :], [self.b_qT], [b_qTs])
                    for ci, (t0, n) in enumerate(CHUNKS):
                        nk = 2 if ci == 0 else NT
                        pO, b_pO = self.bank(4 + ccnt % 2)
                        G_ = nk // 2

                        def emit_qk(g, gc):
                            sp_t = self.pst[gc % 2]
                            bS = [self.pb[(gc % 2) * 2], self.pb[(gc % 2) * 2 + 1]]
                            for jj in range(2):
                                kt = g * 2 + jj
                                k.op("pe", "matmul", [b_kTs, b_qTs], [bS[jj]], sp_t[:, jj * 512:jj * 512 + n],
                                     lhsT=kTs[0:Dh, kt * 128:(kt + 1) * 128], rhs=qTs[0:Dh, t0:t0 + n],
                                     start=True, stop=True)

                        def emit_rest(g, gc):
                            sp_t = self.pst[gc % 2]
                            bS = [self.pb[(gc % 2) * 2], self.pb[(gc % 2) * 2 + 1]]
                            Pt, b_Pt = Pts[gc % 2]
                            k.op("act", "activation", bS, [b_Pt], out=Pt[:, :, :n],
                                 in_=sp_t.rearrange("p (a b) -> p a b", b=512)[:, :, :n], func=AF.Exp, scale=scale)
                            for jj in range(2):
                                kt = g * 2 + jj
                                k.op("pe", "matmul", [b_vs, b_Pt], [b_pO], pO[0:dv, :n], lhsT=vs[:, kt, :],
                                     rhs=Pt[:, jj, :n], start=(kt == 0), stop=(kt == nk - 1))
                            eng_ = "dve"
                            if g == 0:
                                k.op(eng_, "tensor_copy", [b_Pt], [b_acc2], out=acc2[:, :, :n], in_=Pt[:, :, :n])
                            else:
                                k.op(eng_, "tensor_tensor", [b_Pt, b_acc2], [b_acc2], out=acc2[:, :, :n],
                                     in0=acc2[:, :, :n], in1=Pt[:, :, :n], op=ALU.add)

                        emit_qk(0, gcnt)
                        for g in range(G_):
                            if g + 1 < G_:
                                emit_qk(g + 1, gcnt + 1)
                            emit_rest(g, gcnt)
                            gcnt += 1
                        k.op("pool", "tensor_tensor", [b_acc2], [b_accs], out=accs[:, :n], in0=acc2[:, 0, :n],
                             in1=acc2[:, 1, :n], op=ALU.add)
                        pL, b_pL = self.bank(6)
                        k.op("pe", "matmul", [b_accs, self.b_cmf], [b_pL], pL[0:dv, :n], lhsT=ones_f,
                             rhs=accs[:, :n], start=True, stop=True)
                        k.op("dve", "reciprocal", [b_pL], [b_rl], out=rl[0:dv, :n], in_=pL[0:dv, :n])
                        ob, b_ob = obs[ccnt % 2]
                        ccnt += 1
                        k.op("dve", "tensor_tensor", [b_pO, b_rl], [b_ob], out=ob[0:dv, :n], in0=pO[0:dv, :n],
                             in1=rl[0:dv, :n], op=ALU.mult)
                        mk, r0 = mixdst(h)
                        k.dma("sp", self.mixT[mk, r0:r0 + dv, t0:t0 + n], ob[0:dv, :n], [b_ob], [self.b_mixT])
            k.barrier()

    def phase_outproj(self, li, l):
        nc, k = self.nc, self.k
        from contextlib import ExitStack
        w = self.W[l]
        with ExitStack() as st:
            wo, b_wo = self.tl(st, "wo", [128, 8, D], BF16)
            mixs = [self.tl(st, f"mix{i}", [128, 8, 512], BF16) for i in range(2)]
            xcs = [self.tl(st, f"xc{i}", [128, 8, 512], F32) for i in range(2)]
            for kt in range(8):
                k.dma("pool", wo[:, kt, :], w["w_out"][kt * 128:(kt + 1) * 128, :], [], [b_wo])
            for ci, (t0, n) in enumerate(CHUNKS):
                j = 1 if ci == 0 else 0
                xc, b_xc = xcs[ci % 2]
                mix, b_mix = mixs[ci % 2]
                k.dma("sp", xc[:, :, :n], self.xT[:, :, t0:t0 + n].rearrange("k p t -> p k t"), [self.b_xT], [b_xc])
                k.dma("sp", mix[:, :, :n], self.mixT[:, :, t0:t0 + n].rearrange("k p t -> p k t"),
                      [self.b_mixT], [b_mix])
                for ot in range(8):
                    pt, b_pt = self.bank(ot % 4)
                    for kt in range(8):
                        k.op("pe", "matmul", [b_wo, b_mix], [b_pt], pt[:, :n],
                             lhsT=wo[:, kt, ot * 128:(ot + 1) * 128], rhs=mix[:, kt, :n],
                             start=(kt == 0), stop=(kt == 7))
                    k.op("dve", "scalar_tensor_tensor", [b_pt, self.b_mT, b_xc], [b_xc], out=xc[:, ot, :n],
                         in0=pt[:, :n], scalar=self.mT[:, li, 16 + ot, j:j + 1], in1=xc[:, ot, :n],
                         op0=ALU.mult, op1=ALU.add)
                k.dma("sp", self.xT[:, :, t0:t0 + n].rearrange("k p t -> p k t"), xc[:, :, :n], [b_xc], [self.b_xT])
            k.barrier()

    def phase_moe_dense(self, li, l):
        nc, k = self.nc, self.k
        from contextlib import ExitStack
        w = self.W[l]
        idf = self.cm_f[:, 0, :]
        with ExitStack() as st:
            XT, b_XT = self.tl(st, "XT", [128, 8, PASS_T], BF16)
            gate, b_gate = self.tl(st, "gate", [128, PASS_NT, 32], F32)
            wsets = []
            for i in range(2):
                wg, b_wg = self.tl(st, f"wg{i}", [128, 8, 512], BF16)
                wu, b_wu = self.tl(st, f"wu{i}", [128, 8, 512], BF16)
                wd, b_wd = self.tl(st, f"wd{i}", [128, 4, D], BF16)
                wsets.append((wg, b_wg, wu, b_wu, wd, b_wd))
            H, b_H = self.tl(st, "H", [128, 4, 512], BF16)
            sg, b_sg = self.tl(st, "sg", [128, 512], F32)
            wr, b_wr = self.tl(st, "wr", [128, 8, 36], F32)
            brt, b_brt = self.tl(st, "brt", [128, 36], F32)
            k.dma("sp", wr[:], w["w_r"].rearrange("(k p) e -> p k e", p=128), [], [b_wr])
            k.dma("sp", brt[:], w["b_r"][0:1, :].partition_broadcast(128), [], [b_brt])
            wcnt = 0
            for pi, subs in enumerate(PASSES):
                p0 = pi * PASS_T
                with ExitStack() as st2:
                    xc, b_xc = self.tl(st2, "xc", [128, 8, 512], F32)
                    gT, b_gT = self.tl(st2, "gT", [128, 8, 512], F32)
                    sq, b_sq = self.tl(st2, "sq", [128, 8, 512], BF16)
                    tmp, b_tmp = self.tl(st2, "tmp", [128, 8, 512], F32)
                    rs, b_rs = self.tl(st2, "rs", [128, 512], F32)
                    sm, b_sm = self.tl(st2, "sm", [128, 160], F32)
                    for (t0, n) in subs:
                        j = 1 if t0 < NCTX else 0
                        lc = t0 - p0
                        k.dma("sp", xc[:, :, :n], self.xT[:, :, t0:t0 + n].rearrange("k p t -> p k t"),
                              [self.b_xT], [b_xc])
                        self.norm_mod(xc, b_xc, n, li, 1, j, lambda kt: gT[:, kt, :n], b_gT, sq, b_sq, rs, b_rs,
                                      tmp, b_tmp, 7)
                        k.op("act", "activation", [b_gT], [b_XT], out=XT[:, :, lc:lc + n], in_=gT[:, :, :n],
                             func=AF.Copy)
                        for s in range(n // 128):
                            ti = (lc + s * 128) // 128
                            pr, b_pr = self.bank(6)
                            for kt in range(8):
                                k.op("pe", "matmul", [b_gT, b_wr], [b_pr], pr[:, 0:36],
                                     lhsT=gT[:, kt, s * 128:(s + 1) * 128], rhs=wr[:, kt, :],
                                     start=(kt == 0), stop=(kt == 7))
                            self.router(pr, b_pr, brt, b_brt, sm, b_sm, gate[:, ti, :], b_gate)
                    k.barrier()
                st3 = ExitStack()
                facc, b_facc = self.tl(st3, "facc", [128, PASS_NT, D], F32)
                for e in range(NEXP):
                    wg, b_wg, wu, b_wu, wd, b_wd = wsets[wcnt % 2]
                    wcnt += 1
                    k.dma("pool", wg[:], w["w_gate"][e].rearrange("(k p) f -> p k f", p=128), [], [b_wg])
                    k.dma("pool", wu[:], w["w_up"][e].rearrange("(k p) f -> p k f", p=128), [], [b_wu])
                    k.dma("pool", wd[:], w["w_down"][e].rearrange("(k p) f -> p k f", p=128), [], [b_wd])
                    for (t0, n) in subs:
                        lc = t0 - p0
                        for ft in range(4):
                            pg, b_pg = self.bank(ft % 2)
                            pu, b_pu = self.bank(2 + ft % 2)
                            for kt in range(8):
                                k.op("pe", "matmul", [b_wg, b_XT], [b_pg], pg[:, :n],
                                     lhsT=wg[:, kt, ft * 128:(ft + 1) * 128], rhs=XT[:, kt, lc:lc + n],
                                     start=(kt == 0), stop=(kt == 7))
                            for kt in range(8):
                                k.op("pe", "matmul", [b_wu, b_XT], [b_pu], pu[:, :n],
                                     lhsT=wu[:, kt, ft * 128:(ft + 1) * 128], rhs=XT[:, kt, lc:lc + n],
                                     start=(kt == 0), stop=(kt == 7))
                            k.op("act", "activation", [b_pg], [b_sg], out=sg[:, :n], in_=pg[:, :n], func=AF.Silu)
                            k.op("dve", "tensor_tensor", [b_sg, b_pu], [b_H], out=H[:, ft, :n], in0=sg[:, :n],
                                 in1=pu[:, :n], op=ALU.mult)
                        for s in range(n // 128):
                            ti = (lc + s * 128) // 128
                            for half in range(2):
                                pd, b_pd = self.bank(4 + half)
                                for ft in range(4):
                                    k.op("pe", "matmul", [b_H, b_wd], [b_pd], pd,
                                         lhsT=H[:, ft, s * 128:(s + 1) * 128],
                                         rhs=wd[:, ft, half * 512:(half + 1) * 512],
                                         start=(ft == 0), stop=(ft == 3))
                                fa = facc[:, ti, half * 512:(half + 1) * 512]
                                if e == 0:
                                    k.op("dve", "tensor_scalar", [b_pd, b_gate], [b_facc], out=fa, in0=pd,
                                         scalar1=gate[:, ti, e:e + 1], scalar2=None, op0=ALU.mult)
                                else:
                                    k.op("dve", "scalar_tensor_tensor", [b_pd, b_gate, b_facc], [b_facc], out=fa,
                                         in0=pd, scalar=gate[:, ti, e:e + 1], in1=fa, op0=ALU.mult, op1=ALU.add)
                k.barrier()
                with ExitStack() as st2:
                    xts = [self.tl(st2, f"xt{i}", [128, 8, 128], F32) for i in range(2)]
                    for ti in range(PASS_NT):
                        t0 = p0 + ti * 128
                        j = 1 if t0 < NCTX else 0
                        xt, b_xt = xts[ti % 2]
                        k.dma("sp", xt[:], self.xT[:, :, t0:t0 + 128].rearrange("k p t -> p k t"),
                              [self.b_xT], [b_xt])
                        for half in range(2):
                            pt, b_pt = self.bank((ti % 2) * 2 + half)
                            for kk in range(4):
                                kt = half * 4 + kk
                                k.op("pe", "transpose", [b_facc, self.b_cmf], [b_pt], pt[:, kk * 128:(kk + 1) * 128],
                                     facc[:, ti, kt * 128:(kt + 1) * 128], idf)
                            for kk in range(4):
                                kt = half * 4 + kk
                                k.op("dve", "scalar_tensor_tensor", [b_pt, self.b_mT, b_xt], [b_xt],
                                     out=xt[:, kt, :], in0=pt[:, kk * 128:(kk + 1) * 128],
                                     scalar=self.mT[:, li, 40 + kt, j:j + 1], in1=xt[:, kt, :],
                                     op0=ALU.mult, op1=ALU.add)
                        k.dma("sp", self.xT[:, :, t0:t0 + 128].rearrange("k p t -> p k t"), xt[:],
                              [b_xt], [self.b_xT])
                    k.barrier()
                st3.close()

    def phase_moe(self, li, l):
        nc, k = self.nc, self.k
        from contextlib import ExitStack
        w = self.W[l]
        idf = self.cm_f[:, 0, :]
        NBLK = 66
        BR = 256
        if not hasattr(self, "Xs"):
            self.Xs_t = nc.dram_tensor("Xs", [NBLK * BR, D], BF16, kind="Internal")
            self.Ys_t = nc.dram_tensor("Ys", [NBLK * BR, D], F32, kind="Internal")
            self.Xs, self.Ys = self.Xs_t.ap(), self.Ys_t.ap()
            self.b_Xs, self.b_Ys = k.buf("Xs"), k.buf("Ys")
        with ExitStack() as st:
            dest, b_dest = self.tl(st, "dest", [128, NT, 2], I32)
            gw, b_gw = self.tl(st, "gw", [128, NT, 2], F32)
            idxG, b_idxG = self.tl(st, "idxG", [128, NBLK], I32)
            with ExitStack() as st2:
                gall, b_gall = self.tl(st2, "gall", [128, NT, D], BF16)
                selA, b_selA = self.tl(st2, "selA", [128, NT, 2, 32], F32)
                rk, b_rk = self.tl(st2, "rk", [128, NT, 2], F32)
                sm, b_sm = self.tl(st2, "sm", [128, 320], F32)
                selb, b_selb = self.tl(st2, "selb", [128, 32], BF16)
                tot, b_tot = self.tl(st2, "tot", [128, 32], F32)
                wr, b_wr = self.tl(st2, "wr", [128, 8, 36], F32)
                brt, b_brt = self.tl(st2, "brt", [128, 36], F32)
                k.dma("sp", wr[:], w["w_r"].rearrange("(k p) e -> p k e", p=128), [], [b_wr])
                k.dma("sp", brt[:], w["b_r"][0:1, :].partition_broadcast(128), [], [b_brt])
                k.op("dve", "memset", [], [b_tot], tot[:], 0.0)
                Ltri = self.cm_b[:, 4, :]
                ones_b = self.cm_b[:, 1, :]
                R, Wm = [b_sm], [b_sm]
                with ExitStack() as st3:
                    xc, b_xc = self.tl(st3, "xc", [128, 8, 512], F32)
                    gT, b_gT = self.tl(st3, "gT", [128, 8, 512], F32)
                    sq, b_sq = self.tl(st3, "sq", [128, 8, 512], BF16)
                    tmp, b_tmp = self.tl(st3, "tmp", [128, 8, 512], F32)
                    rs, b_rs = self.tl(st3, "rs", [128, 512], F32)
                    for (t0, n) in CHUNKS:
                        j = 1 if t0 < NCTX else 0
                        k.dma("sp", xc[:, :, :n], self.xT[:, :, t0:t0 + n].rearrange("k p t -> p k t"),
                              [self.b_xT], [b_xc])
                        self.norm_mod(xc, b_xc, n, li, 1, j, lambda kt: gT[:, kt, :n], b_gT, sq, b_sq, rs, b_rs,
                                      tmp, b_tmp, 7)
                        for s in range(n // 128):
                            ti = (t0 + s * 128) // 128
                            pr, b_pr = self.bank(6)
                            for kt in range(8):
                                k.op("pe", "matmul", [b_gT, b_wr], [b_pr], pr[:, 0:36],
                                     lhsT=gT[:, kt, s * 128:(s + 1) * 128], rhs=wr[:, kt, :],
                                     start=(kt == 0), stop=(kt == 7))
                            f = self.router2(pr, b_pr, brt, b_brt, sm, b_sm)
                            k.op("dve", "tensor_tensor", R, [b_selb], out=selb[:], in0=f["sel1"], in1=f["sel2"], op=ALU.add)
                            k.op("dve", "tensor_copy", R, [b_selA], out=selA[:, ti, 0, :], in_=f["sel1"])
                            k.op("dve", "tensor_copy", R, [b_selA], out=selA[:, ti, 1, :], in_=f["sel2"])
                            pc, b_pc = self.bank(5)
                            k.op("pe", "matmul", [b_selb, self.b_cmb], [b_pc], pc[:, 0:32], lhsT=Ltri, rhs=selb[:],
                                 start=True, stop=True)
                            k.op("pe", "matmul", [b_selb, self.b_cmb], [b_pc], pc[:, 32:64], lhsT=ones_b, rhs=selb[:],
                                 start=True, stop=True)
                            slot = sm[:, 200:232]
                            k.op("dve", "tensor_tensor", [b_pc, b_tot], Wm, out=slot, in0=pc[:, 0:32], in1=tot[:], op=ALU.add)
                            k.op("dve", "tensor_tensor", [b_pc, b_tot], [b_tot], out=tot[:], in0=pc[:, 32:64], in1=tot[:],
                                 op=ALU.add)
                            t32 = sm[:, 232:264]
                            for kk, nm_ in enumerate(("sel1", "sel2")):
                                k.op("dve", "tensor_tensor", R, Wm, out=t32, in0=f[nm_], in1=slot, op=ALU.mult)
                                k.op("dve", "tensor_reduce", R, [b_rk], out=rk[:, ti, kk:kk + 1], in_=t32, axis=AX.X, op=ALU.add)
                            k.op("dve", "tensor_copy", R, [b_gw], out=gw[:, ti, 0:1], in_=f["gA"])
                            k.op("dve", "tensor_copy", R, [b_gw], out=gw[:, ti, 1:2], in_=f["gB"])
                            for half in range(2):
                                pt, b_pt = self.bank(half)
                                for kk in range(4):
                                    kt = half * 4 + kk
                                    k.op("pe", "transpose", [b_gT, self.b_cmf], [b_pt], pt[:, kk * 128:(kk + 1) * 128],
                                         gT[:, kt, s * 128:(s + 1) * 128], idf)
                                if half == 0:
                                    k.op("act", "activation", [b_pt], [b_gall], out=gall[:, ti, 0:512], in_=pt, func=AF.Copy)
                                else:
                                    k.op("dve", "tensor_copy", [b_pt], [b_gall], out=gall[:, ti, 512:1024], in_=pt)
                with ExitStack() as st3:
                    nb, b_nb = self.tl(st3, "nb", [128, 32], F32)
                    pe_, b_pe = self.tl(st3, "pend", [128, 32], F32)
                    pst, b_pst = self.tl(st3, "pstart", [128, 32], F32)
                    onesr, b_onesr = self.tl(st3, "onesr", [128, 32], F32)
                    bio, b_bio = self.tl(st3, "bio", [128, NBLK], F32)
                    bioi, b_bioi = self.tl(st3, "bioi", [128, NBLK], I32)
                    bexp, b_bexp = self.tl(st3, "bexp", [128, NBLK], F32)
                    basei, b_basei = self.tl(st3, "basei", [128, 1], I32)
                    basef, b_basef = self.tl(st3, "basef", [128, 1], F32)
                    idf32, b_idf32 = self.tl(st3, "idf32", [128, NBLK], F32)
                    k.op("dve", "tensor_scalar", [b_tot], [b_nb], out=nb[:], in0=tot[:], scalar1=0.0, scalar2=None,
                         op0=ALU.is_gt)
                    for jj in range(1, 36):
                        k.op("dve", "scalar_tensor_tensor", [b_tot, b_nb], [b_nb], out=nb[:], in0=tot[:],
                             scalar=float(BR * jj), in1=nb[:], op0=ALU.is_gt, op1=ALU.add)
                    k.op("dve", "memset", [], [b_onesr], onesr[:], 1.0)
                    k.op("dve", "tensor_tensor_scan", [b_onesr, b_nb], [b_pe], out=pe_[:], data0=onesr[:], data1=nb[:],
                         initial=0.0, op0=ALU.mult, op1=ALU.add)
                    k.op("dve", "tensor_tensor", [b_pe, b_nb], [b_pst], out=pst[:], in0=pe_[:], in1=nb[:], op=ALU.subtract)
                    k.op("dve", "tensor_scalar", [b_pst], [b_pst], out=pst[:], in0=pst[:], scalar1=float(BR), scalar2=None,
                         op0=ALU.mult)
                    k.op("pool", "iota", [], [b_bioi], bioi[:], pattern=[[1, NBLK]], base=0, channel_multiplier=0)
                    k.op("dve", "tensor_copy", [b_bioi], [b_bio], out=bio[:], in_=bioi[:])
                    k.op("dve", "tensor_scalar", [b_bio, b_pe], [b_bexp], out=bexp[:], in0=bio[:], scalar1=pe_[:, 0:1],
                         scalar2=None, op0=ALU.is_ge)
                    for e in range(1, 32):
                        k.op("dve", "scalar_tensor_tensor", [b_bio, b_pe, b_bexp], [b_bexp], out=bexp[:], in0=bio[:],
                             scalar=pe_[:, e:e + 1], in1=bexp[:], op0=ALU.is_ge, op1=ALU.add)
                    k.op("dve", "tensor_scalar", [b_bexp], [b_bexp], out=bexp[:], in0=bexp[:], scalar1=31.0, scalar2=None,
                         op0=ALU.min)
                    k.op("pool", "iota", [], [b_basei], basei[:], pattern=[[0, 1]], base=0, channel_multiplier=1)
                    k.op("dve", "tensor_copy", [b_basei], [b_basef], out=basef[:], in_=basei[:])
                    k.op("dve", "tensor_scalar", [b_bexp, b_basef], [b_idf32], out=idf32[:], in0=bexp[:], scalar1=128.0,
                         scalar2=basef[:, 0:1], op0=ALU.mult, op1=ALU.add)
                    k.op("dve", "tensor_copy", [b_idf32], [b_idxG], out=idxG[:], in_=idf32[:])
                    t32 = sm[:, 232:264]
                    df = sm[:, 264:266]
                    for ti in range(NT):
                        for kk in range(2):
                            k.op("dve", "tensor_tensor", [b_selA, b_pst], Wm, out=t32, in0=selA[:, ti, kk, :], in1=pst[:],
                                 op=ALU.mult)
                            k.op("dve", "tensor_reduce", R, Wm, out=df[:, kk:kk + 1], in_=t32, axis=AX.X, op=ALU.add)
                        k.op("dve", "tensor_tensor", R + [b_rk], Wm, out=df, in0=df, in1=rk[:, ti, :], op=ALU.add)
                        k.op("dve", "tensor_copy", R, [b_dest], out=dest[:, ti, :], in_=df)
                        for kk in range(2):
                            k.dma("pool", None, None, [b_gall, b_dest], [self.b_Xs],
                                  indirect=lambda e, kk=kk, ti=ti: e.indirect_dma_start(
                                      out=self.Xs_t[:, :],
                                      out_offset=bass.IndirectOffsetOnAxis(ap=dest[:, ti, kk:kk + 1], axis=0),
                                      in_=gall[:, ti, :], in_offset=None))
                k.barrier()
            with ExitStack() as st2:
                wsets = []
                for i in range(2):
                    wg, b_wg = self.tl(st2, f"wg{i}", [128, 8, 512], BF16)
                    wu, b_wu = self.tl(st2, f"wu{i}", [128, 8, 512], BF16)
                    wd, b_wd = self.tl(st2, f"wd{i}", [128, 4, D], BF16)
                    wsets.append((wg, b_wg, wu, b_wu, wd, b_wd))
                xes = [self.tl(st2, f"xe{i}", [128, 2, D], BF16) for i in range(2)]
                XeTs = [self.tl(st2, f"XeT{i}", [128, 8, BR], BF16) for i in range(2)]
                H, b_H = self.tl(st2, "H", [128, 4, BR], BF16)
                sg, b_sg = self.tl(st2, "sg", [128, 4 * BR], F32)
                Yts = [self.tl(st2, f"Yt{i}", [128, D], F32) for i in range(2)]
                idb = self.cm_b[:, 0, :]
                wgt = self.Wt[l]["w_gate"]
                wut = self.Wt[l]["w_up"]
                wdt = self.Wt[l]["w_down"]
                ycnt = 0
                for b in range(NBLK):
                    wg, b_wg, wu, b_wu, wd, b_wd = wsets[b % 2]
                    xe, b_xe = xes[b % 2]
                    XeT, b_XeT = XeTs[b % 2]
                    for (wt_, dst, b_dst) in ((wgt, wg, b_wg), (wut, wu, b_wu), (wdt, wd, b_wd)):
                        k.dma("pool", None, None, [b_idxG], [b_dst],
                              indirect=lambda e, wt_=wt_, dst=dst, b=b: e.indirect_dma_start(
                                  out=dst[:].rearrange("p k f -> p (k f)"), out_offset=None, in_=wt_[:, :],
                                  in_offset=bass.IndirectOffsetOnAxis(ap=idxG[:, b:b + 1], axis=0)))
                    k.dma("sp", xe[:], self.Xs[b * BR:(b + 1) * BR, :].rearrange("(s p) d -> p s d", p=128),
                          [self.b_Xs], [b_xe])
                    for s_ in range(2):
                        pt, b_pt = self.bank(6 + s_)
                        ptb = pt.bitcast(BF16)
                        for kt in range(8):
                            k.op("pe", "transpose", [b_xe, self.b_cmb], [b_pt], ptb[:, kt * 128:(kt + 1) * 128],
                                 xe[:, s_, kt * 128:(kt + 1) * 128], idb)
                        src_v = ptb.rearrange("p (a b) -> p a b", b=128)
                        if s_ == 0:
                            k.op("act", "activation", [b_pt], [b_XeT], out=XeT[:, :, 0:128], in_=src_v, func=AF.Copy)
                        else:
                            k.op("dve", "tensor_copy", [b_pt], [b_XeT], out=XeT[:, :, 128:256], in_=src_v)
                    pg = self.pst[0]
                    pu = self.pst[1]
                    bg = [self.pb[0], self.pb[1]]
                    bu = [self.pb[2], self.pb[3]]
                    for ft in range(4):
                        for kt in range(8):
                            k.op("pe", "matmul", [b_wg, b_XeT], [bg[ft // 2]], pg[:, ft * BR:(ft + 1) * BR],
                                 lhsT=wg[:, kt, ft * 128:(ft + 1) * 128], rhs=XeT[:, kt, :], start=(kt == 0), stop=(kt == 7))
                    for ft in range(4):
                        for kt in range(8):
                            k.op("pe", "matmul", [b_wu, b_XeT], [bu[ft // 2]], pu[:, ft * BR:(ft + 1) * BR],
                                 lhsT=wu[:, kt, ft * 128:(ft + 1) * 128], rhs=XeT[:, kt, :], start=(kt == 0), stop=(kt == 7))
                    k.op("act", "activation", bg, [b_sg], out=sg[:], in_=pg[:, :], func=AF.Silu)
                    k.op("dve", "tensor_tensor", [b_sg] + bu, [b_H], out=H[:].rearrange("p a b -> p (a b)"), in0=sg[:],
                         in1=pu[:, :], op=ALU.mult)
                    for s_ in range(2):
                        Yt, b_Yt = Yts[ycnt % 2]
                        ycnt += 1
                        for half in range(2):
                            pd, b_pd = self.bank(4 + half)
                            for ft in range(4):
                                k.op("pe", "matmul", [b_H, b_wd], [b_pd], pd, lhsT=H[:, ft, s_ * 128:(s_ + 1) * 128],
                                     rhs=wd[:, ft, half * 512:(half + 1) * 512], start=(ft == 0), stop=(ft == 3))
                            if half == 0:
                                k.op("act", "activation", [b_pd], [b_Yt], out=Yt[:, 0:512], in_=pd, func=AF.Copy)
                            else:
                                k.op("dve", "tensor_copy", [b_pd], [b_Yt], out=Yt[:, 512:1024], in_=pd)
                        r0 = b * BR + s_ * 128
                        k.dma("sp", self.Ys[r0:r0 + 128, :], Yt[:], [b_Yt], [self.b_Ys])
                k.barrier()
            with ExitStack() as st2:
                xts = [self.tl(st2, f"xt{i}", [128, 8, 128], F32) for i in range(2)]
                Y0s = [self.tl(st2, f"Y0{i}", [128, D], F32) for i in range(2)]
                Y1s = [self.tl(st2, f"Y1{i}", [128, D], F32) for i in range(2)]
                for ti in range(NT):
                    t0 = ti * 128
                    j = 1 if t0 < NCTX else 0
                    xt, b_xt = xts[ti % 2]
                    Y0, b_Y0 = Y0s[ti % 2]
                    Y1, b_Y1 = Y1s[ti % 2]
                    k.dma("sp", xt[:], self.xT[:, :, t0:t0 + 128].rearrange("k p t -> p k t"), [self.b_xT], [b_xt])
                    for kk, (Yk, b_Yk) in enumerate(((Y0, b_Y0), (Y1, b_Y1))):
                        k.dma("pool", None, None, [self.b_Ys, b_dest], [b_Yk],
                              indirect=lambda e, kk=kk, Yk=Yk, ti=ti: e.indirect_dma_start(
                                  out=Yk[:, :], out_offset=None, in_=self.Ys_t[:, :],
                                  in_offset=bass.IndirectOffsetOnAxis(ap=dest[:, ti, kk:kk + 1], axis=0)))
                    k.op("dve", "tensor_scalar", [b_Y0, b_gw], [b_Y0], out=Y0[:], in0=Y0[:], scalar1=gw[:, ti, 0:1],
                         scalar2=None, op0=ALU.mult)
                    k.op("dve", "scalar_tensor_tensor", [b_Y1, b_gw, b_Y0], [b_Y0], out=Y0[:], in0=Y1[:],
                         scalar=gw[:, ti, 1:2], in1=Y0[:], op0=ALU.mult, op1=ALU.add)
                    for half in range(2):
                        pt, b_pt = self.bank((ti % 2) * 2 + half)
                        for kk in range(4):
                            kt = half * 4 + kk
                            k.op("pe", "transpose", [b_Y0, self.b_cmf], [b_pt], pt[:, kk * 128:(kk + 1) * 128],
                                 Y0[:, kt * 128:(kt + 1) * 128], idf)
                        for kk in range(4):
                            kt = half * 4 + kk
                            k.op("dve", "scalar_tensor_tensor", [b_pt, self.b_mT, b_xt], [b_xt],
                                 out=xt[:, kt, :], in0=pt[:, kk * 128:(kk + 1) * 128],
                                 scalar=self.mT[:, li, 40 + kt, j:j + 1], in1=xt[:, kt, :],
                                 op0=ALU.mult, op1=ALU.add)
                    k.dma("sp", self.xT[:, :, t0:t0 + 128].rearrange("k p t -> p k t"), xt[:], [b_xt], [self.b_xT])
                k.barrier()

    def router2(self, pr, b_pr, brt, b_brt, sm, b_sm):
        k = self.k
        R, W_ = [b_sm], [b_sm]
        lg = sm[:, 0:36]
        m1 = sm[:, 36:37]
        nm1 = sm[:, 37:38]
        oh1 = sm[:, 40:44]
        e1 = sm[:, 44:48]
        s1 = sm[:, 48:49]
        pg = sm[:, 49:50]
        pen = sm[:, 52:56]
        lg2m = sm[:, 56:88]
        top8 = sm[:, 88:96]
        d21 = sm[:, 96:97]
        ex = sm[:, 97:98]
        den = sm[:, 98:99]
        w1 = sm[:, 99:100]
        w2 = sm[:, 100:101]
        gA = sm[:, 101:102]
        gB = sm[:, 102:103]
        sel1 = sm[:, 104:136]
        sel2 = sm[:, 136:168]
        k.op("dve", "tensor_tensor", [b_pr, b_brt], W_, out=lg, in0=pr[:, 0:36], in1=brt[:, :], op=ALU.add)
        k.op("dve", "tensor_reduce", R, W_, out=m1, in_=lg[:, 0:4], axis=AX.X, op=ALU.max)
        k.op("dve", "tensor_scalar", R, W_, out=oh1, in0=lg[:, 0:4], scalar1=m1, scalar2=None, op0=ALU.is_equal)
        k.op("dve", "tensor_scalar", R, W_, out=nm1, in0=m1, scalar1=-1.0, scalar2=None, op0=ALU.mult)
        k.op("act", "activation", R, W_, out=e1, in_=lg[:, 0:4], func=AF.Exp, bias=nm1, scale=1.0)
        k.op("dve", "tensor_reduce", R, W_, out=s1, in_=e1, axis=AX.X, op=ALU.add)
        k.op("dve", "reciprocal", R, W_, out=pg, in_=s1)
        k.op("dve", "tensor_scalar", R, W_, out=pen, in0=oh1, scalar1=1e30, scalar2=-1e30, op0=ALU.mult, op1=ALU.add)
        k.op("dve", "tensor_tensor", R, W_, out=lg2m.rearrange("p (g i) -> p g i", i=8),
             in0=lg[:, 4:36].rearrange("p (g i) -> p g i", i=8),
             in1=pen.unsqueeze(2).broadcast_to([128, 4, 8]), op=ALU.add)
        k.op("dve", "max", R, W_, out=top8, in_=lg2m)
        k.op("dve", "tensor_tensor", R, W_, out=d21, in0=top8[:, 1:2], in1=top8[:, 0:1], op=ALU.subtract)
        k.op("act", "activation", R, W_, out=ex, in_=d21, func=AF.Exp)
        k.op("dve", "tensor_scalar", R, W_, out=den, in0=ex, scalar1=1.0, scalar2=None, op0=ALU.add)
        k.op("dve", "reciprocal", R, W_, out=w1, in_=den)
        k.op("dve", "tensor_tensor", R, W_, out=w2, in0=ex, in1=w1, op=ALU.mult)
        k.op("dve", "tensor_tensor", R, W_, out=gA, in0=w1, in1=pg, op=ALU.mult)
        k.op("dve", "tensor_tensor", R, W_, out=gB, in0=w2, in1=pg, op=ALU.mult)
        k.op("dve", "tensor_scalar", R, W_, out=sel1, in0=lg2m, scalar1=top8[:, 0:1], scalar2=None, op0=ALU.is_equal)
        k.op("dve", "tensor_scalar", R, W_, out=sel2, in0=lg2m, scalar1=top8[:, 1:2], scalar2=None, op0=ALU.is_equal)
        return dict(sel1=sel1, sel2=sel2, gA=gA, gB=gB)

    def router(self, pr, b_pr, brt, b_brt, sm, b_sm, gout, b_gate):
        k = self.k
        R, W_ = [b_sm], [b_sm]
        lg = sm[:, 0:36]
        m1 = sm[:, 36:37]
        nm1 = sm[:, 37:38]
        oh1 = sm[:, 40:44]
        e1 = sm[:, 44:48]
        s1 = sm[:, 48:49]
        pg = sm[:, 49:50]
        pen = sm[:, 52:56]
        lg2m = sm[:, 56:88]
        top8 = sm[:, 88:96]
        d21 = sm[:, 96:97]
        ex = sm[:, 97:98]
        den = sm[:, 98:99]
        w1 = sm[:, 99:100]
        w2 = sm[:, 100:101]
        gA = sm[:, 101:102]
        gB = sm[:, 102:103]
        sel = sm[:, 104:136]
        k.op("dve", "tensor_tensor", [b_pr, b_brt], W_, out=lg, in0=pr[:, 0:36], in1=brt[:, :], op=ALU.add)
        k.op("dve", "tensor_reduce", R, W_, out=m1, in_=lg[:, 0:4], axis=AX.X, op=ALU.max)
        k.op("dve", "tensor_scalar", R, W_, out=oh1, in0=lg[:, 0:4], scalar1=m1, scalar2=None, op0=ALU.is_equal)
        k.op("dve", "tensor_scalar", R, W_, out=nm1, in0=m1, scalar1=-1.0, scalar2=None, op0=ALU.mult)
        k.op("act", "activation", R, W_, out=e1, in_=lg[:, 0:4], func=AF.Exp, bias=nm1, scale=1.0)
        k.op("dve", "tensor_reduce", R, W_, out=s1, in_=e1, axis=AX.X, op=ALU.add)
        k.op("dve", "reciprocal", R, W_, out=pg, in_=s1)
        k.op("dve", "tensor_scalar", R, W_, out=pen, in0=oh1, scalar1=1e30, scalar2=-1e30, op0=ALU.mult, op1=ALU.add)
        k.op("dve", "tensor_tensor", R, W_, out=lg2m.rearrange("p (g i) -> p g i", i=8),
             in0=lg[:, 4:36].rearrange("p (g i) -> p g i", i=8),
             in1=pen.unsqueeze(2).broadcast_to([128, 4, 8]), op=ALU.add)
        k.op("dve", "max", R, W_, out=top8, in_=lg2m)
        k.op("dve", "tensor_tensor", R, W_, out=d21, in0=top8[:, 1:2], in1=top8[:, 0:1], op=ALU.subtract)
        k.op("act", "activation", R, W_, out=ex, in_=d21, func=AF.Exp)
        k.op("dve", "tensor_scalar", R, W_, out=den, in0=ex, scalar1=1.0, scalar2=None, op0=ALU.add)
        k.op("dve", "reciprocal", R, W_, out=w1, in_=den)
        k.op("dve", "tensor_tensor", R, W_, out=w2, in0=ex, in1=w1, op=ALU.mult)
        k.op("dve", "tensor_tensor", R, W_, out=gA, in0=w1, in1=pg, op=ALU.mult)
        k.op("dve", "tensor_tensor", R, W_, out=gB, in0=w2, in1=pg, op=ALU.mult)
        k.op("dve", "tensor_scalar", R, W_, out=sel, in0=lg2m, scalar1=top8[:, 0:1], scalar2=gA,
             op0=ALU.is_equal, op1=ALU.mult)
        k.op("dve", "tensor_scalar", R, W_, out=lg2m, in0=lg2m, scalar1=top8[:, 1:2], scalar2=gB,
             op0=ALU.is_equal, op1=ALU.mult)
        k.op("dve", "tensor_tensor", R, [b_gate], out=gout, in0=sel, in1=lg2m, op=ALU.add)

    def s5_params(self, st, are, aim, ldt, P, C, tag):
        k = self.k
        pool_t, b_t = self.tl(st, "s5p" + tag, [128, 24, C], F32)
        R, Wr = [b_t], [b_t]
        nxt = [0]

        def new():
            i = nxt[0]
            nxt[0] += 1
            assert i < 24
            return pool_t[0:P, i, :]

        def ts(out, in0, s1, s2, op0, op1=None):
            if op1 is None:
                k.op("dve", "tensor_scalar", R, Wr, out=out, in0=in0, scalar1=s1, scalar2=None, op0=op0)
            else:
                k.op("dve", "tensor_scalar", R, Wr, out=out, in0=in0, scalar1=s1, scalar2=s2, op0=op0, op1=op1)

        def tt(out, a, b, op):
            k.op("dve", "tensor_tensor", R, Wr, out=out, in0=a, in1=b, op=op)

        def stt(out, in0, sc, in1, op0, op1):
            k.op("dve", "scalar_tensor_tensor", R, Wr, out=out, in0=in0, scalar=sc, in1=in1, op0=op0, op1=op1)

        dt = new()
        k.op("act", "activation", R + [self._b_s5raw], Wr, out=dt, in_=ldt, func=AF.Exp)
        xr = new()
        k.op("dve", "tensor_tensor", R + [self._b_s5raw], Wr, out=xr, in0=dt, in1=are, op=ALU.mult)
        p = new()
        ts(p, xr, 1.0 / 120, None, ALU.mult)
        for c in (1.0 / 24, 1.0 / 6, 0.5, 1.0):
            stt(p, p, c, xr, ALU.add, ALU.mult)
        mag = new()
        ts(mag, p, 1.0, None, ALU.add)
        th = new()
        k.op("dve", "tensor_tensor", R + [self._b_s5raw], Wr, out=th, in0=dt, in1=aim, op=ALU.mult)
        kk = new()
        ts(kk, th, math.pi, None, ALU.is_gt)
        for j in range(2, 6):
            stt(kk, th, (2 * j - 1) * math.pi, kk, ALU.is_gt, ALU.add)
        thr = new()
        stt(thr, kk, -2.0 * math.pi, th, ALU.mult, ALU.add)
        x8 = new()
        ts(x8, thr, 0.125, None, ALU.mult)
        x2 = new()
        tt(x2, x8, x8, ALU.mult)
        s = new()
        ts(s, x2, 1.0 / 362880, None, ALU.mult)
        for c in (-1.0 / 5040, 1.0 / 120, -1.0 / 6):
            stt(s, s, c, x2, ALU.add, ALU.mult)
        stt(s, s, 1.0, x8, ALU.add, ALU.mult)
        cc = new()
        ts(cc, x2, 1.0 / 40320, None, ALU.mult)
        for c in (-1.0 / 720, 1.0 / 24, -0.5):
            stt(cc, cc, c, x2, ALU.add, ALU.mult)
        ts(cc, cc, 1.0, None, ALU.add)
        s_b, c_b, t_b = new(), new(), new()
        cur_c, cur_s, oth_c, oth_s = cc, s, c_b, s_b
        for _ in range(3):
            stt(oth_s, cur_c, 2.0, cur_s, ALU.mult, ALU.mult)
            tt(t_b, cur_s, cur_s, ALU.mult)
            tt(oth_c, cur_c, cur_c, ALU.mult)
            tt(oth_c, oth_c, t_b, ALU.subtract)
            cur_c, cur_s, oth_c, oth_s = oth_c, oth_s, cur_c, cur_s
        c1, s1 = cur_c, cur_s
        abr, abi = new(), new()
        tt(abr, mag, c1, ALU.mult)
        tt(abi, mag, s1, ALU.mult)
        den = new()
        k.op("dve", "tensor_tensor", R + [self._b_s5raw], Wr, out=den, in0=are, in1=are, op=ALU.mult)
        k.op("dve", "tensor_tensor", R + [self._b_s5raw], Wr, out=t_b, in0=aim, in1=aim, op=ALU.mult)
        tt(den, den, t_b, ALU.add)
        k.op("dve", "reciprocal", R, Wr, out=den, in_=den)
        nr = new()
        ts(nr, abr, -1.0, None, ALU.add)
        zr, zi = new(), new()
        k.op("dve", "tensor_tensor", R + [self._b_s5raw], Wr, out=zr, in0=nr, in1=are, op=ALU.mult)
        k.op("dve", "tensor_tensor", R + [self._b_s5raw], Wr, out=t_b, in0=abi, in1=aim, op=ALU.mult)
        tt(zr, zr, t_b, ALU.add)
        tt(zr, zr, den, ALU.mult)
        k.op("dve", "tensor_tensor", R + [self._b_s5raw], Wr, out=zi, in0=abi, in1=are, op=ALU.mult)
        k.op("dve", "tensor_tensor", R + [self._b_s5raw], Wr, out=t_b, in0=nr, in1=aim, op=ALU.mult)
        tt(zi, zi, t_b, ALU.subtract)
        tt(zi, zi, den, ALU.mult)
        return dict(r=mag, c1=c1, s1=s1, zr=zr, zi=zi, buf=b_t)

    def phase_even(self, li, l):
        nc, k = self.nc, self.k
        from contextlib import ExitStack
        w = self.W[l]
        if not hasattr(self, "uT"):
            self.uT = self.scratch("uT", [4, 128, T], BF16)
            self.b_uT = k.buf("uT")
            self.yT = self.scratch("yT", [4, 128, T], F32)
            self.b_yT = k.buf("yT")
        idf = self.cm_f[:, 0, :]
        with ExitStack() as st:
            wi, b_wi = self.tl(st, "wi", [128, 8, 928], BF16)
            wuq, b_wuq = self.tl(st, "wuq", [128, 2, 768], BF16)
            wukv, b_wukv = self.tl(st, "wukv", [128, 1024], BF16)
            rope, b_rope = self.tl(st, "rope", [128, 2, T], F32)
            xcs = [self.tl(st, f"xc{i}", [128, 8, 512], F32) for i in range(2)]
            hTs = [self.tl(st, f"hT{i}", [128, 8, 512], BF16) for i in range(2)]
            sq, b_sq = self.tl(st, "sq", [128, 8, 512], BF16)
            tmp, b_tmp = self.tl(st, "tmp", [128, 8, 512], F32)
            rs, b_rs = self.tl(st, "rs", [128, 512], F32)
            gn, b_gn = self.tl(st, "gn", [128, 5], F32)
            qos = [self.tl(st, f"qo{i}", [128, 512], BF16) for i in range(2)]
            vts = [self.tl(st, f"vt{i}", [128, 512], BF16) for i in range(2)]
            uos = [self.tl(st, f"uo{i}", [128, 512], BF16) for i in range(2)]
            cqf, b_cqf = self.tl(st, "cqf", [128, 3, 512], F32)
            cqs, b_cqs = self.tl(st, "cqs", [128, 3, 512], BF16)
            cqn, b_cqn = self.tl(st, "cqn", [128, 3, 512], BF16)
            rs2, b_rs2 = self.tl(st, "rs2", [128, 2, 512], F32)
            krb, b_krb = self.tl(st, "krb", [32, 512], BF16)
            wk = self.qk_work(st)
            for kt in range(8):
                k.dma("pool", wi[:, kt, :], w["w_in"][kt * 128:(kt + 1) * 128, :], [], [b_wi])
            for i in range(2):
                k.dma("pool", wuq[:, i, :], w["w_uq"][i * 128:(i + 1) * 128, :], [], [b_wuq])
            k.dma("pool", wukv[:], w["w_ukv"], [], [b_wukv])
            k.dma("sp", rope[:], self.rope_m_d, [], [b_rope])
            k.dma("sp", gn[:, 0:1], w["gq"][0:128, :], [], [b_gn])
            k.dma("sp", gn[:, 1:2], w["gq"][128:256, :], [], [b_gn])
            k.dma("sp", gn[:, 2:3], w["gkv"], [], [b_gn])
            k.dma("sp", gn[0:96, 3:4], w["mqn"], [], [b_gn])
            k.dma("sp", gn[0:96, 4:5], w["mkn"], [], [b_gn])
            Rm = self.cm_b[0:96, 3, 0:96]
            ones_b = self.cm_b[:, 1, :]
            idb = self.cm_b[0:32, 0, 0:32]
            cnt = 0
            ucnt = 0
            for ci, (t0, n) in enumerate(CHUNKS):
                j = 1 if ci == 0 else 0
                xc, b_xc = xcs[ci % 2]
                hT, b_hT = hTs[ci % 2]
                k.dma("sp", xc[:, :, :n], self.xT[:, :, t0:t0 + n].rearrange("k p t -> p k t"), [self.b_xT], [b_xc])
                self.norm_mod(xc, b_xc, n, li, 0, j, lambda kt: hT[:, kt, :n], b_hT, sq, b_sq, rs, b_rs,
                              tmp, b_tmp, 0)
                for ut in range(4):
                    pt, b_pt = self.bank(1 + ut % 2)
                    for kt in range(8):
                        k.op("pe", "matmul", [b_wi, b_hT], [b_pt], pt[:, :n], lhsT=wi[:, kt, ut * 128:(ut + 1) * 128],
                             rhs=hT[:, kt, :n], start=(kt == 0), stop=(kt == 7))
                    uo, b_uo = uos[ucnt % 2]
                    ucnt += 1
                    k.op("act", "activation", [b_pt], [b_uo], out=uo[:, :n], in_=pt[:, :n], func=AF.Copy)
                    k.dma("sp", self.uT[ut, :, t0:t0 + n], uo[:, :n], [b_uo], [self.b_uT])
                if self.stop == "E1u":
                    continue
                for i3 in range(3):
                    pt, b_pt = self.bank(1 + i3 % 2)
                    c0 = 512 + i3 * 128
                    for kt in range(8):
                        k.op("pe", "matmul", [b_wi, b_hT], [b_pt], pt[:, :n], lhsT=wi[:, kt, c0:c0 + 128],
                             rhs=hT[:, kt, :n], start=(kt == 0), stop=(kt == 7))
                    k.op("act", "activation", [b_pt], [b_cqf], out=cqf[:, i3, :n], in_=pt[:, :n], func=AF.Copy)
                    k.op("act", "activation", [b_cqf], [b_cqs], out=cqs[:, i3, :n], in_=cqf[:, i3, :n], func=AF.Square)
                if self.stop == "E1c1":
                    continue
                pt, b_pt = self.bank(1)
                for kt in range(8):
                    k.op("pe", "matmul", [b_wi, b_hT], [b_pt], pt[0:32, :n], lhsT=wi[:, kt, 896:928],
                         rhs=hT[:, kt, :n], start=(kt == 0), stop=(kt == 7))
                k.op("act", "activation", [b_pt], [b_krb], out=krb[:, :n], in_=pt[0:32, :n], func=AF.Copy)
                if self.stop == "E1c2":
                    continue
                p7, b_p7 = self.bank(7)
                for i in range(2):
                    k.op("pe", "matmul", [b_cqs, self.b_cmb], [b_p7], p7[:, :n], lhsT=ones_b, rhs=cqs[:, i, :n],
                         start=(i == 0), stop=(i == 1))
                k.op("act", "activation", [b_p7], [b_rs2], out=rs2[:, 0, :n], in_=p7[:, :n], func=AF.Sqrt,
                     scale=1.0 / 256, bias=EPS)
                k.op("pe", "matmul", [b_cqs, self.b_cmb], [b_p7], p7[:, :n], lhsT=ones_b, rhs=cqs[:, 2, :n],
                     start=True, stop=True)
                k.op("act", "activation", [b_p7], [b_rs2], out=rs2[:, 1, :n], in_=p7[:, :n], func=AF.Sqrt,
                     scale=1.0 / 128, bias=EPS)
                k.op("dve", "reciprocal", [b_rs2], [b_rs2], out=rs2[:, :, :n], in_=rs2[:, :, :n])
                for i3 in range(3):
                    k.op("dve", "scalar_tensor_tensor", [b_cqf, b_gn, b_rs2], [b_cqn], out=cqn[:, i3, :n],
                         in0=cqf[:, i3, :n], scalar=gn[:, i3:i3 + 1], in1=rs2[:, 0 if i3 < 2 else 1, :n],
                         op0=ALU.mult, op1=ALU.mult)
                if self.stop == "E1c":
                    continue
                for hh in range(16 if self.stop != "E1q" else 8):
                    h = hh % 8
                    pt, b_pt = self.bank(1 + hh % 2)
                    if hh < 8:
                        for i in range(2):
                            k.op("pe", "matmul", [b_wuq, b_cqn], [b_pt], pt[0:96, :n],
                                 lhsT=wuq[:, i, h * 96:(h + 1) * 96], rhs=cqn[:, i, :n], start=(i == 0), stop=(i == 1))
                    else:
                        k.op("pe", "matmul", [b_wukv, b_cqn], [b_pt], pt[0:64, :n],
                             lhsT=wukv[:, h * 128:h * 128 + 64], rhs=cqn[:, 2, :n], start=True, stop=True)
                        k.op("pe", "matmul", [b_krb, self.b_cmb], [b_pt], pt[64:96, :n], lhsT=idb, rhs=krb[:, :n],
                             start=True, stop=True)
                    qo, b_qo = qos[cnt % 2]
                    cnt += 1
                    gi = 3 if hh < 8 else 4
                    self.qk_norm_rope(pt, b_pt, 96, n, gn[0:96, gi:gi + 1], b_gn, rope[0:96, 0, t0:t0 + n],
                                      rope[0:96, 1, t0:t0 + n], b_rope, Rm, qo[0:96, :n], b_qo, wk, 3, 4)
                    if hh < 8:
                        k.dma("sp", self.qT[h, 0:96, t0:t0 + n], qo[0:96, :n], [b_qo], [self.b_qT])
                    else:
                        k.dma("sp", self.kT[h, 0:96, t0:t0 + n], qo[0:96, :n], [b_qo], [self.b_kT])
                if self.stop in ("E1q", "E1k"):
                    continue
                for s in range(n // 128):
                    pv, b_pv = self.bank(5 + s % 2)
                    vt, b_vt = vts[s % 2]
                    k.op("pe", "matmul", [b_wukv, b_cqn], [b_pv], pv, lhsT=cqn[:, 2, s * 128:(s + 1) * 128],
                         rhs=wukv[:].rearrange("p (h c) -> p h c", c=128)[:, :, 64:128], start=True, stop=True)
                    k.op("act", "activation", [b_pv], [b_vt], out=vt[:], in_=pv, func=AF.Copy)
                    k.dma("sp", self.vS[t0 + s * 128:t0 + (s + 1) * 128, :], vt[:], [b_vt], [self.b_vS])
            k.barrier()
        if self.stop and self.stop.startswith("E1"):
            return
        self.phase_s5(li, l)
        if self.stop in ("S5", "S5prep"):
            return
        with ExitStack() as st:
            wgl, b_wgl = self.tl(st, "wgl", [128, 4, 512], BF16)
            dsk, b_dsk = self.tl(st, "dsk", [128, 4], F32)
            ycs = [self.tl(st, f"yc{i}", [128, 4, 512], F32) for i in range(2)]
            ucs = [self.tl(st, f"uc{i}", [128, 4, 512], BF16) for i in range(2)]
            gpre, b_gpre = self.tl(st, "gpre", [128, 4, 512], F32)
            gb, b_gb = self.tl(st, "gb", [128, 4, 512], BF16)
            sig, b_sig = self.tl(st, "sig", [128, 512], F32)
            sos = [self.tl(st, f"so{i}", [128, 512], BF16) for i in range(2)]
            for kt in range(4):
                k.dma("pool", wgl[:, kt, :], w["w_glu"][kt * 128:(kt + 1) * 128, :], [], [b_wgl])
                k.dma("sp", dsk[:, kt:kt + 1], w["s5_d"][kt * 128:(kt + 1) * 128, :], [], [b_dsk])
            scnt = 0
            for ci, (t0, n) in enumerate(CHUNKS):
                yc, b_yc = ycs[ci % 2]
                uc, b_uc = ucs[ci % 2]
                k.dma("sp", yc[:, :, :n], self.yT[:, :, t0:t0 + n].rearrange("k p t -> p k t"), [self.b_yT], [b_yc])
                k.dma("sp", uc[:, :, :n], self.uT[:, :, t0:t0 + n].rearrange("k p t -> p k t"), [self.b_uT], [b_uc])
                for ut in range(4):
                    k.op("dve", "scalar_tensor_tensor", [b_uc, b_dsk, b_yc], [b_gpre], out=gpre[:, ut, :n],
                         in0=uc[:, ut, :n], scalar=dsk[:, ut:ut + 1], in1=yc[:, ut, :n], op0=ALU.mult, op1=ALU.add)
                k.op("act", "activation", [b_gpre], [b_gb], out=gb[:, :, :n], in_=gpre[:, :, :n],
                     func=AF.Gelu_apprx_tanh)
                for nt_ in range(4):
                    pt, b_pt = self.bank(nt_ % 2)
                    for kt in range(4):
                        k.op("pe", "matmul", [b_wgl, b_gb], [b_pt], pt[:, :n], lhsT=wgl[:, kt, nt_ * 128:(nt_ + 1) * 128],
                             rhs=gb[:, kt, :n], start=(kt == 0), stop=(kt == 3))
                    k.op("act", "activation", [b_pt], [b_sig], out=sig[:, :n], in_=pt[:, :n], func=AF.Sigmoid)
                    so, b_so = sos[scnt % 2]
                    scnt += 1
                    k.op("dve", "tensor_tensor", [b_sig, b_gb], [b_so], out=so[:, :n], in0=sig[:, :n],
                         in1=gb[:, nt_, :n], op=ALU.mult)
                    k.dma("sp", self.mixT[nt_, :, t0:t0 + n], so[:, :n], [b_so], [self.b_mixT])
            k.barrier()
        if self.stop == "GLU":
            return
        self.phase_attention(8, 8, lambda h: h, 96, 64, 96 ** -0.5, lambda h: (4 + h // 2, (h % 2) * 64))

    def _s5_table_thunks(self, k, sp2, q, Ct, b_Ct, St, b_St, scb, tA, b_tA, tB, b_tB, b_sp2):
        sc_, b_sc = scb
        th = []
        th.append(lambda: k.op("dve", "memset", [], [b_Ct], Ct[:, 0:1], 1.0))
        th.append(lambda: k.op("pool", "memset", [], [b_St], St[:, 0:1], 0.0))
        th.append(lambda: k.op("dve", "tensor_copy", [b_sp2], [b_sc], out=sc_[:, 0:2], in_=sp2[:, 1:3, q]))
        ln = 1
        cur = 0
        while ln < T:
            m = min(ln, T - ln)
            ck = sc_[:, cur:cur + 1]
            sk = sc_[:, cur + 1:cur + 2]
            for m0 in range(0, m, 1024):
                mm = min(1024, m - m0)

                def lvl(m0=m0, mm=mm, ln=ln, ck=ck, sk=sk):
                    k.op("act", "activation", [b_St, b_sc], [b_tA], out=tA[:, :mm], in_=St[:, m0:m0 + mm],
                         func=AF.Copy, scale=sk)
                    k.op("act", "activation", [b_Ct, b_sc], [b_tB], out=tB[:, 0, :mm], in_=Ct[:, m0:m0 + mm],
                         func=AF.Copy, scale=sk)
                    k.op("act", "activation", [b_St, b_sc], [b_tB], out=tB[:, 1, :mm], in_=St[:, m0:m0 + mm],
                         func=AF.Copy, scale=ck)
                    k.op("dve", "scalar_tensor_tensor", [b_Ct, b_sc, b_tA], [b_Ct], out=Ct[:, ln + m0:ln + m0 + mm],
                         in0=Ct[:, m0:m0 + mm], scalar=ck, in1=tA[:, :mm], op0=ALU.mult, op1=ALU.subtract)
                    k.op("pool", "tensor_tensor", [b_tB], [b_St], out=St[:, ln + m0:ln + m0 + mm], in0=tB[:, 0, :mm],
                         in1=tB[:, 1, :mm], op=ALU.add)
                th.append(lvl)
            nx = 2 if cur == 0 else 0

            def sqr(ck=ck, sk=sk, nx=nx):
                k.op("dve", "scalar_tensor_tensor", [b_sc], [b_sc], out=sc_[:, nx + 1:nx + 2], in0=ck, scalar=2.0,
                     in1=sk, op0=ALU.mult, op1=ALU.mult)
                k.op("dve", "tensor_tensor", [b_sc], [b_sc], out=sc_[:, 4:5], in0=sk, in1=sk, op=ALU.mult)
                k.op("dve", "tensor_tensor", [b_sc], [b_sc], out=sc_[:, nx:nx + 1], in0=ck, in1=ck, op=ALU.mult)
                k.op("dve", "tensor_tensor", [b_sc], [b_sc], out=sc_[:, nx:nx + 1], in0=sc_[:, nx:nx + 1],
                     in1=sc_[:, 4:5], op=ALU.subtract)
            th.append(sqr)
            cur = nx
            ln *= 2
        return th

    def phase_s5(self, li, l):
        nc, k = self.nc, self.k
        from contextlib import ExitStack
        w = self.W[l]
        idf = self.cm_f[:, 0, :]
        with ExitStack() as st:
            Bl, b_Bl = self.tl(st, "Bl", [128, 2, 4, 2, 128], BF16)
            Cl, b_Cl = self.tl(st, "Cl", [128, 2, 4, 2, 128], BF16)
            Bl3, b_Bl3 = self.tl(st, "Bl3", [128, 2, 4, 2, 128], BF16)
            Cl3, b_Cl3 = self.tl(st, "Cl3", [128, 2, 4, 2, 128], BF16)
            sp2, b_sp2 = self.tl(st, "sp2", [128, 3, 32], F32)
            self._b_s5raw = k.buf("s5raw")
            with ExitStack() as st1:
                raw2, _ = self.tl(st1, "raw2", [128, 3, 32], F32)
                raw1, _ = self.tl(st1, "raw1", [64, 3, 64], F32)
                nat1, b_nat1 = self.tl(st1, "nat1", [64, 2, 64], F32)
                nat2, b_nat2 = self.tl(st1, "nat2", [32, 2, 128], F32)
                ldb, b_ldb = self.tl(st1, "ldb", [128, 64], F32)
                for i, nm_ in enumerate(("a_re", "a_im")):
                    k.dma("sp", nat1[:, i, :], w[nm_].rearrange("d g n -> (d g) n"), [], [b_nat1])
                    k.dma("sp", nat2[:, i, :], w[nm_].rearrange("d (q s) n -> (d q) (s n)", s=2), [], [b_nat2])
                k.dma("sp", ldb[:], w["log_dt"].rearrange("d g o -> o (d g)").partition_broadcast(128), [], [b_ldb])
                for i in range(2):
                    pt, b_pt = self.bank(i)
                    k.op("pe", "transpose", [b_nat1, self.b_cmf], [b_pt], pt[0:64, 0:64], nat1[:, i, :], idf[0:64, 0:64])
                    k.op("dve", "tensor_copy", [b_pt], [self._b_s5raw], out=raw1[:, i, :], in_=pt[0:64, 0:64])
                    pt, b_pt = self.bank(2 + i)
                    k.op("pe", "transpose", [b_nat2, self.b_cmf], [b_pt], pt[:, 0:32], nat2[:, i, :], idf[0:32, 0:32])
                    k.op("dve", "tensor_copy", [b_pt], [self._b_s5raw], out=raw2[:, i, :], in_=pt[:, 0:32])
                k.op("dve", "tensor_copy", [b_ldb], [self._b_s5raw], out=raw1[:, 2, :], in_=ldb[0:64, :])
                for gs in range(2):
                    k.op("dve", "tensor_copy", [b_ldb], [self._b_s5raw], out=raw2[gs * 64:(gs + 1) * 64, 2, :],
                         in_=ldb[gs * 64:(gs + 1) * 64, :].rearrange("p (q s) -> p q s", s=2)[:, :, gs])
                pr2 = self.s5_params(st1, raw2[:, 0, :], raw2[:, 1, :], raw2[:, 2, :], 128, 32, "b")
                for i, nm_ in enumerate(("r", "c1", "s1")):
                    k.op("dve", "tensor_copy", [pr2["buf"]], [b_sp2], out=sp2[:, i, :], in_=pr2[nm_])
                pr1 = self.s5_params(st1, raw1[:, 0, :], raw1[:, 1, :], raw1[:, 2, :], 64, 64, "a")
                braw, b_braw = self.tl(st1, "braw", [64, 2, 64, 16], F32)
                bb, b_bb = self.tl(st1, "bb", [64, 2, 64, 16], F32)
                btmp, b_btmp = self.tl(st1, "btmp", [64, 64, 16], F32)
                for i, nm_ in enumerate(("b_re", "b_im")):
                    for d_ in range(2):
                        for gh in range(2):
                            k.dma("sp", braw[:, i, d_ * 32 + gh * 16:d_ * 32 + gh * 16 + 16, :],
                                  w[nm_][d_, gh * 16:(gh + 1) * 16].rearrange("g n p -> n g p"), [], [b_braw])
                zr = pr1["zr"].unsqueeze(2).broadcast_to([64, 64, 16])
                zi = pr1["zi"].unsqueeze(2).broadcast_to([64, 64, 16])
                pb1 = pr1["buf"]
                k.op("dve", "tensor_tensor", [b_braw, pb1], [b_bb], out=bb[:, 0], in0=braw[:, 0], in1=zr, op=ALU.mult)
                k.op("dve", "tensor_tensor", [b_braw, pb1], [b_btmp], out=btmp[:], in0=braw[:, 1], in1=zi, op=ALU.mult)
                k.op("dve", "tensor_tensor", [b_bb, b_btmp], [b_bb], out=bb[:, 0], in0=bb[:, 0], in1=btmp[:],
                     op=ALU.subtract)
                k.op("dve", "tensor_tensor", [b_braw, pb1], [b_bb], out=bb[:, 1], in0=braw[:, 1], in1=zr, op=ALU.mult)
                k.op("dve", "tensor_tensor", [b_braw, pb1], [b_btmp], out=btmp[:], in0=braw[:, 0], in1=zi, op=ALU.mult)
                k.op("dve", "tensor_tensor", [b_bb, b_btmp], [b_bb], out=bb[:, 1], in0=bb[:, 1], in1=btmp[:],
                     op=ALU.add)
                m2b = self.mask2[:, 0:2].unsqueeze(2).broadcast_to([128, 2, 64])
                tcnt = 0
                for d in range(2):
                    for ut in range(4):
                        for c in range(2):
                            pt, b_pt = self.bank(tcnt % 4)
                            tcnt += 1
                            g0 = d * 32 + ut * 8
                            k.op("pe", "transpose", [b_bb, self.b_cmf], [b_pt], pt[:, 0:64],
                                 bb[:, c, g0:g0 + 8, :].rearrange("n g p -> n (g p)"), idf[0:64, 0:64])
                            k.op("dve", "tensor_tensor", [b_pt, self.b_mask2], [b_Bl],
                                 out=Bl[:, d, ut, c, :].rearrange("p (s n) -> p s n", n=64),
                                 in0=pt[:, 0:64].unsqueeze(1).broadcast_to([128, 2, 64]), in1=m2b, op=ALU.mult)
                craw, b_craw = self.tl(st1, "craw", [128, 2, 8, 64], F32)
                cm_, b_cm_ = self.tl(st1, "cmk", [128, 2, 64], F32)
                k.dma("sp", craw[:, 0], w["c_re"].rearrange("d (u g) p n -> (g p) (d u) n", g=8), [], [b_craw])
                k.dma("sp", craw[:, 1], w["c_im"].rearrange("d (u g) p n -> (g p) (d u) n", g=8), [], [b_craw])
                for d in range(2):
                    for ut in range(4):
                        for c in range(2):
                            pt, b_pt = self.bank(tcnt % 4)
                            tcnt += 1
                            k.op("dve", "scalar_tensor_tensor", [b_craw, self.b_mask2], [b_cm_], out=cm_[:],
                                 in0=craw[:, c, d * 4 + ut, :].unsqueeze(1).broadcast_to([128, 2, 64]),
                                 scalar=(1.0 if c == 0 else -1.0), in1=m2b, op0=ALU.mult, op1=ALU.mult)
                            k.op("pe", "transpose", [b_cm_, self.b_cmf], [b_pt], pt[:, 0:128],
                                 cm_[:].rearrange("p s n -> p (s n)"), idf)
                            k.op("act", "activation", [b_pt], [b_Cl], out=Cl[:, d, ut, c, :], in_=pt[:, 0:128],
                                 func=AF.Copy)
                k.op("dve", "tensor_scalar", [b_Bl, self.b_mask2], [b_Bl3], out=Bl3[:].rearrange("p a b c n -> p (a b c n)"),
                     in0=Bl[:].rearrange("p a b c n -> p (a b c n)"), scalar1=self.mask2[:, 2:3], scalar2=None, op0=ALU.mult)
                k.op("dve", "tensor_copy", [b_Cl], [b_Cl3], out=Cl3[:].rearrange("p a b c n -> p (a b c n)"),
                     in_=Cl[:].rearrange("p a b c n -> p (a b c n)"))
                k.op("dve", "memset", [], [b_Cl3], Cl3[:].rearrange("p a b c n -> p (a b c) n")[:, :, 64:96], 0.0)
                k.barrier()
            if self.stop == "S5prep":
                return
            uTs2 = [self.tl(st, "uTs0", [128, T], BF16)] * 2
            tabs = [(self.tl(st, f"Ct{i}", [128, T], F32), self.tl(st, f"St{i}", [128, T], F32)) for i in range(2)]
            itc = 0
            Gr, b_Gr = self.tl(st, "Gr", [128, T], F32)
            Gi, b_Gi = self.tl(st, "Gi", [128, T], F32)
            Hs = [[self.tl(st, f"H{d}{c}", [128, T], BF16) for c in range(2)] for d in range(2)]
            bss = [[self.tl(st, f"bs{i}{c}", [128, 512], F32) for c in range(2)] for i in range(2)]
            mAs = [self.tl(st, f"mA{i}", [128, 2, 512], F32) for i in range(2)]
            mBs = [self.tl(st, f"mB{i}", [128, 2, 512], F32) for i in range(2)]
            tA, b_tA = self.tl(st, "tA", [128, 1024], F32)
            tB, b_tB = self.tl(st, "tB", [128, 2, 1024], F32)
            sc2 = [self.tl(st, f"scal{i}", [128, 8], F32) for i in range(2)]
            yst = [self.tl(st, f"yst{i}", [128, 512], F32) for i in range(2)]
            ycnt = 0
            for ut in range(4):
                uTs, b_uTs = uTs2[ut % 2]
                k.dma("sp", uTs[:], self.uT[ut], [self.b_uT], [b_uTs])
                for jq in range(4):
                    r0 = 32 * jq
                    for d in range(2):
                        (Ct, b_Ct), (St, b_St) = tabs[itc % 2]
                        itc += 1
                        q = d * 16 + ut * 4 + jq
                        rcol = sp2[:, 0, q:q + 1]
                        if itc == 1:
                            pend = self._s5_table_thunks(k, sp2, q, Ct, b_Ct, St, b_St, sc2[0], tA, b_tA, tB, b_tB, b_sp2)
                            for th in pend:
                                th()
                        nxt_thunks = []
                        if itc < 64:
                            it_n = itc
                            ut_n, rem = divmod(it_n, 8)
                            jq_n, d_n = divmod(rem, 2)
                            q_n = d_n * 16 + ut_n * 4 + jq_n
                            (Ct_n, b_Ct_n), (St_n, b_St_n) = tabs[it_n % 2]
                            nxt_thunks = self._s5_table_thunks(k, sp2, q_n, Ct_n, b_Ct_n, St_n, b_St_n, sc2[it_n % 2],
                                                               tA, b_tA, tB, b_tB, b_sp2)
                        per_step = (len(nxt_thunks) + 17) // 18

                        def drain(nth=per_step):
                            for _ in range(nth):
                                if nxt_thunks:
                                    nxt_thunks.pop(0)()
                        def tab(tt_, t0, n):
                            if d == 0:
                                return tt_[:, t0:t0 + n]
                            if t0 < NCTX:
                                return tt_[:, 0:NCTX][:, ::-1]
                            lo = T + NCTX - t0 - n
                            return tt_[:, lo:lo + n][:, ::-1]
                        for ci, (t0, n) in enumerate(CHUNKS):
                            pr_, b_pr = self.bank(4 + 2 * (ci % 2))
                            pi_, b_pi = self.bank(5 + 2 * (ci % 2))
                            bsr, b_bsr = bss[ci % 2][0]
                            bsi, b_bsi = bss[ci % 2][1]
                            if jq < 3:
                                lr, li_ = Bl[r0:r0 + 32, d, ut, 0, :], Bl[r0:r0 + 32, d, ut, 1, :]
                                ur = uTs[r0:r0 + 32, t0:t0 + n]
                            else:
                                lr, li_ = Bl3[64:128, d, ut, 0, :], Bl3[64:128, d, ut, 1, :]
                                ur = uTs[64:128, t0:t0 + n]
                            k.op("pe", "matmul", [b_Bl, b_Bl3, b_uTs], [b_pr], pr_[:, :n], lhsT=lr, rhs=ur,
                                 start=True, stop=True)
                            k.op("pe", "matmul", [b_Bl, b_Bl3, b_uTs], [b_pi], pi_[:, :n], lhsT=li_, rhs=ur,
                                 start=True, stop=True)
                            k.op("act", "activation", [b_pr], [b_bsr], out=bsr[:, :n], in_=pr_[:, :n], func=AF.Copy)
                            k.op("act", "activation", [b_pi], [b_bsi], out=bsi[:, :n], in_=pi_[:, :n], func=AF.Copy)
                            cT, sT = tab(Ct, t0, n), tab(St, t0, n)
                            mA, b_mA = mAs[ci % 2]
                            mB, b_mB = mBs[ci % 2]
                            k.op("dve", "tensor_tensor", [b_bsr, b_Ct], [b_mA], out=mA[:, 0, :n], in0=bsr[:, :n], in1=cT,
                                 op=ALU.mult)
                            k.op("dve", "tensor_tensor", [b_bsi, b_St], [b_mA], out=mA[:, 1, :n], in0=bsi[:, :n], in1=sT,
                                 op=ALU.mult)
                            k.op("dve", "tensor_tensor", [b_mA], [b_Gr], out=Gr[:, t0:t0 + n], in0=mA[:, 0, :n],
                                 in1=mA[:, 1, :n], op=ALU.add)
                            k.op("dve", "tensor_tensor", [b_bsi, b_Ct], [b_mB], out=mB[:, 0, :n], in0=bsi[:, :n], in1=cT,
                                 op=ALU.mult)
                            k.op("pool", "tensor_tensor", [b_bsr, b_St], [b_mB], out=mB[:, 1, :n], in0=bsr[:, :n], in1=sT,
                                 op=ALU.mult)
                            k.op("pool", "tensor_tensor", [b_mB], [b_Gi], out=Gi[:, t0:t0 + n], in0=mB[:, 0, :n],
                                 in1=mB[:, 1, :n], op=ALU.subtract)
                            drain()
                        for G_, b_G in ((Gr, b_Gr), (Gi, b_Gi)):
                            if d == 0:
                                segs = [(G_[:, 0:NCTX], 0.0), (G_[:, NCTX:T], G_[:, NCTX - 1:NCTX])]
                            else:
                                segs = [(G_[:, 0:NCTX][:, ::-1], 0.0), (G_[:, NCTX:T][:, ::-1], G_[:, 0:1])]
                            for seg, init in segs:
                                nn = seg.shape[1]
                                k.op("dve", "tensor_tensor_scan", [b_G, b_sp2], [b_G], out=seg,
                                     data0=rcol.broadcast_to([128, nn]), data1=seg, initial=init,
                                     op0=ALU.mult, op1=ALU.add)
                        Hr, b_Hr = Hs[d][0]
                        Hi, b_Hi = Hs[d][1]
                        for ci, (t0, n) in enumerate(CHUNKS):
                            cT, sT = tab(Ct, t0, n), tab(St, t0, n)
                            mA, b_mA = mAs[ci % 2]
                            mB, b_mB = mBs[ci % 2]
                            k.op("dve", "tensor_tensor", [b_Gr, b_Ct], [b_mA], out=mA[:, 0, :n], in0=Gr[:, t0:t0 + n],
                                 in1=cT, op=ALU.mult)
                            k.op("dve", "tensor_tensor", [b_Gi, b_St], [b_mA], out=mA[:, 1, :n], in0=Gi[:, t0:t0 + n],
                                 in1=sT, op=ALU.mult)
                            k.op("dve", "tensor_tensor", [b_mA], [b_Hr], out=Hr[:, t0:t0 + n], in0=mA[:, 0, :n],
                                 in1=mA[:, 1, :n], op=ALU.subtract)
                            k.op("dve", "tensor_tensor", [b_Gr, b_St], [b_mB], out=mB[:, 0, :n], in0=Gr[:, t0:t0 + n],
                                 in1=sT, op=ALU.mult)
                            k.op("pool", "tensor_tensor", [b_Gi, b_Ct], [b_mB], out=mB[:, 1, :n], in0=Gi[:, t0:t0 + n],
                                 in1=cT, op=ALU.mult)
                            k.op("pool", "tensor_tensor", [b_mB], [b_Hi], out=Hi[:, t0:t0 + n], in0=mB[:, 0, :n],
                                 in1=mB[:, 1, :n], op=ALU.add)
                            drain()
                        drain(10 ** 6)
                    for ci, (t0, n) in enumerate(CHUNKS):
                        py, b_py = self.bank(ci % 2)
                        i4 = 0
                        for d in range(2):
                            for c in range(2):
                                Hc, b_Hc = Hs[d][c]
                                if jq < 3:
                                    k.op("pe", "matmul", [b_Cl, b_Hc], [b_py], py[r0:r0 + 32, :n],
                                         lhsT=Cl[:, d, ut, c, r0:r0 + 32], rhs=Hc[:, t0:t0 + n],
                                         start=(i4 == 0), stop=(i4 == 3))
                                else:
                                    k.op("pe", "matmul", [b_Cl3, b_Hc], [b_py], py[64:128, :n],
                                         lhsT=Cl3[:, d, ut, c, 64:128], rhs=Hc[:, t0:t0 + n],
                                         start=(i4 == 0), stop=(i4 == 3))
                                i4 += 1
                        ys, b_ys = yst[ycnt % 2]
                        ycnt += 1
                        c0_ = r0 if jq < 3 else 64
                        k.op("act", "activation", [b_py], [b_ys], out=ys[c0_:r0 + 32, :n], in_=py[c0_:r0 + 32, :n],
                             func=AF.Copy)
                        k.dma("sp", self.yT[ut, r0:r0 + 32, t0:t0 + n], ys[r0:r0 + 32, :n], [b_ys], [self.b_yT])
            k.barrier()


def layer_inputs(inp, l):
    f = np.ascontiguousarray
    d = {}
    d[f"w_ada{l}"] = f(inp["w_ada"][l])
    d[f"b_ada{l}"] = f(inp["b_ada"][l][None, :])
    d[f"n1{l}"] = f(inp["norm1_g"][l][None, :])
    d[f"n2{l}"] = f(inp["norm2_g"][l][None, :])
    d[f"w_r{l}"] = f(np.concatenate([inp["moe_w_r1"][l], inp["moe_w_r2"][l]], axis=1))
    d[f"b_r{l}"] = f(np.concatenate([inp["moe_b_r1"][l], inp["moe_b_r2"][l]])[None, :])
    d[f"w_gate{l}"] = f(inp["moe_w_gate"][l].reshape(NEXP, 8, 128, 512).transpose(0, 2, 1, 3)).reshape(NEXP * 128, 4096)
    d[f"w_up{l}"] = f(inp["moe_w_up"][l].reshape(NEXP, 8, 128, 512).transpose(0, 2, 1, 3)).reshape(NEXP * 128, 4096)
    d[f"w_down{l}"] = f(inp["moe_w_down"][l].reshape(NEXP, 4, 128, D).transpose(0, 2, 1, 3)).reshape(NEXP * 128, 4096)
    i = l // 2
    if l % 2 == 1:
        d[f"w_out{l}"] = f(inp["w_out_o"][i])
        d[f"w_qkv{l}"] = f(inp["w_qkv_o"][i])
        d[f"qn{l}"] = f(inp["gqa_qn"][i][:, None])
        d[f"kn{l}"] = f(inp["gqa_kn"][i][:, None])
    else:
        d[f"w_out{l}"] = f(inp["w_out_e"][i])
        d[f"w_in{l}"] = f(inp["w_in_e"][i])
        d[f"a_re{l}"] = f(inp["s5_a_re"][i])
        d[f"a_im{l}"] = f(inp["s5_a_im"][i])
        d[f"log_dt{l}"] = f(inp["s5_log_dt"][i][:, :, None])
        d[f"b_re{l}"] = f(inp["s5_b_re"][i])
        d[f"b_im{l}"] = f(inp["s5_b_im"][i])
        d[f"c_re{l}"] = f(inp["s5_c_re"][i])
        d[f"c_im{l}"] = f(inp["s5_c_im"][i])
        d[f"s5_d{l}"] = f(inp["s5_d"][i][:, None])
        d[f"w_glu{l}"] = f(inp["s5_w_glu"][i])
        d[f"gq{l}"] = f(inp["mla_gq"][i][:, None])
        d[f"w_uq{l}"] = f(inp["mla_w_uq"][i])
        d[f"gkv{l}"] = f(inp["mla_gkv"][i][:, None])
        d[f"w_ukv{l}"] = f(inp["mla_w_ukv"][i])
        d[f"mqn{l}"] = f(inp["mla_qn"][i][:, None])
        d[f"mkn{l}"] = f(inp["mla_kn"][i][:, None])
    return d


def core_inputs(inp, b, shared):
    d = dict(shared)
    d["x"] = np.ascontiguousarray(inp["x"][b])
    d["ctx"] = np.ascontiguousarray(inp["ctx"][b])
    d["cvec"] = np.ascontiguousarray(np.stack([inp["c"][b], inp["c_ctx"]], axis=0))
    return d


def run_layers(inp, layers, batches, stop=None, trace=False):
    inp = {k_: np.asarray(v, dtype=np.float32) for k_, v in inp.items()}
    prog = Prog(layers, stop)
    nc = prog.build()
    shared = make_consts()
    for l in layers:
        shared.update(layer_inputs(inp, l))
    in_maps = []
    for b in batches:
        m = core_inputs(inp, b, shared)
        in_maps.append({name: m[name] for name in prog.din})
    if trace:
        res = run_bass_kernel_spmd(nc, in_maps, core_ids=list(range(len(batches))), trace=True)
        print("EXEC_TIME_NS", layers, stop, res.exec_time_ns)
    else:
        res = run_bass_kernel_spmd(nc, in_maps, core_ids=list(range(len(batches))))
    if stop:
        return [r for r in res.results]
    return [r["y"] for r in res.results]


def kernel(**inputs):
    outs = run_layers(inputs, [0, 1, 2, 3], [0, 1, 2, 3])
    return np.stack(outs, axis=0).astype(np.float32)
```
